# Optimizing a Trainium2 kernel written in Bass

```python
import jax, jax.numpy as jnp
from jax import lax
import numpy as np

D_MODEL = 1024
BATCH = 4
SEQ = 4096
DEPTH = 1

MIX_WIDTH = D_MODEL
HGRN_WIDTH = MIX_WIDTH // 2
HGRN_HEADS = 4
HGRN_HEAD_DIM = HGRN_WIDTH // HGRN_HEADS
HGRN_CHUNK = 64
GMLP_WIDTH = MIX_WIDTH - HGRN_WIDTH
GMLP_GROUPS = 4
GMLP_GROUP_DIM = GMLP_WIDTH // GMLP_GROUPS
GMLP_CHUNK = 128
IN_PROJ_WIDTH = 4 * HGRN_WIDTH + 2 * GMLP_WIDTH
N_EXPERTS = 32
TOP_K = 4
D_FF = D_MODEL
SWIGLU_LIMIT = 7.0
SWIGLU_ALPHA = 1.702
MOE_BLOCK = 256
EPS = 1e-6

kernel_name = "hybrid_hgrn2_gmlp_moe_adaln"


def rms_norm(x, g):
    xf = x.astype(jnp.float32)
    y = xf * lax.rsqrt(jnp.mean(xf * xf, axis=-1, keepdims=True) + EPS)
    return (y * g.astype(jnp.float32)).astype(x.dtype)


def layer_norm(x, g, b):
    xf = x.astype(jnp.float32)
    mu = jnp.mean(xf, axis=-1, keepdims=True)
    var = jnp.mean(jnp.square(xf - mu), axis=-1, keepdims=True)
    y = (xf - mu) * lax.rsqrt(var + EPS)
    return (y * g.astype(jnp.float32) + b.astype(jnp.float32)).astype(x.dtype)


def hgrn2_chunk_scan(q, k, v, logf):
    B, S, H, DK = q.shape
    DV = v.shape[-1]
    nc = S // HGRN_CHUNK

    def to_chunks(t):
        return t.reshape(B, nc, HGRN_CHUNK, H, t.shape[-1]).transpose(1, 0, 3, 2, 4)

    causal = jnp.tril(jnp.ones((HGRN_CHUNK, HGRN_CHUNK), dtype=bool))[:, :, None]

    def step(state, inp):
        qc, kc, vc, gc = inp
        b = jnp.cumsum(gc, axis=2)
        o_inter = jnp.einsum('bhtk,bhkv->bhtv', qc * jnp.exp(b), state)
        diff = b[:, :, :, None, :] - b[:, :, None, :, :]
        decay = jnp.exp(jnp.where(causal, diff, -jnp.inf))
        scores = jnp.einsum('bhtk,bhsk,bhtsk->bhts', qc, kc, decay)
        o_intra = jnp.einsum('bhts,bhsv->bhtv', scores, vc)
        b_last = b[:, :, -1:, :]
        k_dec = kc * jnp.exp(b_last - b)
        new_state = jnp.exp(b_last[:, :, 0, :])[..., None] * state + jnp.einsum('bhsk,bhsv->bhkv', k_dec, vc)
        return new_state, o_inter + o_intra

    state0 = jnp.zeros((B, H, DK, DV), jnp.float32)
    _, o = lax.scan(step, state0, (to_chunks(q), to_chunks(k), to_chunks(v), to_chunks(logf)))
    return o.transpose(1, 0, 3, 2, 4).reshape(B, S, H, DV)


def hgrn2_mixer(zq, zf, zi, zg, lb, norm_g):
    B, S, _ = zq.shape
    shp = (B, S, HGRN_HEADS, HGRN_HEAD_DIM)
    lb = lb.astype(jnp.float32)
    f = lb + (1.0 - lb) * jax.nn.sigmoid(zf.astype(jnp.float32))
    logf = jnp.log(f).reshape(shp)
    k = (1.0 - f).reshape(shp)
    q = zq.astype(jnp.float32).reshape(shp)
    v = zi.astype(jnp.float32).reshape(shp)
    o = hgrn2_chunk_scan(q, k, v, logf).astype(zq.dtype)
    o = rms_norm(o, norm_g.reshape(HGRN_HEADS, HGRN_HEAD_DIM))
    o = o * jax.nn.silu(zg).reshape(shp)
    return o.reshape(B, S, HGRN_WIDTH)


def gmlp_mixer(zu, zv, ln_g, ln_b, ws, bs, norm_g):
    B, S, _ = zu.shape
    nc = S // GMLP_CHUNK
    shp = (B, nc, GMLP_CHUNK, GMLP_GROUPS, GMLP_GROUP_DIM)
    u = jax.nn.gelu(zu, approximate=False)
    v = layer_norm(jax.nn.gelu(zv, approximate=False), ln_g, ln_b).reshape(shp)
    ws_causal = ws * jnp.tril(jnp.ones((GMLP_CHUNK, GMLP_CHUNK), ws.dtype))
    sv = jnp.einsum('gts,bnsgd->bntgd', ws_causal, v) + bs.T[None, None, :, :, None]
    y = u.reshape(shp) * sv
    y = rms_norm(y, norm_g.reshape(GMLP_GROUPS, GMLP_GROUP_DIM))
    return y.reshape(B, S, GMLP_WIDTH)


def moe_ffn(h, router_w, router_b, w_gate_up, b_gate_up, w_down, b_down):
    N, D = h.shape
    logits = h.astype(jnp.float32) @ router_w.astype(jnp.float32) + router_b.astype(jnp.float32)
    top_val, top_idx = lax.top_k(logits, TOP_K)
    gates = jax.nn.softmax(top_val, axis=-1)

    NK = N * TOP_K
    flat_e = top_idx.reshape(-1)
    flat_g = gates.reshape(-1)
    order = jnp.argsort(flat_e)
    sorted_e = flat_e[order]
    tok = (order // TOP_K).astype(jnp.int32)
    counts = jnp.bincount(flat_e, length=N_EXPERTS)
    padded = ((counts + MOE_BLOCK - 1) // MOE_BLOCK) * MOE_BLOCK
    start_sorted = jnp.cumsum(counts) - counts
    pad_end = jnp.cumsum(padded)
    start_pad = pad_end - padded
    dest = start_pad[sorted_e] + (jnp.arange(NK) - start_sorted[sorted_e])

    P = ((NK + N_EXPERTS * (MOE_BLOCK - 1) + MOE_BLOCK - 1) // MOE_BLOCK) * MOE_BLOCK
    n_blocks = P // MOE_BLOCK
    row_tok = jnp.full((P,), N, jnp.int32).at[dest].set(tok)
    row_gate = jnp.zeros((P,), jnp.float32).at[dest].set(flat_g[order])
    block_start = jnp.arange(n_blocks) * MOE_BLOCK
    block_expert = jnp.minimum(jnp.searchsorted(pad_end, block_start, side='right'), N_EXPERTS - 1)

    h_pad = jnp.concatenate([h, jnp.zeros((1, D), h.dtype)], axis=0)
    xb = h_pad[row_tok].reshape(n_blocks, MOE_BLOCK, D)

    def expert_block(args):
        xblk, e = args
        gu = xblk @ w_gate_up[e] + b_gate_up[e]
        gate, up = gu[:, :D_FF], gu[:, D_FF:]
        gate = jnp.minimum(gate, SWIGLU_LIMIT)
        up = jnp.clip(up, -SWIGLU_LIMIT, SWIGLU_LIMIT)
        glu = gate * jax.nn.sigmoid(SWIGLU_ALPHA * gate)
        return ((up + 1.0) * glu) @ w_down[e] + b_down[e]

    yb = lax.map(expert_block, (xb, block_expert)).reshape(P, D)
    yb = yb * row_gate.astype(yb.dtype)[:, None]
    out = jnp.zeros((N + 1, D), yb.dtype).at[row_tok].add(yb)
    return out[:N]


def setup_inputs(seed: int = 0) -> dict:
    key = jax.random.key(seed)
    ks = jax.random.split(key, 24)
    f32 = jnp.float32
    L = DEPTH
    nrm = lambda k, shape, s: (jax.random.normal(k, shape, f32) * s)
    return {
        "x": nrm(ks[0], (BATCH, SEQ, D_MODEL), 1.0),
        "c": nrm(ks[1], (BATCH, D_MODEL), 1.0),
        "ada_w": nrm(ks[2], (L, D_MODEL, 6 * D_MODEL), 0.5 * D_MODEL ** -0.5),
        "ada_b": nrm(ks[3], (L, 6 * D_MODEL), 0.02),
        "norm_mix_g": 1.0 + nrm(ks[4], (L, D_MODEL), 0.05),
        "w_in": nrm(ks[5], (L, D_MODEL, IN_PROJ_WIDTH), D_MODEL ** -0.5),
        "lb_params": nrm(ks[6], (L + 1, HGRN_WIDTH), 1.0),
        "hgrn_norm_g": 1.0 + nrm(ks[7], (L, HGRN_WIDTH), 0.05),
        "gmlp_ln_g": 1.0 + nrm(ks[8], (L, GMLP_WIDTH), 0.05),
        "gmlp_ln_b": nrm(ks[9], (L, GMLP_WIDTH), 0.02),
        "gmlp_ws": nrm(ks[10], (L, GMLP_GROUPS, GMLP_CHUNK, GMLP_CHUNK), GMLP_CHUNK ** -0.5),
        "gmlp_bs": 1.0 + nrm(ks[11], (L, GMLP_GROUPS, GMLP_CHUNK), 0.1),
        "gmlp_norm_g": 1.0 + nrm(ks[12], (L, GMLP_WIDTH), 0.05),
        "w_out": nrm(ks[13], (L, MIX_WIDTH, D_MODEL), MIX_WIDTH ** -0.5),
        "norm_ffn_g": 1.0 + nrm(ks[14], (L, D_MODEL), 0.05),
        "router_w": nrm(ks[15], (L, D_MODEL, N_EXPERTS), D_MODEL ** -0.5),
        "router_b": nrm(ks[16], (L, N_EXPERTS), 0.01),
        "w_gate_up": nrm(ks[17], (L, N_EXPERTS, D_MODEL, 2 * D_FF), D_MODEL ** -0.5),
        "b_gate_up": nrm(ks[18], (L, N_EXPERTS, 2 * D_FF), 0.02),
        "w_down": nrm(ks[19], (L, N_EXPERTS, D_FF, D_MODEL), D_FF ** -0.5),
        "b_down": nrm(ks[20], (L, N_EXPERTS, D_MODEL), 0.02),
        "final_g": 1.0 + nrm(ks[21], (D_MODEL,), 0.05),
    }


def reference(x, c, ada_w, ada_b, norm_mix_g, w_in, lb_params, hgrn_norm_g, gmlp_ln_g, gmlp_ln_b,
              gmlp_ws, gmlp_bs, gmlp_norm_g, w_out, norm_ffn_g, router_w, router_b, w_gate_up,
              b_gate_up, w_down, b_down, final_g):
    B, S, D = x.shape
    lb_all = jnp.cumsum(jax.nn.softmax(lb_params.astype(jnp.float32), axis=0), axis=0)
    c_act = jax.nn.silu(c)
    splits = [HGRN_WIDTH, 2 * HGRN_WIDTH, 3 * HGRN_WIDTH, 4 * HGRN_WIDTH, 4 * HGRN_WIDTH + GMLP_WIDTH]
    for l in range(DEPTH):
        mod = (c_act @ ada_w[l] + ada_b[l])[:, None, :]
        shift1, scale1, gate1, shift2, scale2, gate2 = jnp.split(mod, 6, axis=-1)

        h = rms_norm(x, norm_mix_g[l]) * (1.0 + scale1) + shift1
        z = h @ w_in[l]
        zq, zf, zi, zg, zu, zv = jnp.split(z, splits, axis=-1)
        y_hgrn = hgrn2_mixer(zq, zf, zi, zg, lb_all[l], hgrn_norm_g[l])
        y_gmlp = gmlp_mixer(zu, zv, gmlp_ln_g[l], gmlp_ln_b[l], gmlp_ws[l], gmlp_bs[l], gmlp_norm_g[l])
        y = jnp.concatenate([y_hgrn, y_gmlp], axis=-1) @ w_out[l]
        x = x + gate1 * y

        h = rms_norm(x, norm_ffn_g[l]) * (1.0 + scale2) + shift2
        y = moe_ffn(h.reshape(B * S, D), router_w[l], router_b[l], w_gate_up[l], b_gate_up[l],
                    w_down[l], b_down[l]).reshape(B, S, D)
        x = x + gate2 * y
    return rms_norm(x, final_g)
```

```python
import os
from contextlib import ExitStack

import numpy as np
import concourse.bass as bass
import concourse.mybir as mybir
from concourse.bass_utils import run_bass_kernel_spmd

F32 = mybir.dt.float32
BF16 = mybir.dt.bfloat16
I32 = mybir.dt.int32
AF = mybir.ActivationFunctionType
ALU = mybir.AluOpType

D = 1024
NT = 16
TOK = NT * 128
NE = 32
CAP = 384
NJ = CAP // 128
NB = (TOK * 4 + NE * (CAP - 1) + CAP - 1) // CAP
NROWS = NB * CAP
MAXBE = (TOK + CAP - 1) // CAP
EPS = 1e-6
BIGOFF = 1.0e6
DBG = bool(int(os.environ.get("MK_DBG", "0")))
NEXP_RUN = int(os.environ.get("MK_NEXP", str(NE)))
NPRE = int(os.environ.get("MK_NPRE", str(NT)))
NMAIN = int(os.environ.get("MK_NMAIN", str(NT)))
COMB = int(os.environ.get("MK_COMB", "1"))
CUT = int(os.environ.get("MK_CUT", "99"))
SKIP = int(os.environ.get("MK_SKIP", "1"))
VAR = os.environ.get("MK_VAR", "")
NEW = NE
NB_RUN = int(os.environ.get("MK_NB", str(NB)))


class Sched:
    ENGS = ("pe", "act", "dve", "pool", "sp")

    def __init__(self, nc, es):
        self.nc, self.es = nc, es
        self.rec = {e: [] for e in self.ENGS}
        self.chan = {}
        self.sems = {}
        self.lastw, self.readers = {}, {}
        self.known = {e: {} for e in self.ENGS}
        self.nsem = 0
        self.dma_final = {}
        self.dma_cur = {}
        self.dma_base = {}
        self.dma_open = {}
        self.grp = None
        self.cur_grp = {e: None for e in self.ENGS}
        self.grp_ap = None

    def _signal(self, ch, inc):
        c = self.chan.get(ch)
        if c is None or c[1] + inc > 30000:
            name = f"s{self.nsem}"
            self.nsem += 1
            self.sems[name] = self.es.enter_context(self.nc.semaphore(name))
            c = [name, 0]
            self.chan[ch] = c
        c[1] += inc
        return (c[0], c[1])

    def op(self, eng, fn, reads=(), writes=(), chan=None):
        if self.grp != self.cur_grp[eng]:
            self.cur_grp[eng] = self.grp
            self.known[eng] = {}
        deps = {}
        closing = set()

        def add(sv):
            if sv is None:
                return
            v = sv[1]
            if sv[0] in self.dma_cur:
                base = self.dma_base.get(sv[0], 0)
                if v <= base:
                    v = base
                else:
                    v = self.dma_cur[sv[0]]
                    closing.add(sv[0])
            if v > deps.get(sv[0], 0):
                deps[sv[0]] = v

        for k in reads:
            add(self.lastw.get(k))
        for k in writes:
            add(self.lastw.get(k))
            for r in self.readers.get(k, ()):
                add(r)
        own = self.chan.get(eng)
        waits = []
        for s, v in deps.items():
            if eng == "pe" and own is not None and s == own[0]:
                continue
            if self.known[eng].get(s, 0) >= v:
                continue
            self.known[eng][s] = v
            waits.append((s, v))
        sig = self._signal(chan if chan else eng, 16 if chan else 1)
        for s in closing:
            self.dma_open[s] = False
        if chan:
            if self.grp is None:
                self.dma_final[chan] = sig
            if not self.dma_open.get(sig[0], False):
                self.dma_open[sig[0]] = True
                self.dma_base[sig[0]] = sig[1] - 16
            self.dma_cur[sig[0]] = sig[1]
        self.rec[eng].append((waits, fn, sig, 16 if chan else 1, self.grp, bool(chan)))
        for k in reads:
            self.readers.setdefault(k, []).append(sig)
        for k in writes:
            self.lastw[k] = sig
            self.readers[k] = []

    def flush(self):
        for ch, sig in self.dma_final.items():
            if self.known["sp"].get(sig[0], 0) < sig[1]:
                self.known["sp"][sig[0]] = sig[1]
                self.rec["sp"].append(([sig], None, None, 0, None, False))
        self.dma_final = {}
        rec, sems = self.rec, self.sems

        def emit(engname):
            def emit_op(e, o):
                waits, fn, sig, inc = o[0], o[1], o[2], o[3]
                for sn, v in waits:
                    e.wait_ge(sems[sn], v)
                if fn is not None:
                    fn(e).then_inc(sems[sig[0]], inc)

            def body(e):
                if engname == "pool":
                    self.pool_bound = e.to_reg(NROWS - 1)
                    self.pool_wbound = e.to_reg(NE * D - 1)
                ops = rec[engname]
                reg = None
                if any(o[4] is not None for o in ops):
                    reg = e.alloc_register("nlive_" + engname)
                    e.reg_load(reg, self.grp_ap)
                i = 0
                while i < len(ops):
                    g = ops[i][4]
                    if g is None:
                        emit_op(e, ops[i])
                        i += 1
                        continue
                    j = i
                    while j < len(ops) and ops[j][4] == g:
                        j += 1
                    tot, base, dlast = {}, {}, {}
                    for o in ops[i:j]:
                        if o[1] is None:
                            continue
                        if o[5]:
                            dlast[o[2][0]] = o[2][1]
                        else:
                            base.setdefault(o[2][0], o[2][1] - o[3])
                            tot[o[2][0]] = tot.get(o[2][0], 0) + o[3]
                    with e.If_lt(reg, g + 1):
                        for sn, t in tot.items():
                            if base[sn] > 0:
                                e.wait_ge(sems[sn], base[sn])
                            e.sem_inc(sems[sn], t)
                    with e.Else():
                        for o in ops[i:j]:
                            emit_op(e, o)
                        for sn, v in dlast.items():
                            e.wait_ge(sems[sn], v)
                    i = j
            return body

        with self.nc.Block() as block:
            block.tensor(emit("pe"))
            block.scalar(emit("act"))
            block.vector(emit("dve"))
            block.gpsimd(emit("pool"))
            block.sync(emit("sp"))
        self.rec = {e: [] for e in self.ENGS}
        self.lastw, self.readers = {}, {}
        self.chan = {}
        self.known = {e: {} for e in self.ENGS}
        self.grp = None
        self.cur_grp = {e: None for e in self.ENGS}


def build():
    nc = bass.Bass("TRN2", target_bir_lowering=False)

    def din(name, shape, dt=F32):
        return nc.dram_tensor(name, list(shape), dt, kind="ExternalInput").ap()

    x_main = din("x_main", [TOK, D])
    x_pre = din("x_pre", [TOK, D])
    pmask_d = din("pmask", [128, 1])
    c_col_d = din("c_col", [128, 8])
    ada_w = din("ada_w", [D, 6 * D])
    ada_b_col_d = din("ada_b_col", [128, 48])
    ada_b_d = din("ada_b", [6 * D])
    g1_col_d = din("g1_col", [128, 8])
    g2_row_d = din("g2_row", [D])
    w_in = din("w_in", [D, 3072])
    lbp_d = din("lbp", [2, 512])
    hg_d = din("hg", [512])
    lng_d = din("lng", [512])
    lnb_d = din("lnb", [512])
    gng_d = din("gng", [512])
    wsT_d = din("wsT", [128, 4, 128])
    bs_col_d = din("bs_col", [128, 4])
    w_out = din("w_out", [D, D])
    rw_d = din("rw", [128, 8, 32])
    rb_d = din("rb", [32])
    wgu = din("wgu", [NE * D, 2 * D])
    bgu_tab = din("bgu_tab", [NE * 128, 16])
    wdn = din("wdn", [NE * D, D])
    bdn = din("bdn", [NE, D])
    fg_d = din("fg", [D])
    out_d = nc.dram_tensor("out", [TOK, D], F32, kind="ExternalOutput").ap()
    x1s = nc.dram_tensor("x1s", [TOK, D], F32, kind="ExternalOutput" if DBG else "Internal").ap()
    XS = nc.dram_tensor("XS", [NROWS, D], BF16, kind="Internal").ap()
    YS = nc.dram_tensor("YS", [NROWS, D], F32, kind="Internal").ap()
    XH = nc.dram_tensor("XH", [TOK, D], BF16, kind="Internal").ap()

    with ExitStack() as ES:
        S = Sched(nc, ES)

        def sb(es, name, shape, dt=F32):
            return es.enter_context(nc.sbuf_tensor("sb_" + name, list(shape), dt))

        def ps(es, name, shape, dt=F32):
            return es.enter_context(nc.psum_tensor("ps_" + name, list(shape), dt))

        def bc(ap, shape):
            return ap.to_broadcast(list(shape))

        ident_bf = sb(ES, "ident_bf", [128, 128], BF16)
        ident_f = sb(ES, "ident_f", [128, 128])
        modT = sb(ES, "modT", [128, 48])
        G1T = sb(ES, "G1T", [128, 8])
        gate2_rep = sb(ES, "gate2_rep", [128, D])
        fg_rep = sb(ES, "fg_rep", [128, D])
        lgt_all = sb(ES, "lgt_all", [128, NT, NE])
        mx8_all = sb(ES, "mx8_all", [128, NT, 8])
        gates_all = sb(ES, "gates_all", [128, NT, NE])
        pos_all = sb(ES, "pos_all", [128, NT, NE])
        base_rep = sb(ES, "base_rep", [128, NE])
        widx = sb(ES, "widx", [128, NB, 8], I32)
        bidx = sb(ES, "bidx", [128, NB], I32)
        sel = sb(ES, "sel", [128, NB])
        nlive_i = sb(ES, "nlive_i", [128, 1], I32)
        dest_all = sb(ES, "dest_all", [128, NT, 4], I32)
        gate_all = sb(ES, "gate_all", [128, NT, 4])
        eps_t = sb(ES, "eps_t", [128, 1])
        pmask = sb(ES, "pmask", [128, 1])

        with ExitStack() as E1:
            gate1_rep = sb(E1, "gate1_rep", [128, D])
            SH2_rep = sb(E1, "SH2_rep", [128, D])
            G2_rep = sb(E1, "G2_rep", [128, D])
            lb_rep = sb(E1, "lb_rep", [128, 512])
            oml_rep = sb(E1, "oml_rep", [128, 512])
            hg_rep = sb(E1, "hg_rep", [128, 512])
            lng_rep = sb(E1, "lng_rep", [128, 512])
            lnb_rep = sb(E1, "lnb_rep", [128, 512])
            gng_rep = sb(E1, "gng_rep", [128, 512])
            wsT_f = sb(E1, "wsT_f", [128, 4, 128])
            wsT_bf = sb(E1, "wsT_bf", [128, 4, 128], BF16)
            bs_col = sb(E1, "bs_col", [128, 4])
            Lst = sb(E1, "Lst", [128, 128])
            cones = sb(E1, "cones", [128, 2])
            cmask = sb(E1, "cmask", [128, 4, 64])
            Ust_bf = sb(E1, "Ust_bf", [128, 128], BF16)
            ones_bf = sb(E1, "ones_bf", [128, 128], BF16)
            ecol = sb(E1, "ecol", [128, NE])
            rw = sb(E1, "rw", [128, 8, 32])
            rb_rep = sb(E1, "rb_rep", [128, 32])
            c_col = sb(E1, "c_col", [128, 8])
            c_sig = sb(E1, "c_sig", [128, 8])
            c_act = sb(E1, "c_act", [128, 8])
            c_rep = sb(E1, "c_rep", [128, 8, 128], BF16)
            ada_b_col = sb(E1, "ada_b_col", [128, 48])
            g1_col = sb(E1, "g1_col", [128, 8])
            adaw_sb = [sb(E1, f"adaw{i}", [128, 8, 256], BF16) for i in range(2)]
            mod_rep = sb(E1, "mod_rep", [128, 256])
            win_sb = sb(E1, "win_sb", [128, 8, 3072], BF16)
            wout_sb = sb(E1, "wout_sb", [128, 8, D], BF16)
            Sst = sb(E1, "Sst", [128, 4, 128])
            Sp_bf = sb(E1, "Sp_bf", [128, 4, 128], BF16)
            xin = [sb(E1, f"xin{i}", [128, D]) for i in range(2)]
            junk = sb(E1, "junk", [128, D], BF16)
            ss = sb(E1, "ss", [128, 16])
            rs = sb(E1, "rs", [128, 16])
            xhat = sb(E1, "xhat", [128, D], BF16)
            hT = sb(E1, "hT", [128, 8, 128], BF16)
            q_sb = sb(E1, "q_sb", [128, 512])
            sg = sb(E1, "sg", [128, 512])
            f_sb = sb(E1, "f_sb", [128, 512])
            g_sb = sb(E1, "g_sb", [128, 512])
            kk = sb(E1, "kk", [128, 512])
            Eexp = sb(E1, "Eexp", [128, 512])
            decs = [sb(E1, f"dec{i}", [128, 8]) for i in range(2)]
            Kd_bfs = [sb(E1, f"Kd_bf{i}", [128, 512], BF16) for i in range(2)]
            Qm_bfs = [sb(E1, f"Qm_bf{i}", [128, 512], BF16) for i in range(2)]
            v_bfs = [sb(E1, f"v_bf{i}", [128, 512], BF16) for i in range(2)]
            ggs = [sb(E1, f"gg{i}", [128, 512]) for i in range(2)]
            u_sbs = [sb(E1, f"u_sb{i}", [128, 512]) for i in range(2)]
            vv = sb(E1, "vv", [128, 512])
            bst = sb(E1, "bst", [128, 6])
            bag = sb(E1, "bag", [128, 2])
            nmr = sb(E1, "nmr", [128, 1])
            vn = sb(E1, "vn", [128, 512])
            vn_bfs = [sb(E1, f"vn_bf{i}", [128, 512], BF16) for i in range(2)]
            QKT = sb(E1, "QKT", [128, 8, 128], BF16)
            scm = sb(E1, "scm", [128, 4, 64], BF16)
            o_t = sb(E1, "o_t", [128, 512])
            yg = sb(E1, "yg", [128, 512])
            cat = sb(E1, "cat", [128, D], BF16)
            catT = sb(E1, "catT", [128, 8, 128], BF16)
            x1t = [sb(E1, f"x1t{i}", [128, D]) for i in range(2)]
            xh2f = sb(E1, "xh2f", [128, D])
            xh2b = [sb(E1, f"xh2b{i}", [128, D], BF16) for i in range(2)]
            h2T = sb(E1, "h2T", [128, 8, 128])
            nmx = sb(E1, "nmx", [128, 1])
            msk = sb(E1, "msk", [128, NE])
            msk_bf = sb(E1, "msk_bf", [128, NE], BF16)
            eexp = sb(E1, "eexp", [128, NE])
            gsum = sb(E1, "gsum", [128, 1])
            pA = ps(E1, "pA", [128, 8, 128], BF16)
            pB = ps(E1, "pB", [128, 512])
            pC = ps(E1, "pC", [128, 512])
            pD = ps(E1, "pD", [128, 512])
            pE = ps(E1, "pE", [128, 512])
            pF = ps(E1, "pF", [128, 512])
            pG = ps(E1, "pG", [128, 512])
            pH = ps(E1, "pH", [128, 4, 128])
            zb = [pB, pC]
            zbk = ["pB", "pC"]
            zctr = [0]

            def zbank():
                i = 0 if "onebank" in VAR else zctr[0] % 2
                zctr[0] += 1
                return zb[i], zbk[i]

            S.op("pool", lambda e: e.memset(ident_f[:], 0.0), writes=["ident_f"])
            S.op("pool", lambda e: e.affine_select(out=ident_f[:], in_=ident_f[:], pattern=[[-1, 128]],
                                                    compare_op=ALU.not_equal, fill=1.0, base=0, channel_multiplier=1),
                 reads=["ident_f"], writes=["ident_f"])
            S.op("pool", lambda e: e.tensor_copy(ident_bf[:], ident_f[:]), reads=["ident_f"], writes=["ident_bf"])
            S.op("pool", lambda e: e.memset(eps_t[:], EPS), writes=["eps_t"])
            S.op("pool", lambda e: e.memset(Lst[:], 1.0), writes=["Lst"])
            S.op("pool", lambda e: e.affine_select(out=Lst[:], in_=Lst[:], pattern=[[-1, 128]], compare_op=ALU.is_gt,
                                                    fill=0.0, base=0, channel_multiplier=1), reads=["Lst"], writes=["Lst"])
            S.op("pool", lambda e: e.memset(Lst[64:128, 0:64], 0.0), reads=["Lst"], writes=["Lst"])
            S.op("pool", lambda e: e.memset(cones[:], 0.0), writes=["cones"])
            S.op("pool", lambda e: e.memset(cones[0:64, 0:1], 1.0), reads=["cones"], writes=["cones"])
            S.op("pool", lambda e: e.memset(cones[64:128, 1:2], 1.0), reads=["cones"], writes=["cones"])
            S.op("pool", lambda e: e.memset(cmask[:], 1.0), writes=["cmask"])
            for hf in range(2):
                S.op("pool", lambda e, hf=hf: e.affine_select(
                    out=cmask[hf * 64:(hf + 1) * 64], in_=cmask[hf * 64:(hf + 1) * 64], pattern=[[0, 4], [1, 64]],
                    compare_op=ALU.is_ge, fill=0.0, base=0, channel_multiplier=-1), reads=["cmask"], writes=["cmask"])
            S.op("pool", lambda e: e.memset(Ust_bf[:], 1.0), writes=["Ust"])
            S.op("pool", lambda e: e.affine_select(out=Ust_bf[:], in_=Ust_bf[:], pattern=[[1, 128]], compare_op=ALU.is_gt,
                                                    fill=0.0, base=0, channel_multiplier=-1), reads=["Ust"], writes=["Ust"])
            S.op("pool", lambda e: e.memset(ones_bf[:], 1.0), writes=["ones_bf"])
            S.op("pool", lambda e: e.iota(ecol[:], pattern=[[CAP, NE]], base=0, channel_multiplier=0,
                                          allow_small_or_imprecise_dtypes=True), writes=["ecol"])
            S.op("pool", lambda e: e.memset(base_rep[:], 0.0), writes=["base_rep"])
            S.op("pool", lambda e: e.memset(Sst[:], 0.0), writes=["Sst"])

            zt = sb(E1, "zt", [128, 1024], BF16)
            S.op("pool", lambda e: e.memset(zt[:], 0.0), writes=["zt"])
            XSv = XS.rearrange("(p r) d -> p (r d)", p=128)
            zfill = [("x", z0) for z0 in range(0, (NROWS // 128) * D, 1024)]
            YSv = YS.rearrange("(p r) d -> p (r d)", p=128)
            zfill += [("y", z0) for z0 in range(0, (NROWS // 128) * D, 512)]
            zt32 = zt[:].bitcast(F32)
            def ld(dst, src, key):
                S.op("sp", lambda e: e.dma_start(out=dst, in_=src), writes=[key], chan="ld_" + key)

            ld(pmask[:], pmask_d, "pmask")
            ld(c_col[:], c_col_d, "c_col")
            ld(ada_b_col[:], ada_b_col_d, "ada_b_col")
            ld(gate1_rep[:], ada_b_d[2 * D:3 * D].partition_broadcast(128), "gate_rep0")
            ld(gate2_rep[:], ada_b_d[5 * D:6 * D].partition_broadcast(128), "gate_rep1")
            ld(SH2_rep[:], ada_b_d[3 * D:4 * D].partition_broadcast(128), "gate_rep2")
            ld(xh2f[:], ada_b_d[4 * D:5 * D].partition_broadcast(128), "gate_rep3")
            ld(G2_rep[:], g2_row_d.partition_broadcast(128), "g2row")
            ld(g1_col[:], g1_col_d, "g1_col")
            ld(lb_rep[:], lbp_d[0].partition_broadcast(128), "lbp0")
            ld(oml_rep[:], lbp_d[1].partition_broadcast(128), "lbp1")
            ld(hg_rep[:], hg_d.partition_broadcast(128), "hg_rep")
            ld(lng_rep[:], lng_d.partition_broadcast(128), "lng_rep")
            ld(lnb_rep[:], lnb_d.partition_broadcast(128), "lnb_rep")
            ld(gng_rep[:], gng_d.partition_broadcast(128), "gng_rep")
            ld(wsT_f[:], wsT_d, "wsT_f")
            ld(bs_col[:], bs_col_d, "bs_col")
            ld(rw[:], rw_d, "rw")
            ld(rb_rep[:], rb_d.partition_broadcast(128), "rb_rep")
            ld(fg_rep[:], fg_d.partition_broadcast(128), "fg_rep")

            S.op("act", lambda e: e.activation(out=c_sig[:], in_=c_col[:], func=AF.Sigmoid), reads=["c_col"], writes=["c_sig"])
            S.op("dve", lambda e: e.tensor_tensor(out=c_act[:], in0=c_col[:], in1=c_sig[:], op=ALU.mult),
                 reads=["c_col", "c_sig"], writes=["c_act"])
            S.op("dve", lambda e: e.tensor_copy(c_rep[:], bc(c_act[:, :].unsqueeze(2), [128, 8, 128])),
                 reads=["c_act"], writes=["c_rep"])
            S.op("dve", lambda e: e.tensor_tensor(out=lb_rep[:], in0=lb_rep[:], in1=oml_rep[:], op=ALU.subtract),
                 reads=["lbp0", "lbp1"], writes=["lbd", "lbp0"])
            S.op("act", lambda e: e.activation(out=oml_rep[:], in_=lb_rep[:], func=AF.Sigmoid, scale=-1.0),
                 reads=["lbd", "lbp1"], writes=["oml_rep", "lbp1"])
            S.op("act", lambda e: e.activation(out=lb_rep[:], in_=lb_rep[:], func=AF.Sigmoid),
                 reads=["lbd", "oml_rep"], writes=["lbd", "lb_rep"])
            S.op("pool", lambda e: e.affine_select(out=wsT_bf[:], in_=wsT_f[:], pattern=[[0, 4], [1, 128]],
                                                    compare_op=ALU.is_ge, fill=0.0, base=0, channel_multiplier=-1),
                 reads=["wsT_f"], writes=["wsT_bf"])

            CW = 256

            def mod_chunk(jj):
                slot = jj % 2
                grp, off = (jj * CW) // D, (jj * CW) % D
                S.op("pool", lambda e, jj=jj, slot=slot: e.dma_start(
                    out=adaw_sb[slot][:], in_=ada_w[:, jj * CW:(jj + 1) * CW].rearrange("(kc p) n -> p kc n", p=128)),
                    writes=[f"adaw{slot}"], chan=f"ld_adaw{slot}")
                for kc in range(8):
                    S.op("pe", lambda e, kc=kc, slot=slot: e.matmul(pD[:, 0:CW], c_rep[:, kc, :], adaw_sb[slot][:, kc, :],
                                                                   start=(kc == 0), stop=(kc == 7)),
                         reads=["c_rep", f"adaw{slot}"], writes=["pD"])
                if grp >= 2:
                    which = {2: 0, 5: 1, 3: 2, 4: 3}[grp]
                    dst = [gate1_rep, gate2_rep, SH2_rep, xh2f][which]
                    S.op("dve", lambda e, dst=dst, off=off: e.tensor_tensor(out=dst[:, off:off + CW], in0=pD[:, 0:CW], in1=dst[:, off:off + CW], op=ALU.add),
                         reads=["pD", f"gate_rep{which}"], writes=[f"gate_rep{which}_{off // 512}"])
                else:
                    S.op("dve", lambda e: e.tensor_copy(mod_rep[:], pD[:, 0:CW]), reads=["pD"], writes=["mod_rep"])
                    for t4 in range(CW // 128):
                        S.op("pe", lambda e, t4=t4: e.transpose(pH[:, t4, :], mod_rep[:, t4 * 128:(t4 + 1) * 128], ident_f[:]),
                             reads=["mod_rep", "ident_f"], writes=["pH"])
                    c0 = jj * (CW // 128)
                    S.op("dve", lambda e, c0=c0: e.tensor_tensor(out=modT[:, c0:c0 + CW // 128], in0=pH[:, 0:CW // 128, 0],
                                                                 in1=ada_b_col[:, c0:c0 + CW // 128], op=ALU.add),
                         reads=["pH", "ada_b_col"], writes=[f"modT{jj}"])
            NEARLY = 2 * D // CW
            for jj in range(NEARLY):
                mod_chunk(jj)
            for kc in range(8):
                for hh in range(3):
                    S.op("pool", lambda e, kc=kc, hh=hh: e.dma_start(out=win_sb[:, kc, hh * 1024:(hh + 1) * 1024],
                                                                     in_=w_in[kc * 128:(kc + 1) * 128, hh * 1024:(hh + 1) * 1024]),
                         writes=[f"win_{kc}_{hh}"], chan="ld_win")
            S.op("pool", lambda e: e.dma_start(out=wout_sb[:], in_=w_out.rearrange("(kc p) n -> p kc n", p=128)),
                 writes=["wout"], chan="ld_wout")

            mk = [f"modT{jj}" for jj in range(8)]
            S.op("dve", lambda e: e.scalar_tensor_tensor(out=G1T[:], in0=modT[:, 8:16], scalar=1.0, in1=g1_col[:],
                                                         op0=ALU.add, op1=ALU.mult), reads=mk + ["g1_col"], writes=["G1T"])
            late = list(range(NEARLY, 6 * D // CW))

            def late_some(nmax):
                for _ in range(nmax):
                    if late:
                        mod_chunk(late.pop(0))
                        if not late:
                                    S.op("dve", lambda e: e.scalar_tensor_tensor(out=G2_rep[:], in0=xh2f[:], scalar=1.0, in1=G2_rep[:],
                                                                                 op0=ALU.add, op1=ALU.mult), reads=["gate_rep3_0", "gate_rep3_1", "g2row"], writes=["G2_rep", "xh2f"])

            SH1 = modT[:, 0:8]
            winall = [f"win_{kc}_{hh}" for kc in range(8) for hh in range(3)]

            def rstd_from(ssap, rsap, n, keys_r, key_w):
                S.op("act", lambda e: e.activation(out=rsap, in_=ssap, func=AF.Ln, scale=1.0 / n, bias=eps_t[:]),
                     reads=keys_r + ["eps_t"], writes=[key_w])
                S.op("act", lambda e: e.activation(out=rsap, in_=rsap, func=AF.Exp, scale=-0.5),
                     reads=[key_w], writes=[key_w])

            def stage1(n, i, prefix):
                p = n % 2
                xsrc = x_pre if prefix else x_main
                xi = xin[p]
                xk = f"xin{p}"
                S.op("sp", lambda e: e.dma_start(out=xi[:], in_=xsrc[i * 128:(i + 1) * 128, :]), writes=[xk], chan="ld_" + xk)
                S.op("act", lambda e: e.activation(out=junk[:], in_=xi[:], func=AF.Square, accum_out=ss[:, 0:1]),
                     reads=[xk], writes=["junk", "ss0"])
                if CUT <= 1:
                    return
                rstd_from(ss[:, 0:1], rs[:, 0:1], D, ["ss0"], "rs0")
                if CUT <= 2:
                    return
                S.op("dve", lambda e: e.tensor_scalar(out=xhat[:], in0=xi[:], scalar1=rs[:, 0:1], scalar2=None, op0=ALU.mult),
                     reads=[xk, "rs0"], writes=["xhat"])
                for kc in range(8):
                    S.op("pe", lambda e, kc=kc: e.transpose(pA[:, kc, :], xhat[:, kc * 128:(kc + 1) * 128], ident_bf[:]),
                         reads=["xhat", "ident_bf"], writes=["pA"])
                for kc in range(8):
                    S.op("act", lambda e, kc=kc: e.activation(out=hT[:, kc, :], in_=pA[:, kc, :], func=AF.Identity,
                                                              scale=G1T[:, kc:kc + 1], bias=modT[:, kc:kc + 1]),
                         reads=["pA", "G1T"] + mk, writes=["hT"])
                yield

                if CUT <= 3:
                    return

                def inproj(nb):
                    zp, zk = zbank()
                    for kc in range(8):
                        S.op("pe", lambda e, kc=kc, zp=zp: e.matmul(zp[:], hT[:, kc, :], win_sb[:, kc, nb * 512:(nb + 1) * 512],
                                                             start=(kc == 0), stop=(kc == 7)),
                             reads=["hT"] + winall, writes=[zk])
                    return zp, zk

                if not prefix:
                    zp, zk = inproj(0)
                    S.op("act", lambda e, zp=zp: e.activation(out=q_sb[:], in_=zp[:], func=AF.Identity), reads=[zk], writes=["q_sb"])
                zp, zk = inproj(1)
                S.op("act", lambda e, zp=zp: e.activation(out=sg[:], in_=zp[:], func=AF.Sigmoid), reads=[zk], writes=["sg"])
                S.op("dve", lambda e: e.tensor_tensor(out=f_sb[:], in0=sg[:], in1=oml_rep[:], op=ALU.mult),
                     reads=["sg", "oml_rep"], writes=["f_sb"])
                S.op("dve", lambda e: e.tensor_tensor(out=f_sb[:], in0=f_sb[:], in1=lb_rep[:], op=ALU.add),
                     reads=["f_sb", "lb_rep"], writes=["f_sb"])
                S.op("act", lambda e: e.activation(out=g_sb[:], in_=f_sb[:], func=AF.Ln), reads=["f_sb"], writes=["g_sb"])
                S.op("dve", lambda e: e.tensor_scalar(out=kk[:], in0=f_sb[:], scalar1=-1.0, scalar2=1.0, op0=ALU.mult, op1=ALU.add),
                     reads=["f_sb"], writes=["kk"])
                if CUT <= 4:
                    return
                zp, zk = inproj(2)
                if "vdve" in VAR:
                    S.op("dve", lambda e, zp=zp: e.tensor_copy(v_bfs[p][:], zp[:]), reads=[zk], writes=[f"v_bf{p}"])
                elif "vnone" in VAR:
                    pass
                else:
                    S.op("act", lambda e, zp=zp: e.activation(out=v_bfs[p][:], in_=zp[:], func=AF.Identity), reads=[zk], writes=[f"v_bf{p}"])
                if CUT <= 5:
                    return
                S.op("pe", lambda e: e.matmul(pD[:], Lst[:], g_sb[:], start=True, stop=True), reads=["Lst", "g_sb"], writes=["pD"])
                for h in range(4):
                    S.op("pe", lambda e, h=h: e.matmul(pE[:, 2 * h:2 * h + 2], g_sb[:, h * 128:(h + 1) * 128], cones[:],
                                                       start=True, stop=True), reads=["g_sb", "cones"], writes=["pE"])
                if CUT <= 6:
                    return
                S.op("act", lambda e: e.activation(out=Eexp[:], in_=pD[:], func=AF.Exp), reads=["pD"], writes=["Eexp"])
                S.op("act", lambda e: e.activation(out=decs[p][:], in_=pE[:, 0:8], func=AF.Exp), reads=["pE"], writes=[f"dec{p}"])
                S.op("dve", lambda e: e.tensor_tensor(out=Kd_bfs[p][:], in0=kk[:], in1=Eexp[:], op=ALU.mult),
                     reads=["kk", "Eexp"], writes=[f"Kd_bf{p}"])
                if not prefix:
                    S.op("act", lambda e: e.activation(out=Eexp[:], in_=pD[:], func=AF.Exp, scale=-1.0), reads=["pD"], writes=["Eexp"])
                    S.op("dve", lambda e: e.tensor_tensor(out=Qm_bfs[p][:], in0=q_sb[:], in1=Eexp[:], op=ALU.mult),
                         reads=["q_sb", "Eexp"], writes=[f"Qm_bf{p}"])
                    yield
                    zp, zk = inproj(3)
                    S.op("act", lambda e, zp=zp: e.activation(out=sg[:], in_=zp[:], func=AF.Sigmoid), reads=[zk], writes=["sg"])
                    S.op("dve", lambda e, zp=zp: e.tensor_tensor(out=ggs[p][:], in0=zp[:], in1=sg[:], op=ALU.mult), reads=[zk, "sg"], writes=[f"gg{p}"])
                    S.op("dve", lambda e: e.tensor_tensor(out=ggs[p][:], in0=ggs[p][:], in1=hg_rep[:], op=ALU.mult),
                         reads=[f"gg{p}", "hg_rep"], writes=[f"gg{p}"])
                    zp, zk = inproj(4)
                    S.op("act", lambda e, zp=zp: e.activation(out=u_sbs[p][:], in_=zp[:], func=AF.Gelu), reads=[zk], writes=[f"u_sb{p}"])
                    yield
                    zp, zk = inproj(5)
                    S.op("act", lambda e, zp=zp: e.activation(out=vv[:], in_=zp[:], func=AF.Gelu), reads=[zk], writes=["vv"])
                    S.op("dve", lambda e: e.bn_stats(bst[:], vv[:]), reads=["vv"], writes=["bst"])
                    S.op("dve", lambda e: e.bn_aggr(bag[:], bst[:]), reads=["bst"], writes=["bag"])
                    rstd_from(bag[:, 1:2], rs[:, 1:2], 1.0, ["bag"], "rs1")
                    S.op("dve", lambda e: e.scalar_tensor_tensor(out=nmr[:], in0=bag[:, 0:1], scalar=-1.0, in1=rs[:, 1:2],
                                                                 op0=ALU.mult, op1=ALU.mult), reads=["bag", "rs1"], writes=["nmr"])
                    S.op("act", lambda e: e.activation(out=vn[:], in_=vv[:], func=AF.Identity, scale=rs[:, 1:2], bias=nmr[:]),
                         reads=["vv", "rs1", "nmr"], writes=["vn"])
                    S.op("dve", lambda e: e.tensor_tensor(out=vn[:], in0=vn[:], in1=lng_rep[:], op=ALU.mult),
                         reads=["vn", "lng_rep"], writes=["vn"])
                    S.op("dve", lambda e: e.tensor_tensor(out=vn_bfs[p][:], in0=vn[:], in1=lnb_rep[:], op=ALU.add),
                         reads=["vn", "lnb_rep"], writes=[f"vn_bf{p}"])
            def stage2(n, i, prefix):
                p = n % 2
                xi = xin[p]
                xk = f"xin{p}"
                if not prefix:
                    for h in range(4):
                        S.op("pe", lambda e, h=h: e.transpose(pA[:, h, :], Qm_bfs[p][:, h * 128:(h + 1) * 128], ident_bf[:]),
                             reads=[f"Qm_bf{p}", "ident_bf"], writes=["pA"])
                    for h in range(4):
                        S.op("pe", lambda e, h=h: e.transpose(pA[:, 4 + h, :], Kd_bfs[p][:, h * 128:(h + 1) * 128], ident_bf[:]),
                             reads=[f"Kd_bf{p}", "ident_bf"], writes=["pA"])
                    S.op("act", lambda e: e.activation(out=QKT[:], in_=pA[:], func=AF.Identity), reads=["pA"], writes=["QKT"])
                    for c in range(2):
                        cs = slice(c * 64, (c + 1) * 64)
                        for h in range(4):
                            S.op("pe", lambda e, h=h, cs=cs: e.matmul(pE[cs, 64 + h * 64:128 + h * 64], QKT[:, 4 + h, cs], QKT[:, h, cs],
                                                                      start=True, stop=True), reads=["QKT"], writes=["pE"])
                    S.op("dve", lambda e: e.tensor_tensor(out=scm[:], in0=pE[:, 64:320].rearrange("p (h t) -> p h t", h=4),
                                                          in1=cmask[:], op=ALU.mult), reads=["pE", "cmask"], writes=["scm"])
                yield
                for c in range(2):
                    cs = slice(c * 64, (c + 1) * 64)
                    S.op("dve", lambda e, c=c: e.tensor_tensor(out=Sst[:], in0=Sst[:],
                                                               in1=bc(decs[p][:, c::2].unsqueeze(2), [128, 4, 128]), op=ALU.mult),
                         reads=["Sst", f"dec{p}"], writes=["Sst"])
                    if not prefix:
                        S.op("act", lambda e: e.activation(out=Sp_bf[:], in_=Sst[:], func=AF.Identity), reads=["Sst"], writes=["Sp_bf"])
                        for h in range(4):
                            hs = slice(h * 128, (h + 1) * 128)
                            S.op("pe", lambda e, h=h, cs=cs, hs=hs: e.matmul(pF[cs, hs], QKT[:, h, cs], Sp_bf[:, h, :], start=True, stop=False),
                                 reads=["QKT", "Sp_bf"], writes=["pF"])
                            S.op("pe", lambda e, h=h, cs=cs, hs=hs: e.matmul(pF[cs, hs], scm[cs, h, :], v_bfs[p][cs, hs], start=False, stop=True),
                                 reads=["scm", f"v_bf{p}"], writes=["pF"])
                    for h in range(4):
                        hs = slice(h * 128, (h + 1) * 128)
                        S.op("pe", lambda e, cs=cs, hs=hs: e.matmul(pG[:, hs], Kd_bfs[p][cs, hs], v_bfs[p][cs, hs], start=True, stop=True),
                             reads=[f"Kd_bf{p}", f"v_bf{p}"], writes=["pG"])
                    S.op("dve", lambda e: e.tensor_tensor(out=Sst[:].rearrange("p h v -> p (h v)"), in0=Sst[:].rearrange("p h v -> p (h v)"),
                                                          in1=pG[:], op=ALU.add), reads=["Sst", "pG"], writes=["Sst"])
                if prefix:
                    return
                yield
                S.op("act", lambda e: e.activation(out=o_t[:], in_=pF[:], func=AF.Identity), reads=["pF"], writes=["o_t"])
                for h in range(4):
                    S.op("act", lambda e, h=h: e.activation(out=junk[:, h * 128:(h + 1) * 128], in_=o_t[:, h * 128:(h + 1) * 128],
                                                            func=AF.Square, accum_out=ss[:, 2 + h:3 + h]),
                         reads=["o_t"], writes=["junk", f"ssh{h}"])
                rstd_from(ss[:, 2:6], rs[:, 2:6], 128.0, [f"ssh{h}" for h in range(4)], "rsh")
                S.op("dve", lambda e: e.tensor_tensor(out=o_t[:].rearrange("p (h v) -> p h v", h=4), in0=o_t[:].rearrange("p (h v) -> p h v", h=4),
                                                      in1=bc(rs[:, 2:6].unsqueeze(2), [128, 4, 128]), op=ALU.mult),
                     reads=["o_t", "rsh"], writes=["o_t"])
                S.op("dve", lambda e: e.tensor_tensor(out=cat[:, 0:512], in0=o_t[:], in1=ggs[p][:], op=ALU.mult),
                     reads=["o_t", f"gg{p}"], writes=["cat_h"])
                yield
                zp, zk = zbank()
                for g in range(4):
                    gs = slice(g * 128, (g + 1) * 128)
                    S.op("pe", lambda e, g=g, gs=gs, zp=zp: e.matmul(zp[:, gs], wsT_bf[:, g, :], vn_bfs[p][:, gs], start=True, stop=True),
                         reads=["wsT_bf", f"vn_bf{p}"], writes=[zk])
                for g in range(4):
                    gs = slice(g * 128, (g + 1) * 128)
                    S.op("dve", lambda e, g=g, gs=gs, zp=zp: e.scalar_tensor_tensor(out=yg[:, gs], in0=zp[:, gs], scalar=bs_col[:, g:g + 1],
                                                                                   in1=u_sbs[p][:, gs], op0=ALU.add, op1=ALU.mult),
                         reads=[zk, "bs_col", f"u_sb{p}"], writes=["yg"])
                for g in range(4):
                    S.op("act", lambda e, g=g: e.activation(out=junk[:, g * 128:(g + 1) * 128], in_=yg[:, g * 128:(g + 1) * 128],
                                                            func=AF.Square, accum_out=ss[:, 6 + g:7 + g]),
                         reads=["yg"], writes=["junk", f"ssg{g}"])
                rstd_from(ss[:, 6:10], rs[:, 6:10], 128.0, [f"ssg{g}" for g in range(4)], "rsg")
                S.op("dve", lambda e: e.tensor_tensor(out=yg[:].rearrange("p (h v) -> p h v", h=4), in0=yg[:].rearrange("p (h v) -> p h v", h=4),
                                                      in1=bc(rs[:, 6:10].unsqueeze(2), [128, 4, 128]), op=ALU.mult),
                     reads=["yg", "rsg"], writes=["yg"])
                S.op("dve", lambda e: e.tensor_tensor(out=cat[:, 512:1024], in0=yg[:], in1=gng_rep[:], op=ALU.mult),
                     reads=["yg", "gng_rep"], writes=["cat_g"])
                yield
                for kc in range(8):
                    S.op("pe", lambda e, kc=kc: e.transpose(pA[:, kc, :], cat[:, kc * 128:(kc + 1) * 128], ident_bf[:]),
                         reads=["cat_h", "cat_g", "ident_bf"], writes=["pA"])
                S.op("act", lambda e: e.activation(out=catT[:], in_=pA[:], func=AF.Identity), reads=["pA"], writes=["catT"])
                x1 = x1t[p]
                x1k = f"x1t{p}"
                for half in range(2):
                    zp, zk = zbank()
                    hsl = slice(half * 512, (half + 1) * 512)
                    for kc in range(8):
                        S.op("pe", lambda e, kc=kc, zp=zp, hsl=hsl: e.matmul(zp[:], catT[:, kc, :], wout_sb[:, kc, hsl],
                                                                             start=(kc == 0), stop=(kc == 7)),
                             reads=["catT", "wout"], writes=[zk])
                    S.op("dve", lambda e, zp=zp, hsl=hsl: e.tensor_tensor(out=x1[:, hsl], in0=zp[:], in1=gate1_rep[:, hsl], op=ALU.mult),
                         reads=[zk, f"gate_rep0_{half}"], writes=[x1k + f"_{half}"])
                    S.op("dve", lambda e, hsl=hsl: e.tensor_tensor(out=x1[:, hsl], in0=x1[:, hsl], in1=xi[:, hsl], op=ALU.add),
                         reads=[x1k + f"_{half}", xk], writes=[x1k + f"_{half}"])
                x1keys = [x1k + "_0", x1k + "_1"]
                S.op("sp", lambda e: e.dma_start(out=x1s[i * 128:(i + 1) * 128, :], in_=x1[:]), reads=x1keys, writes=[f"x1s{i}"],
                     chan="st_" + x1k)
                yield
                S.op("act", lambda e: e.activation(out=junk[:], in_=x1[:], func=AF.Square, accum_out=ss[:, 10:11]),
                     reads=x1keys, writes=["junk", "ss10"])
                rstd_from(ss[:, 10:11], rs[:, 10:11], D, ["ss10"], "rs10")
                xb = xh2b[p]
                xbk = f"xh2b{p}"
                S.op("dve", lambda e: e.tensor_scalar(out=xh2f[:], in0=x1[:], scalar1=rs[:, 10:11], scalar2=None, op0=ALU.mult),
                     reads=x1keys + ["rs10"], writes=["xh2f"])
                S.op("dve", lambda e: e.tensor_tensor(out=xh2f[:], in0=xh2f[:], in1=G2_rep[:], op=ALU.mult), reads=["xh2f", "G2_rep"], writes=["xh2f"])
                S.op("dve", lambda e: e.tensor_tensor(out=xh2f[:], in0=xh2f[:], in1=SH2_rep[:], op=ALU.add), reads=["xh2f", "gate_rep2_0", "gate_rep2_1"], writes=["xh2f"])
                S.op("act", lambda e: e.activation(out=xb[:], in_=xh2f[:], func=AF.Identity), reads=["xh2f"], writes=[xbk])
                for hf in range(2):
                    for t4 in range(4):
                        kc = hf * 4 + t4
                        S.op("pe", lambda e, kc=kc, t4=t4: e.transpose(pH[:, t4, :], xh2f[:, kc * 128:(kc + 1) * 128], ident_f[:]),
                             reads=["xh2f", "ident_f"], writes=["pH"])
                    ksl = slice(hf * 4, hf * 4 + 4)
                    S.op("dve", lambda e, ksl=ksl: e.tensor_copy(h2T[:, ksl, :], pH[:]), reads=["pH"], writes=[f"h2T{hf}"])
                for kc in range(8):
                    S.op("pe", lambda e, kc=kc: e.matmul(pE[:, 320:352], h2T[:, kc, :], rw[:, kc, :], start=(kc == 0), stop=(kc == 7)),
                         reads=["h2T0", "h2T1", "rw"], writes=["pE"])
                lg_i = lgt_all[:, i, :]
                mx_i = mx8_all[:, i, :]
                gt_i = gates_all[:, i, :]
                ps_i = pos_all[:, i, :]
                S.op("dve", lambda e: e.tensor_tensor(out=lg_i, in0=pE[:, 320:352], in1=rb_rep[:], op=ALU.add),
                     reads=["pE", "rb_rep"], writes=[f"lgt{i}"])
                S.op("dve", lambda e: e.max(mx_i, lg_i), reads=[f"lgt{i}"], writes=[f"mx8{i}"])
                S.op("dve", lambda e: e.tensor_scalar(out=msk[:], in0=lg_i, scalar1=mx8_all[:, i, 3:4], scalar2=None, op0=ALU.is_ge),
                     reads=[f"lgt{i}", f"mx8{i}"], writes=["msk"])
                S.op("dve", lambda e: e.tensor_copy(msk_bf[:], msk[:]), reads=["msk"], writes=["msk_bf"])
                S.op("dve", lambda e: e.tensor_scalar(out=nmx[:], in0=mx8_all[:, i, 0:1], scalar1=-1.0, scalar2=None, op0=ALU.mult),
                     reads=[f"mx8{i}"], writes=["nmx"])
                S.op("act", lambda e: e.activation(out=eexp[:], in_=lg_i, func=AF.Exp, bias=nmx[:]), reads=[f"lgt{i}", "nmx"], writes=["eexp"])
                S.op("dve", lambda e: e.scalar_tensor_tensor(out=eexp[:], in0=eexp[:], scalar=1.0, in1=msk[:], op0=ALU.mult, op1=ALU.mult, accum_out=gsum[:]),
                     reads=["eexp", "msk"], writes=["eexp", "gsum"])
                S.op("dve", lambda e: e.reciprocal(gsum[:], gsum[:]), reads=["gsum"], writes=["gsum"])
                S.op("dve", lambda e: e.tensor_scalar(out=gt_i, in0=eexp[:], scalar1=gsum[:, 0:1], scalar2=None, op0=ALU.mult),
                     reads=["eexp", "gsum"], writes=[f"gates{i}"])
                S.op("pe", lambda e: e.matmul(pE[:, 352:384], Ust_bf[:], msk_bf[:], start=True, stop=True), reads=["Ust", "msk_bf"], writes=["pE"])
                S.op("pe", lambda e: e.matmul(pE[:, 384:416], ones_bf[:], msk_bf[:], start=True, stop=True), reads=["ones_bf", "msk_bf"], writes=["pE"])
                S.op("dve", lambda e: e.tensor_tensor(out=ps_i, in0=pE[:, 352:384], in1=base_rep[:], op=ALU.add),
                     reads=["pE", "base_rep"], writes=[f"pos{i}"])
                S.op("dve", lambda e: e.tensor_tensor(out=base_rep[:], in0=base_rep[:], in1=pE[:, 384:416], op=ALU.add),
                     reads=["pE", "base_rep"], writes=["base_rep"])
                S.op("sp", lambda e: e.dma_start(out=XH[i * 128:(i + 1) * 128, :], in_=xb[:]), reads=[xbk], writes=[f"XH{i}"], chan="st_" + xbk)

            jobs = [(i, True) for i in range(NPRE)] + [(i, False) for i in range(NMAIN)]

            def drive(gens):
                live = list(gens)
                while live:
                    for g in list(live):
                        try:
                            next(g)
                        except StopIteration:
                            live.remove(g)

            def stage2_gen(n):
                i, pre = jobs[n]
                if (not pre) and i == 0:
                    S.op("dve", lambda e: e.tensor_scalar(out=Sst[:], in0=Sst[:], scalar1=pmask[:, 0:1], scalar2=None, op0=ALU.mult),
                         reads=["Sst", "pmask"], writes=["Sst"])
                return stage2(n, i, pre)

            zper = (len(zfill) + max(len(jobs), 1) - 1) // max(len(jobs), 1)

            def zero_some():
                for _ in range(zper):
                    if zfill:
                        which, z0 = zfill.pop(0)
                        if which == "x":
                            S.op("sp", lambda e, z0=z0: e.dma_start(out=XSv[:, z0:z0 + 1024], in_=zt[:]), reads=["zt"], writes=[f"XSz{z0}"], chan="st_zt")
                        else:
                            S.op("sp", lambda e, z0=z0: e.dma_start(out=YSv[:, z0:z0 + 512], in_=zt32), reads=["zt"], writes=[f"YSz{z0}"], chan="st_zt")

            if jobs:
                drive([stage1(0, *jobs[0])])
            for n in range(len(jobs)):
                zero_some()
                late_some(len(late) if not jobs[n][1] else 1)
                gens = []
                if n + 1 < len(jobs):
                    gens.append(stage1(n + 1, *jobs[n + 1]))
                gens.append(stage2_gen(n))
                drive(gens)
            while zfill:
                zero_some() if zper else zfill.clear()
            late_some(len(late))
            S.flush()

        with ExitStack() as ED:
            nbe = sb(ED, "nbe", [128, NE])
            tmp32 = sb(ED, "tmp32", [128, NE])
            padded = sb(ED, "padded", [128, NE])
            ones32 = sb(ED, "ones32", [128, NE])
            pad_end = sb(ED, "pad_end", [128, NE])
            start_pad = sb(ED, "start_pad", [128, NE])
            kblk = sb(ED, "kblk", [128, NB])
            cmp = sb(ED, "cmp", [128, NB, NE])
            be_rep = sb(ED, "be_rep", [128, NB])
            kcp = sb(ED, "kcp", [128, 8])
            pcol = sb(ED, "pcol", [128, 1])
            widx_f = sb(ED, "widx_f", [128, NB, 8])
            bidx_f = sb(ED, "bidx_f", [128, NB])
            destf = sb(ED, "destf", [128, NE])
            oh = sb(ED, "oh", [128, NE])
            dk = sb(ED, "dk", [128, 4])
            junk32 = sb(ED, "junk32", [128, NE])
            xb2 = [sb(ED, f"xb2_{i}", [128, D], BF16) for i in range(2)]

            S.op("dve", lambda e: e.tensor_scalar(out=nbe[:], in0=base_rep[:], scalar1=0.5, scalar2=None, op0=ALU.is_gt), writes=["nbe"])
            for j in range(1, MAXBE):
                S.op("dve", lambda e, j=j: e.tensor_scalar(out=tmp32[:], in0=base_rep[:], scalar1=float(j * CAP) + 0.5, scalar2=None, op0=ALU.is_gt),
                     writes=["tmp32"])
                S.op("dve", lambda e: e.tensor_tensor(out=nbe[:], in0=nbe[:], in1=tmp32[:], op=ALU.add), reads=["nbe", "tmp32"], writes=["nbe"])
            S.op("dve", lambda e: e.tensor_scalar(out=padded[:], in0=nbe[:], scalar1=float(CAP), scalar2=None, op0=ALU.mult), reads=["nbe"], writes=["padded"])
            S.op("pool", lambda e: e.memset(ones32[:], 1.0), writes=["ones32"])
            nlive_f = sb(ED, "nlive_f", [128, 1])
            S.op("dve", lambda e: e.tensor_reduce(out=nlive_f[:], in_=nbe[:], axis=mybir.AxisListType.X, op=ALU.add), reads=["nbe"], writes=["nlive_f"])
            S.op("dve", lambda e: e.tensor_copy(nlive_i[:], nlive_f[:]), reads=["nlive_f"], writes=["nlive_i"])
            S.op("dve", lambda e: e.tensor_tensor_scan(out=pad_end[:], data0=ones32[:], data1=padded[:], initial=0.0, op0=ALU.mult, op1=ALU.add),
                 reads=["ones32", "padded"], writes=["pad_end"])
            S.op("dve", lambda e: e.tensor_tensor(out=start_pad[:], in0=pad_end[:], in1=padded[:], op=ALU.subtract),
                 reads=["pad_end", "padded"], writes=["start_pad"])
            S.op("pool", lambda e: e.iota(kblk[:], pattern=[[CAP, NB]], base=0, channel_multiplier=0, allow_small_or_imprecise_dtypes=True), writes=["kblk"])
            S.op("dve", lambda e: e.tensor_tensor(out=cmp[:], in0=bc(pad_end[:, :].unsqueeze(1), [128, NB, NE]),
                                                  in1=bc(kblk[:, :].unsqueeze(2), [128, NB, NE]), op=ALU.is_le),
                 reads=["pad_end", "kblk"], writes=["cmp"])
            S.op("dve", lambda e: e.tensor_reduce(out=be_rep[:], in_=cmp[:], axis=mybir.AxisListType.X, op=ALU.add), reads=["cmp"], writes=["be_rep"])
            S.op("dve", lambda e: e.tensor_scalar(out=be_rep[:], in0=be_rep[:], scalar1=float(NE - 1), scalar2=None, op0=ALU.min), reads=["be_rep"], writes=["be_rep"])
            S.op("pool", lambda e: e.iota(kcp[:], pattern=[[128, 8]], base=0, channel_multiplier=1, allow_small_or_imprecise_dtypes=True), writes=["kcp"])
            S.op("pool", lambda e: e.iota(pcol[:], pattern=[[0, 1]], base=0, channel_multiplier=1, allow_small_or_imprecise_dtypes=True), writes=["pcol"])
            S.op("dve", lambda e: e.tensor_scalar(out=bidx_f[:], in0=be_rep[:], scalar1=float(D), scalar2=None, op0=ALU.mult), reads=["be_rep"], writes=["bidx_f"])
            S.op("dve", lambda e: e.tensor_tensor(out=widx_f[:], in0=bc(bidx_f[:, :].unsqueeze(2), [128, NB, 8]),
                                                  in1=bc(kcp[:, :].unsqueeze(1), [128, NB, 8]), op=ALU.add),
                 reads=["bidx_f", "kcp"], writes=["widx_f"])
            S.op("dve", lambda e: e.tensor_copy(widx[:], widx_f[:]), reads=["widx_f"], writes=["widx"])
            S.op("dve", lambda e: e.tensor_scalar(out=bidx_f[:], in0=be_rep[:], scalar1=128.0, scalar2=pcol[:, 0:1], op0=ALU.mult, op1=ALU.add),
                 reads=["be_rep", "pcol", "widx_f"], writes=["bidx_f"])
            S.op("dve", lambda e: e.tensor_copy(bidx[:], bidx_f[:]), reads=["bidx_f"], writes=["bidx"])
            S.op("dve", lambda e: e.tensor_scalar(out=sel[:], in0=be_rep[:], scalar1=pcol[:, 0:1], scalar2=None, op0=ALU.is_equal),
                 reads=["be_rep", "pcol"], writes=["sel"])
            for i in range(NMAIN):
                xb = xb2[i % 2]
                xbk = f"xb2_{i % 2}"
                S.op("sp", lambda e, i=i, xb=xb: e.dma_start(out=xb[:], in_=XH[i * 128:(i + 1) * 128, :]), writes=[xbk], chan="ld_" + xbk)
                S.op("dve", lambda e, i=i: e.tensor_tensor(out=destf[:], in0=pos_all[:, i, :], in1=start_pad[:], op=ALU.add),
                     reads=["start_pad"], writes=["destf"])
                for k in range(4):
                    S.op("dve", lambda e, i=i, k=k: e.tensor_scalar(out=oh[:], in0=lgt_all[:, i, :], scalar1=mx8_all[:, i, k:k + 1], scalar2=None, op0=ALU.is_equal),
                         writes=["oh"])
                    S.op("dve", lambda e, k=k: e.scalar_tensor_tensor(out=junk32[:], in0=oh[:], scalar=1.0, in1=destf[:], op0=ALU.mult, op1=ALU.mult,
                                                                      accum_out=dk[:, k:k + 1]), reads=["oh", "destf"], writes=["junk32", f"dk{k}"])
                    S.op("dve", lambda e, i=i, k=k: e.scalar_tensor_tensor(out=junk32[:], in0=oh[:], scalar=1.0, in1=gates_all[:, i, :], op0=ALU.mult, op1=ALU.mult,
                                                                           accum_out=gate_all[:, i, k:k + 1]), reads=["oh"], writes=["junk32", f"gate_all{i}_{k}"])
                S.op("dve", lambda e, i=i: e.tensor_copy(dest_all[:, i, :], dk[:]), reads=[f"dk{k}" for k in range(4)], writes=[f"dest_all{i}"])
                for k in range(4):
                    S.op("pool", lambda e, i=i, k=k, xb=xb: e.indirect_dma_start(
                        out=XS[:, :], out_offset=bass.IndirectOffsetOnAxis(ap=dest_all[:, i, k:k + 1], axis=0),
                        in_=xb[:, :], in_offset=None, bounds_check=S.pool_bound, oob_is_err=False),
                        reads=[xbk, f"dest_all{i}"], writes=[f"XS_{i}_{k}"], chan="sc_" + xbk)
            S.flush()

        with ExitStack() as E2:
            wg_sb = [sb(E2, f"wg{i}", [128, 8, 2 * D], BF16) for i in range(2)]
            wd_sb = [sb(E2, f"wd{i}", [128, 8, D], BF16) for i in range(2)]
            xg = [sb(E2, f"xg{i}", [128, NJ, D], BF16) for i in range(2)]
            bgc = [sb(E2, f"bgc{i}", [128, 16]) for i in range(2)]
            sel_full = sb(E2, "sel_full", [32, NB, 128], BF16)
            bd_f = sb(E2, "bd_f", [32, D])
            bd_bf = sb(E2, "bd_bf", [32, D], BF16)
            XTs = [sb(E2, f"XT{i}", [128, 8, CAP], BF16) for i in range(2)]
            bgc7 = [sb(E2, f"bgc7_{i}", [128, 8]) for i in range(2)]
            AT = sb(E2, "AT", [128, 8, CAP], BF16)
            gmin = [sb(E2, f"gmin{i}", [128, CAP]) for i in range(2)]
            sgm = [sb(E2, f"sgm{i}", [128, CAP]) for i in range(2)]
            up1 = [sb(E2, f"up1{i}", [128, CAP]) for i in range(2)]
            yst = [sb(E2, f"yst{i}", [128, NJ, D]) for i in range(2)]
            qA = ps(E2, "qA", [128, 8, 128], BF16)
            qg = [ps(E2, f"qg{i}", [128, 512]) for i in range(2)]
            qu = [ps(E2, f"qu{i}", [128, 512]) for i in range(2)]
            qy = [ps(E2, f"qy{i}", [128, 512]) for i in range(2)]

            S.op("sp", lambda e: e.dma_start(out=bd_f[:], in_=bdn), writes=["bd_f"], chan="ld_bd_f")
            S.op("dve", lambda e: e.tensor_copy(bd_bf[:], bd_f[:]), reads=["bd_f"], writes=["bd_bf"])
            S.op("dve", lambda e: e.tensor_copy(sel_full[:], bc(sel[0:32, :].unsqueeze(2), [32, NB, 128])), writes=["sel_full"])

            def prefetch(k):
                sl = k % 2
                for kc in range(8):
                    S.op("pool", lambda e, kc=kc: e.indirect_dma_start(
                        out=wg_sb[sl][:, kc, :], out_offset=None, in_=wgu[:, :],
                        in_offset=bass.IndirectOffsetOnAxis(ap=widx[:, k, kc:kc + 1], axis=0),
                        bounds_check=S.pool_wbound, oob_is_err=False), writes=[f"wg{sl}_{kc}"], chan=f"ld_wg{sl}")
                for kc in range(8):
                    S.op("pool", lambda e, kc=kc: e.indirect_dma_start(
                        out=wd_sb[sl][:, kc, :], out_offset=None, in_=wdn[:, :],
                        in_offset=bass.IndirectOffsetOnAxis(ap=widx[:, k, kc:kc + 1], axis=0),
                        bounds_check=S.pool_wbound, oob_is_err=False), writes=[f"wd{sl}_{kc}"], chan=f"ld_wd{sl}")
                S.op("pool", lambda e: e.indirect_dma_start(
                    out=bgc[sl][:, :], out_offset=None, in_=bgu_tab[:, :],
                    in_offset=bass.IndirectOffsetOnAxis(ap=bidx[:, k:k + 1], axis=0),
                    bounds_check=S.pool_wbound, oob_is_err=False), writes=[f"bgc{sl}"], chan=f"ld_bgc{sl}")
                S.op("sp", lambda e: e.dma_start(out=xg[sl][:], in_=XS[k * CAP:(k + 1) * CAP, :].rearrange("(j p) d -> p j d", p=128)),
                     writes=[f"xg{sl}"], chan=f"ld_xg{sl}")

            if NB_RUN > 0:
                prefetch(0)

            def xprep(k):
                sl = k % 2
                XT = XTs[sl]
                for j in range(NJ):
                    for kc in range(8):
                        S.op("pe", lambda e, j=j, kc=kc: e.transpose(qA[:, kc, :], xg[sl][:, j, kc * 128:(kc + 1) * 128], ident_bf[:]),
                             reads=[f"xg{sl}"], writes=["qA"])
                    S.op("act", lambda e, j=j: e.activation(out=XT[:, :, j * 128:(j + 1) * 128], in_=qA[:], func=AF.Identity),
                         reads=["qA"], writes=[f"XT{sl}_{j}"])
                S.op("dve", lambda e: e.tensor_scalar(out=bgc7[sl][:], in0=bgc[sl][:, 8:16], scalar1=7.0, scalar2=None, op0=ALU.add),
                     reads=[f"bgc{sl}"], writes=[f"bgc7_{sl}"])

            def block(k):
                sl = k % 2
                XT = XTs[sl]
                if k + 1 < NB_RUN:
                    prefetch(k + 1)
                xtk = [f"XT{sl}_{j}" for j in range(NJ)]
                wgk = [f"wg{sl}_{kc}" for kc in range(8)]
                wdk = [f"wd{sl}_{kc}" for kc in range(8)]
                for fc in range(8):
                    b = fc % 2
                    for kc in range(8):
                        S.op("pe", lambda e, fc=fc, kc=kc, b=b: e.matmul(qg[b][:, 0:CAP], wg_sb[sl][:, kc, fc * 128:(fc + 1) * 128], XT[:, kc, :],
                                                                         start=(kc == 0), stop=(kc == 7)),
                             reads=xtk + wgk, writes=[f"qg{b}"])
                    for kc in range(8):
                        S.op("pe", lambda e, fc=fc, kc=kc, b=b: e.matmul(qu[b][:, 0:CAP], wg_sb[sl][:, kc, D + fc * 128:D + (fc + 1) * 128], XT[:, kc, :],
                                                                         start=(kc == 0), stop=(kc == 7)),
                             reads=xtk + wgk, writes=[f"qu{b}"])
                    S.op("dve", lambda e, fc=fc, b=b: e.tensor_scalar(out=gmin[b][:], in0=qg[b][:, 0:CAP], scalar1=bgc[sl][:, fc:fc + 1], scalar2=7.0,
                                                                      op0=ALU.add, op1=ALU.min), reads=[f"qg{b}", f"bgc{sl}"], writes=[f"gmin{b}"])
                    S.op("act", lambda e, b=b: e.activation(out=sgm[b][:], in_=gmin[b][:], func=AF.Silu, scale=1.702),
                         reads=[f"gmin{b}"], writes=[f"sgm{b}"])
                    S.op("act", lambda e, fc=fc, b=b: e.activation(out=up1[b][:], in_=qu[b][:, 0:CAP], func=AF.Relu, bias=bgc7[sl][:, fc:fc + 1]),
                         reads=[f"qu{b}", f"bgc7_{sl}"], writes=[f"up1{b}"])
                    S.op("dve", lambda e, b=b: e.tensor_scalar(out=up1[b][:], in0=up1[b][:], scalar1=14.0, scalar2=-6.0, op0=ALU.min, op1=ALU.add),
                         reads=[f"up1{b}"], writes=[f"up1{b}"])
                    S.op("dve", lambda e, fc=fc, b=b: e.scalar_tensor_tensor(out=AT[:, fc, :], in0=sgm[b][:], scalar=1.0 / 1.702, in1=up1[b][:],
                                                                             op0=ALU.mult, op1=ALU.mult),
                         reads=[f"up1{b}", f"sgm{b}"], writes=[f"AT{fc}"])
                if k + 1 < NB_RUN:
                    xprep(k + 1)
                atk = [f"AT{fc}" for fc in range(8)]
                ys = yst[sl]
                n = 0
                for j in range(NJ):
                    for half in range(2):
                        b = n % 2
                        n += 1
                        hsl = slice(half * 512, (half + 1) * 512)
                        for fc in range(8):
                            S.op("pe", lambda e, j=j, fc=fc, b=b, hsl=hsl: e.matmul(qy[b][:], AT[:, fc, j * 128:(j + 1) * 128], wd_sb[sl][:, fc, hsl],
                                                                                   start=(fc == 0), stop=False),
                                 reads=atk + wdk, writes=[f"qy{b}"])
                        S.op("pe", lambda e, b=b, hsl=hsl: e.matmul(qy[b][:], sel_full[:, k, :], bd_bf[:, hsl], start=False, stop=True),
                             reads=["bd_bf", "sel_full"], writes=[f"qy{b}"])
                        S.op("act", lambda e, j=j, b=b, hsl=hsl: e.activation(out=ys[:, j, hsl], in_=qy[b][:], func=AF.Identity),
                             reads=[f"qy{b}"], writes=[f"yst{sl}_{j}_{half}"])
                S.op("sp", lambda e: e.dma_start(out=YS[k * CAP:(k + 1) * CAP, :].rearrange("(j p) d -> p j d", p=128), in_=ys[:]),
                     reads=[f"yst{sl}_{j}_{h}" for j in range(NJ) for h in range(2)], writes=[f"YS{k}"], chan=f"st_yst{sl}")

            for i2 in range(2):
                S.op("pool", lambda e, i2=i2: e.memset(yst[i2][:], 0.0), writes=[f"yst{i2}_{j}_{h}" for j in range(NJ) for h in range(2)])
            if NB_RUN > 0:
                xprep(0)
            S.grp_ap = nlive_i[0:1, 0:1]
            for k in range(NB_RUN):
                S.grp = k if SKIP else None
                block(k)
            S.grp = None
            S.flush()

        with ExitStack() as E3:
            x1c = [sb(E3, f"x1c{i}", [128, D]) for i in range(2)]
            yk = [[sb(E3, f"yk{i}_{k}", [128, D]) for k in range(4)] for i in range(2)]
            acc = [sb(E3, f"acc{i}", [128, D]) for i in range(2)]
            acc2 = [sb(E3, f"accb{i}", [128, D]) for i in range(2)]
            ot = [sb(E3, f"ot{i}", [128, D]) for i in range(2)]
            cjunk = sb(E3, "cjunk", [128, D], BF16)
            css = sb(E3, "css", [128, 2])
            crs = sb(E3, "crs", [128, 2])
            for i2 in range(2):
                for k in range(4):
                    S.op("pool", lambda e, i2=i2, k=k: e.memset(yk[i2][k][:], 0.0), writes=[f"yk{i2}_{k}"])
            for i in range(NMAIN if COMB else 0):
                b = i % 2
                S.op("sp", lambda e, i=i, b=b: e.dma_start(out=x1c[b][:], in_=x1s[i * 128:(i + 1) * 128, :]), writes=[f"x1c{b}"], chan=f"ld_x1c{b}")
                for k in range(4):
                    S.op("pool", lambda e, i=i, b=b, k=k: e.indirect_dma_start(
                        out=yk[b][k][:, :], out_offset=None, in_=YS[:, :],
                        in_offset=bass.IndirectOffsetOnAxis(ap=dest_all[:, i, k:k + 1], axis=0),
                        bounds_check=S.pool_bound, oob_is_err=False), writes=[f"yk{b}_{k}"], chan=f"ld_yk{b}_{k}")
                S.op("act", lambda e, i=i, b=b: e.activation(out=acc[b][:], in_=yk[b][0][:], func=AF.Identity, scale=gate_all[:, i, 0:1]),
                     reads=[f"yk{b}_0"], writes=[f"acc{b}"])
                for k in range(1, 4):
                    S.op("dve", lambda e, i=i, b=b, k=k: e.scalar_tensor_tensor(out=acc[b][:], in0=yk[b][k][:], scalar=gate_all[:, i, k:k + 1], in1=acc[b][:],
                                                                               op0=ALU.mult, op1=ALU.add), reads=[f"yk{b}_{k}", f"acc{b}"], writes=[f"acc{b}"])
                S.op("dve", lambda e, b=b: e.tensor_tensor(out=acc2[b][:], in0=acc[b][:], in1=gate2_rep[:], op=ALU.mult), reads=[f"acc{b}"], writes=[f"accb{b}"])
                S.op("dve", lambda e, b=b: e.tensor_tensor(out=acc2[b][:], in0=acc2[b][:], in1=x1c[b][:], op=ALU.add), reads=[f"accb{b}", f"x1c{b}"], writes=[f"accb{b}"])
                S.op("act", lambda e, b=b: e.activation(out=cjunk[:], in_=acc2[b][:], func=AF.Square, accum_out=css[:, b:b + 1]), reads=[f"accb{b}"], writes=["cjunk", f"css{b}"])
                S.op("act", lambda e, b=b: e.activation(out=crs[:, b:b + 1], in_=css[:, b:b + 1], func=AF.Ln, scale=1.0 / D, bias=eps_t[:]), reads=[f"css{b}"], writes=[f"crs{b}"])
                S.op("act", lambda e, b=b: e.activation(out=crs[:, b:b + 1], in_=crs[:, b:b + 1], func=AF.Exp, scale=-0.5), reads=[f"crs{b}"], writes=[f"crs{b}"])
                S.op("dve", lambda e, b=b: e.scalar_tensor_tensor(out=ot[b][:], in0=acc2[b][:], scalar=crs[:, b:b + 1], in1=fg_rep[:], op0=ALU.mult, op1=ALU.mult),
                     reads=[f"accb{b}", f"crs{b}"], writes=[f"ot{b}"])
                S.op("sp", lambda e, i=i, b=b: e.dma_start(out=out_d[i * 128:(i + 1) * 128, :], in_=ot[b][:]), reads=[f"ot{b}"], writes=[f"out{i}"],
                     chan=f"st_ot{b}")
            S.flush()
    return nc


def make_in_maps(x, c, ada_w, ada_b, norm_mix_g, w_in, lb_params, hgrn_norm_g, gmlp_ln_g, gmlp_ln_b,
                 gmlp_ws, gmlp_bs, gmlp_norm_g, w_out, norm_ffn_g, router_w, router_b, w_gate_up,
                 b_gate_up, w_down, b_down, final_g):
    f = lambda a: np.ascontiguousarray(np.asarray(a, dtype=np.float32))
    x = f(x)
    c = f(c)
    col = lambda v: f(np.asarray(v, np.float32).reshape(-1, 128).T)
    shared = dict(
        ada_w=f(ada_w[0]), ada_b_col=col(ada_b[0]), ada_b=f(ada_b[0]),
        g1_col=col(norm_mix_g[0]), g2_row=f(norm_ffn_g[0]),
        w_in=f(w_in[0]), lbp=f(lb_params), hg=f(hgrn_norm_g[0]), lng=f(gmlp_ln_g[0]), lnb=f(gmlp_ln_b[0]),
        gng=f(gmlp_norm_g[0]),
        wsT=f(np.transpose(np.asarray(gmlp_ws[0], np.float32), (2, 0, 1))),
        bs_col=f(np.asarray(gmlp_bs[0], np.float32).T),
        w_out=f(w_out[0]),
        rw=f(np.asarray(router_w[0], np.float32).reshape(8, 128, NE).transpose(1, 0, 2)),
        rb=f(router_b[0]),
        wgu=f(np.asarray(w_gate_up[0], np.float32).reshape(NE * D, 2 * D)),
        bgu_tab=f(np.asarray(b_gate_up[0], np.float32).reshape(NE, 16, 128).transpose(0, 2, 1).reshape(NE * 128, 16)),
        wdn=f(np.asarray(w_down[0], np.float32).reshape(NE * D, D)), bdn=f(b_down[0]), fg=f(final_g),
    )
    maps = []
    for core in range(8):
        b, half = core // 2, core % 2
        m = dict(shared)
        m["x_main"] = f(x[b, half * TOK:(half + 1) * TOK])
        m["x_pre"] = f(x[b, 0:TOK])
        m["pmask"] = np.full((128, 1), float(half), np.float32)
        m["c_col"] = col(c[b])
        maps.append(m)
    return maps


def kernel(**inputs):
    maps = make_in_maps(**inputs)
    nc = build()
    res = run_bass_kernel_spmd(nc, maps, core_ids=list(range(8)))
    out = np.empty((4, 2 * TOK, D), np.float32)
    for core in range(8):
        b, half = core // 2, core % 2
        out[b, half * TOK:(half + 1) * TOK] = res.results[core]["out"]
    if DBG:
        kernel.dbg = [r for r in res.results]
    return out
```

```python
import os
from contextlib import ExitStack

import numpy as np
import concourse.bass as bass
import concourse.mybir as mybir
from concourse.bass_utils import run_bass_kernel_spmd

F32 = mybir.dt.float32
BF16 = mybir.dt.bfloat16
I32 = mybir.dt.int32
AF = mybir.ActivationFunctionType
ALU = mybir.AluOpType

D = 1024
NT = 16
TOK = NT * 128
NE = 32
CAP = 384
NJ = CAP // 128
NB = (TOK * 4 + NE * (CAP - 1) + CAP - 1) // CAP
NROWS = NB * CAP
MAXBE = (TOK + CAP - 1) // CAP
EPS = 1e-6
BIGOFF = 1.0e6
DBG = bool(int(os.environ.get("MK_DBG", "0")))
NEXP_RUN = int(os.environ.get("MK_NEXP", str(NE)))
NPRE = int(os.environ.get("MK_NPRE", str(NT)))
NMAIN = int(os.environ.get("MK_NMAIN", str(NT)))
COMB = int(os.environ.get("MK_COMB", "1"))
CUT = int(os.environ.get("MK_CUT", "99"))
SKIP = int(os.environ.get("MK_SKIP", "1"))
VAR = os.environ.get("MK_VAR", "")
NEW = NE
NB_RUN = int(os.environ.get("MK_NB", str(NB)))


class Sched:
    ENGS = ("pe", "act", "dve", "pool", "sp")

    def __init__(self, nc, es):
        self.nc, self.es = nc, es
        self.rec = {e: [] for e in self.ENGS}
        self.chan = {}
        self.sems = {}
        self.lastw, self.readers = {}, {}
        self.known = {e: {} for e in self.ENGS}
        self.nsem = 0
        self.dma_final = {}
        self.dma_cur = {}
        self.dma_base = {}
        self.dma_open = {}
        self.grp = None
        self.cur_grp = {e: None for e in self.ENGS}
        self.grp_ap = None

    def _signal(self, ch, inc):
        c = self.chan.get(ch)
        if c is None or c[1] + inc > 30000:
            name = f"s{self.nsem}"
            self.nsem += 1
            self.sems[name] = self.es.enter_context(self.nc.semaphore(name))
            c = [name, 0]
            self.chan[ch] = c
        c[1] += inc
        return (c[0], c[1])

    def op(self, eng, fn, reads=(), writes=(), chan=None):
        if self.grp != self.cur_grp[eng]:
            self.cur_grp[eng] = self.grp
            self.known[eng] = {}
        deps = {}
        closing = set()

        def add(sv):
            if sv is None:
                return
            v = sv[1]
            if sv[0] in self.dma_cur:
                base = self.dma_base.get(sv[0], 0)
                if v <= base:
                    v = base
                else:
                    v = self.dma_cur[sv[0]]
                    closing.add(sv[0])
            if v > deps.get(sv[0], 0):
                deps[sv[0]] = v

        for k in reads:
            add(self.lastw.get(k))
        for k in writes:
            add(self.lastw.get(k))
            for r in self.readers.get(k, ()):
                add(r)
        own = self.chan.get(eng)
        waits = []
        for s, v in deps.items():
            if eng == "pe" and own is not None and s == own[0]:
                continue
            if self.known[eng].get(s, 0) >= v:
                continue
            self.known[eng][s] = v
            waits.append((s, v))
        sig = self._signal(chan if chan else eng, 16 if chan else 1)
        for s in closing:
            self.dma_open[s] = False
        if chan:
            if self.grp is None:
                self.dma_final[chan] = sig
            if not self.dma_open.get(sig[0], False):
                self.dma_open[sig[0]] = True
                self.dma_base[sig[0]] = sig[1] - 16
            self.dma_cur[sig[0]] = sig[1]
        self.rec[eng].append((waits, fn, sig, 16 if chan else 1, self.grp, bool(chan)))
        for k in reads:
            self.readers.setdefault(k, []).append(sig)
        for k in writes:
            self.lastw[k] = sig
            self.readers[k] = []

    def flush(self):
        for ch, sig in self.dma_final.items():
            if self.known["sp"].get(sig[0], 0) < sig[1]:
                self.known["sp"][sig[0]] = sig[1]
                self.rec["sp"].append(([sig], None, None, 0, None, False))
        self.dma_final = {}
        rec, sems = self.rec, self.sems

        def emit(engname):
            def emit_op(e, o):
                waits, fn, sig, inc = o[0], o[1], o[2], o[3]
                for sn, v in waits:
                    e.wait_ge(sems[sn], v)
                if fn is not None:
                    fn(e).then_inc(sems[sig[0]], inc)

            def body(e):
                if engname == "pool":
                    self.pool_bound = e.to_reg(NROWS - 1)
                    self.pool_wbound = e.to_reg(NE * D - 1)
                ops = rec[engname]
                reg = None
                if any(o[4] is not None for o in ops):
                    reg = e.alloc_register("nlive_" + engname)
                    e.reg_load(reg, self.grp_ap)
                i = 0
                while i < len(ops):
                    g = ops[i][4]
                    if g is None:
                        emit_op(e, ops[i])
                        i += 1
                        continue
                    j = i
                    while j < len(ops) and ops[j][4] == g:
                        j += 1
                    tot, base, dlast = {}, {}, {}
                    for o in ops[i:j]:
                        if o[1] is None:
                            continue
                        if o[5]:
                            dlast[o[2][0]] = o[2][1]
                        else:
                            base.setdefault(o[2][0], o[2][1] - o[3])
                            tot[o[2][0]] = tot.get(o[2][0], 0) + o[3]
                    with e.If_lt(reg, g + 1):
                        for sn, t in tot.items():
                            if base[sn] > 0:
                                e.wait_ge(sems[sn], base[sn])
                            e.sem_inc(sems[sn], t)
                    with e.Else():
                        for o in ops[i:j]:
                            emit_op(e, o)
                        for sn, v in dlast.items():
                            e.wait_ge(sems[sn], v)
                    i = j
            return body

        with self.nc.Block() as block:
            block.tensor(emit("pe"))
            block.scalar(emit("act"))
            block.vector(emit("dve"))
            block.gpsimd(emit("pool"))
            block.sync(emit("sp"))
        self.rec = {e: [] for e in self.ENGS}
        self.lastw, self.readers = {}, {}
        self.chan = {}
        self.known = {e: {} for e in self.ENGS}
        self.grp = None
        self.cur_grp = {e: None for e in self.ENGS}


def build():
    nc = bass.Bass("TRN2", target_bir_lowering=False)

    def din(name, shape, dt=F32):
        return nc.dram_tensor(name, list(shape), dt, kind="ExternalInput").ap()

    x_main = din("x_main", [TOK, D])
    x_pre = din("x_pre", [TOK, D])
    pmask_d = din("pmask", [128, 1])
    c_col_d = din("c_col", [128, 8])
    ada_w = din("ada_w", [D, 6 * D])
    ada_b_col_d = din("ada_b_col", [128, 48])
    ada_b_d = din("ada_b", [6 * D])
    g1_col_d = din("g1_col", [128, 8])
    g2_row_d = din("g2_row", [D])
    w_in = din("w_in", [D, 3072])
    lbp_d = din("lbp", [2, 512])
    hg_d = din("hg", [512])
    lng_d = din("lng", [512])
    lnb_d = din("lnb", [512])
    gng_d = din("gng", [512])
    wsT_d = din("wsT", [128, 4, 128])
    bs_col_d = din("bs_col", [128, 4])
    w_out = din("w_out", [D, D])
    rw_d = din("rw", [128, 8, 32])
    rb_d = din("rb", [32])
    wgu = din("wgu", [NE * D, 2 * D])
    bgu_tab = din("bgu_tab", [NE * 128, 16])
    wdn = din("wdn", [NE * D, D])
    bdn = din("bdn", [NE, D])
    fg_d = din("fg", [D])
    out_d = nc.dram_tensor("out", [TOK, D], F32, kind="ExternalOutput").ap()
    x1s = nc.dram_tensor("x1s", [TOK, D], F32, kind="ExternalOutput" if DBG else "Internal").ap()
    XS = nc.dram_tensor("XS", [NROWS, D], BF16, kind="Internal").ap()
    YS = nc.dram_tensor("YS", [NROWS, D], F32, kind="Internal").ap()
    XH = nc.dram_tensor("XH", [TOK, D], BF16, kind="Internal").ap()

    with ExitStack() as ES:
        S = Sched(nc, ES)

        def sb(es, name, shape, dt=F32):
            return es.enter_context(nc.sbuf_tensor("sb_" + name, list(shape), dt))

        def ps(es, name, shape, dt=F32):
            return es.enter_context(nc.psum_tensor("ps_" + name, list(shape), dt))

        def bc(ap, shape):
            return ap.to_broadcast(list(shape))

        ident_bf = sb(ES, "ident_bf", [128, 128], BF16)
        ident_f = sb(ES, "ident_f", [128, 128])
        modT = sb(ES, "modT", [128, 48])
        G1T = sb(ES, "G1T", [128, 8])
        gate2_rep = sb(ES, "gate2_rep", [128, D])
        fg_rep = sb(ES, "fg_rep", [128, D])
        lgt_all = sb(ES, "lgt_all", [128, NT, NE])
        mx8_all = sb(ES, "mx8_all", [128, NT, 8])
        gates_all = sb(ES, "gates_all", [128, NT, NE])
        pos_all = sb(ES, "pos_all", [128, NT, NE])
        base_rep = sb(ES, "base_rep", [128, NE])
        widx = sb(ES, "widx", [128, NB, 8], I32)
        bidx = sb(ES, "bidx", [128, NB], I32)
        sel = sb(ES, "sel", [128, NB])
        nlive_i = sb(ES, "nlive_i", [128, 1], I32)
        dest_all = sb(ES, "dest_all", [128, NT, 4], I32)
        gate_all = sb(ES, "gate_all", [128, NT, 4])
        eps_t = sb(ES, "eps_t", [128, 1])
        pmask = sb(ES, "pmask", [128, 1])

        with ExitStack() as E1:
            gate1_rep = sb(E1, "gate1_rep", [128, D])
            SH2_rep = sb(E1, "SH2_rep", [128, D])
            G2_rep = sb(E1, "G2_rep", [128, D])
            lb_rep = sb(E1, "lb_rep", [128, 512])
            oml_rep = sb(E1, "oml_rep", [128, 512])
            hg_rep = sb(E1, "hg_rep", [128, 512])
            lng_rep = sb(E1, "lng_rep", [128, 512])
            lnb_rep = sb(E1, "lnb_rep", [128, 512])
            gng_rep = sb(E1, "gng_rep", [128, 512])
            wsT_f = sb(E1, "wsT_f", [128, 4, 128])
            wsT_bf = sb(E1, "wsT_bf", [128, 4, 128], BF16)
            bs_col = sb(E1, "bs_col", [128, 4])
            Lst = sb(E1, "Lst", [128, 128])
            cones = sb(E1, "cones", [128, 2])
            cmask = sb(E1, "cmask", [128, 4, 64])
            Ust_bf = sb(E1, "Ust_bf", [128, 128], BF16)
            ones_bf = sb(E1, "ones_bf", [128, 128], BF16)
            ecol = sb(E1, "ecol", [128, NE])
            rw = sb(E1, "rw", [128, 8, 32])
            rb_rep = sb(E1, "rb_rep", [128, 32])
            c_col = sb(E1, "c_col", [128, 8])
            c_sig = sb(E1, "c_sig", [128, 8])
            c_act = sb(E1, "c_act", [128, 8])
            c_rep = sb(E1, "c_rep", [128, 8, 128], BF16)
            ada_b_col = sb(E1, "ada_b_col", [128, 48])
            g1_col = sb(E1, "g1_col", [128, 8])
            adaw_sb = [sb(E1, f"adaw{i}", [128, 8, 256], BF16) for i in range(2)]
            mod_rep = sb(E1, "mod_rep", [128, 256])
            win_sb = sb(E1, "win_sb", [128, 8, 3072], BF16)
            wout_sb = sb(E1, "wout_sb", [128, 8, D], BF16)
            Sst = sb(E1, "Sst", [128, 4, 128])
            Sp_bf = sb(E1, "Sp_bf", [128, 4, 128], BF16)
            xin = [sb(E1, f"xin{i}", [128, D]) for i in range(2)]
            junk = sb(E1, "junk", [128, D], BF16)
            ss = sb(E1, "ss", [128, 16])
            rs = sb(E1, "rs", [128, 16])
            xhat = sb(E1, "xhat", [128, D], BF16)
            hT = sb(E1, "hT", [128, 8, 128], BF16)
            q_sb = sb(E1, "q_sb", [128, 512])
            sg = sb(E1, "sg", [128, 512])
            sg2 = sb(E1, "sg2", [128, 512], BF16)
            f_sb = sb(E1, "f_sb", [128, 512])
            g_sb = sb(E1, "g_sb", [128, 512])
            kk = sb(E1, "kk", [128, 512])
            Eexp = sb(E1, "Eexp", [128, 512])
            decs = [sb(E1, f"dec{i}", [128, 8]) for i in range(2)]
            Kd_bfs = [sb(E1, f"Kd_bf{i}", [128, 512], BF16) for i in range(2)]
            Qm_bfs = [sb(E1, f"Qm_bf{i}", [128, 512], BF16) for i in range(2)]
            v_bfs = [sb(E1, f"v_bf{i}", [128, 512], BF16) for i in range(2)]
            ggs = [sb(E1, f"gg{i}", [128, 512]) for i in range(2)]
            u_sbs = [sb(E1, f"u_sb{i}", [128, 512]) for i in range(2)]
            vv = sb(E1, "vv", [128, 512])
            bst = sb(E1, "bst", [128, 6])
            bag = sb(E1, "bag", [128, 2])
            nmr = sb(E1, "nmr", [128, 1])
            vn = sb(E1, "vn", [128, 512])
            vn_bfs = [sb(E1, f"vn_bf{i}", [128, 512], BF16) for i in range(2)]
            QKT = sb(E1, "QKT", [128, 8, 128], BF16)
            scm = sb(E1, "scm", [128, 4, 64], BF16)
            o_t = sb(E1, "o_t", [128, 512])
            yg = sb(E1, "yg", [128, 512])
            cat = sb(E1, "cat", [128, D], BF16)
            catT = sb(E1, "catT", [128, 8, 128], BF16)
            x1t = [sb(E1, f"x1t{i}", [128, D]) for i in range(2)]
            xh2f = sb(E1, "xh2f", [128, D])
            xh2b = [sb(E1, f"xh2b{i}", [128, D], BF16) for i in range(2)]
            h2T = sb(E1, "h2T", [128, 8, 128])
            nmx = sb(E1, "nmx", [128, 1])
            msk = sb(E1, "msk", [128, NE])
            msk_bf = sb(E1, "msk_bf", [128, NE], BF16)
            eexp = sb(E1, "eexp", [128, NE])
            gsum = sb(E1, "gsum", [128, 1])
            pA = ps(E1, "pA", [128, 8, 128], BF16)
            pB = ps(E1, "pB", [128, 512])
            pC = ps(E1, "pC", [128, 512])
            pD = ps(E1, "pD", [128, 512])
            pE = ps(E1, "pE", [128, 512])
            pF = ps(E1, "pF", [128, 512])
            pG = ps(E1, "pG", [128, 512])
            pH = ps(E1, "pH", [128, 4, 128])
            zb = [pB, pC]
            zbk = ["pB", "pC"]
            zctr = [0]

            def zbank():
                i = 0 if "onebank" in VAR else zctr[0] % 2
                zctr[0] += 1
                return zb[i], zbk[i]

            S.op("pool", lambda e: e.memset(ident_f[:], 0.0), writes=["ident_f"])
            S.op("pool", lambda e: e.affine_select(out=ident_f[:], in_=ident_f[:], pattern=[[-1, 128]],
                                                    compare_op=ALU.not_equal, fill=1.0, base=0, channel_multiplier=1),
                 reads=["ident_f"], writes=["ident_f"])
            S.op("pool", lambda e: e.tensor_copy(ident_bf[:], ident_f[:]), reads=["ident_f"], writes=["ident_bf"])
            S.op("pool", lambda e: e.memset(eps_t[:], EPS), writes=["eps_t"])
            S.op("pool", lambda e: e.memset(Lst[:], 1.0), writes=["Lst"])
            S.op("pool", lambda e: e.affine_select(out=Lst[:], in_=Lst[:], pattern=[[-1, 128]], compare_op=ALU.is_gt,
                                                    fill=0.0, base=0, channel_multiplier=1), reads=["Lst"], writes=["Lst"])
            S.op("pool", lambda e: e.memset(Lst[64:128, 0:64], 0.0), reads=["Lst"], writes=["Lst"])
            S.op("pool", lambda e: e.memset(cones[:], 0.0), writes=["cones"])
            S.op("pool", lambda e: e.memset(cones[0:64, 0:1], 1.0), reads=["cones"], writes=["cones"])
            S.op("pool", lambda e: e.memset(cones[64:128, 1:2], 1.0), reads=["cones"], writes=["cones"])
            S.op("pool", lambda e: e.memset(cmask[:], 1.0), writes=["cmask"])
            for hf in range(2):
                S.op("pool", lambda e, hf=hf: e.affine_select(
                    out=cmask[hf * 64:(hf + 1) * 64], in_=cmask[hf * 64:(hf + 1) * 64], pattern=[[0, 4], [1, 64]],
                    compare_op=ALU.is_ge, fill=0.0, base=0, channel_multiplier=-1), reads=["cmask"], writes=["cmask"])
            S.op("pool", lambda e: e.memset(Ust_bf[:], 1.0), writes=["Ust"])
            S.op("pool", lambda e: e.affine_select(out=Ust_bf[:], in_=Ust_bf[:], pattern=[[1, 128]], compare_op=ALU.is_gt,
                                                    fill=0.0, base=0, channel_multiplier=-1), reads=["Ust"], writes=["Ust"])
            S.op("pool", lambda e: e.memset(ones_bf[:], 1.0), writes=["ones_bf"])
            S.op("pool", lambda e: e.iota(ecol[:], pattern=[[CAP, NE]], base=0, channel_multiplier=0,
                                          allow_small_or_imprecise_dtypes=True), writes=["ecol"])
            S.op("pool", lambda e: e.memset(base_rep[:], 0.0), writes=["base_rep"])
            S.op("pool", lambda e: e.memset(Sst[:], 0.0), writes=["Sst"])

            zt = sb(E1, "zt", [128, 1024], BF16)
            S.op("pool", lambda e: e.memset(zt[:], 0.0), writes=["zt"])
            XSv = XS.rearrange("(p r) d -> p (r d)", p=128)
            zfill = list(range(0, (NROWS // 128) * D, 1024))
            def ld(dst, src, key):
                S.op("sp", lambda e: e.dma_start(out=dst, in_=src), writes=[key], chan="ld_" + key)

            ld(pmask[:], pmask_d, "pmask")
            ld(c_col[:], c_col_d, "c_col")
            ld(ada_b_col[:], ada_b_col_d, "ada_b_col")
            ld(gate1_rep[:], ada_b_d[2 * D:3 * D].partition_broadcast(128), "gate_rep0")
            ld(gate2_rep[:], ada_b_d[5 * D:6 * D].partition_broadcast(128), "gate_rep1")
            ld(SH2_rep[:], ada_b_d[3 * D:4 * D].partition_broadcast(128), "gate_rep2")
            ld(xh2f[:], ada_b_d[4 * D:5 * D].partition_broadcast(128), "gate_rep3")
            ld(G2_rep[:], g2_row_d.partition_broadcast(128), "g2row")
            ld(g1_col[:], g1_col_d, "g1_col")
            ld(lb_rep[:], lbp_d[0].partition_broadcast(128), "lbp0")
            ld(oml_rep[:], lbp_d[1].partition_broadcast(128), "lbp1")
            ld(hg_rep[:], hg_d.partition_broadcast(128), "hg_rep")
            ld(lng_rep[:], lng_d.partition_broadcast(128), "lng_rep")
            ld(lnb_rep[:], lnb_d.partition_broadcast(128), "lnb_rep")
            ld(gng_rep[:], gng_d.partition_broadcast(128), "gng_rep")
            ld(wsT_f[:], wsT_d, "wsT_f")
            ld(bs_col[:], bs_col_d, "bs_col")
            ld(rw[:], rw_d, "rw")
            ld(rb_rep[:], rb_d.partition_broadcast(128), "rb_rep")
            ld(fg_rep[:], fg_d.partition_broadcast(128), "fg_rep")

            S.op("act", lambda e: e.activation(out=c_sig[:], in_=c_col[:], func=AF.Sigmoid), reads=["c_col"], writes=["c_sig"])
            S.op("dve", lambda e: e.tensor_tensor(out=c_act[:], in0=c_col[:], in1=c_sig[:], op=ALU.mult),
                 reads=["c_col", "c_sig"], writes=["c_act"])
            S.op("dve", lambda e: e.tensor_copy(c_rep[:], bc(c_act[:, :].unsqueeze(2), [128, 8, 128])),
                 reads=["c_act"], writes=["c_rep"])
            S.op("dve", lambda e: e.tensor_tensor(out=lb_rep[:], in0=lb_rep[:], in1=oml_rep[:], op=ALU.subtract),
                 reads=["lbp0", "lbp1"], writes=["lbd", "lbp0"])
            S.op("act", lambda e: e.activation(out=oml_rep[:], in_=lb_rep[:], func=AF.Sigmoid, scale=-1.0),
                 reads=["lbd", "lbp1"], writes=["oml_rep", "lbp1"])
            S.op("act", lambda e: e.activation(out=lb_rep[:], in_=lb_rep[:], func=AF.Sigmoid),
                 reads=["lbd", "oml_rep"], writes=["lbd", "lb_rep"])
            S.op("pool", lambda e: e.affine_select(out=wsT_bf[:], in_=wsT_f[:], pattern=[[0, 4], [1, 128]],
                                                    compare_op=ALU.is_ge, fill=0.0, base=0, channel_multiplier=-1),
                 reads=["wsT_f"], writes=["wsT_bf"])

            CW = 256

            def mod_chunk(jj):
                slot = jj % 2
                grp, off = (jj * CW) // D, (jj * CW) % D
                S.op("pool", lambda e, jj=jj, slot=slot: e.dma_start(
                    out=adaw_sb[slot][:], in_=ada_w[:, jj * CW:(jj + 1) * CW].rearrange("(kc p) n -> p kc n", p=128)),
                    writes=[f"adaw{slot}"], chan=f"ld_adaw{slot}")
                for kc in range(8):
                    S.op("pe", lambda e, kc=kc, slot=slot: e.matmul(pD[:, 0:CW], c_rep[:, kc, :], adaw_sb[slot][:, kc, :],
                                                                   start=(kc == 0), stop=(kc == 7)),
                         reads=["c_rep", f"adaw{slot}"], writes=["pD"])
                if grp >= 2:
                    which = {2: 0, 5: 1, 3: 2, 4: 3}[grp]
                    dst = [gate1_rep, gate2_rep, SH2_rep, xh2f][which]
                    S.op("dve", lambda e, dst=dst, off=off: e.tensor_tensor(out=dst[:, off:off + CW], in0=pD[:, 0:CW], in1=dst[:, off:off + CW], op=ALU.add),
                         reads=["pD", f"gate_rep{which}"], writes=[f"gate_rep{which}_{off // 512}"])
                else:
                    S.op("dve", lambda e: e.tensor_copy(mod_rep[:], pD[:, 0:CW]), reads=["pD"], writes=["mod_rep"])
                    for t4 in range(CW // 128):
                        S.op("pe", lambda e, t4=t4: e.transpose(pH[:, t4, :], mod_rep[:, t4 * 128:(t4 + 1) * 128], ident_f[:]),
                             reads=["mod_rep", "ident_f"], writes=["pH"])
                    c0 = jj * (CW // 128)
                    S.op("dve", lambda e, c0=c0: e.tensor_tensor(out=modT[:, c0:c0 + CW // 128], in0=pH[:, 0:CW // 128, 0],
                                                                 in1=ada_b_col[:, c0:c0 + CW // 128], op=ALU.add),
                         reads=["pH", "ada_b_col"], writes=[f"modT{jj}"])
            NEARLY = 2 * D // CW
            for jj in range(NEARLY):
                mod_chunk(jj)
            for kc in range(8):
                for hh in range(3):
                    S.op("pool", lambda e, kc=kc, hh=hh: e.dma_start(out=win_sb[:, kc, hh * 1024:(hh + 1) * 1024],
                                                                     in_=w_in[kc * 128:(kc + 1) * 128, hh * 1024:(hh + 1) * 1024]),
                         writes=[f"win_{kc}_{hh}"], chan="ld_win")
            S.op("pool", lambda e: e.dma_start(out=wout_sb[:], in_=w_out.rearrange("(kc p) n -> p kc n", p=128)),
                 writes=["wout"], chan="ld_wout")

            mk = [f"modT{jj}" for jj in range(8)]
            S.op("dve", lambda e: e.scalar_tensor_tensor(out=G1T[:], in0=modT[:, 8:16], scalar=1.0, in1=g1_col[:],
                                                         op0=ALU.add, op1=ALU.mult), reads=mk + ["g1_col"], writes=["G1T"])
            late = list(range(NEARLY, 6 * D // CW))

            def late_some(nmax):
                for _ in range(nmax):
                    if late:
                        mod_chunk(late.pop(0))
                        if not late:
                                    S.op("dve", lambda e: e.scalar_tensor_tensor(out=G2_rep[:], in0=xh2f[:], scalar=1.0, in1=G2_rep[:],
                                                                                 op0=ALU.add, op1=ALU.mult), reads=["gate_rep3_0", "gate_rep3_1", "g2row"], writes=["G2_rep", "xh2f"])

            SH1 = modT[:, 0:8]
            winall = [f"win_{kc}_{hh}" for kc in range(8) for hh in range(3)]

            def rstd_from(ssap, rsap, n, keys_r, key_w):
                S.op("act", lambda e: e.activation(out=rsap, in_=ssap, func=AF.Ln, scale=1.0 / n, bias=eps_t[:]),
                     reads=keys_r + ["eps_t"], writes=[key_w])
                S.op("act", lambda e: e.activation(out=rsap, in_=rsap, func=AF.Exp, scale=-0.5),
                     reads=[key_w], writes=[key_w])

            def stage1(n, i, prefix):
                p = n % 2
                xsrc = x_pre if prefix else x_main
                xi = xin[p]
                xk = f"xin{p}"
                S.op("sp", lambda e: e.dma_start(out=xi[:], in_=xsrc[i * 128:(i + 1) * 128, :]), writes=[xk], chan="ld_" + xk)
                S.op("act", lambda e: e.activation(out=junk[:], in_=xi[:], func=AF.Square, accum_out=ss[:, 0:1]),
                     reads=[xk], writes=["junk", "ss0"])
                if CUT <= 1:
                    return
                rstd_from(ss[:, 0:1], rs[:, 0:1], D, ["ss0"], "rs0")
                if CUT <= 2:
                    return
                S.op("dve", lambda e: e.tensor_scalar(out=xhat[:], in0=xi[:], scalar1=rs[:, 0:1], scalar2=None, op0=ALU.mult),
                     reads=[xk, "rs0"], writes=["xhat"])
                for kc in range(8):
                    S.op("pe", lambda e, kc=kc: e.transpose(pA[:, kc, :], xhat[:, kc * 128:(kc + 1) * 128], ident_bf[:]),
                         reads=["xhat", "ident_bf"], writes=["pA"])
                for kc in range(8):
                    S.op("act", lambda e, kc=kc: e.activation(out=hT[:, kc, :], in_=pA[:, kc, :], func=AF.Identity,
                                                              scale=G1T[:, kc:kc + 1], bias=modT[:, kc:kc + 1]),
                         reads=["pA", "G1T"] + mk, writes=["hT"])
                yield

                if CUT <= 3:
                    return

                def inproj(nb):
                    zp, zk = zbank()
                    for kc in range(8):
                        S.op("pe", lambda e, kc=kc, zp=zp: e.matmul(zp[:], hT[:, kc, :], win_sb[:, kc, nb * 512:(nb + 1) * 512],
                                                             start=(kc == 0), stop=(kc == 7)),
                             reads=["hT"] + winall, writes=[zk])
                    return zp, zk

                if prefix:
                    if not prefix:
                        zp, zk = inproj(0)
                        S.op("act", lambda e, zp=zp: e.activation(out=q_sb[:], in_=zp[:], func=AF.Identity), reads=[zk], writes=["q_sb"])
                    zp, zk = inproj(1)
                    S.op("act", lambda e, zp=zp: e.activation(out=sg[:], in_=zp[:], func=AF.Sigmoid), reads=[zk], writes=["sg"])
                    S.op("dve", lambda e: e.tensor_tensor(out=f_sb[:], in0=sg[:], in1=oml_rep[:], op=ALU.mult),
                         reads=["sg", "oml_rep"], writes=["f_sb"])
                    S.op("dve", lambda e: e.tensor_tensor(out=f_sb[:], in0=f_sb[:], in1=lb_rep[:], op=ALU.add),
                         reads=["f_sb", "lb_rep"], writes=["f_sb"])
                    S.op("act", lambda e: e.activation(out=g_sb[:], in_=f_sb[:], func=AF.Ln), reads=["f_sb"], writes=["g_sb"])
                    S.op("dve", lambda e: e.tensor_scalar(out=kk[:], in0=f_sb[:], scalar1=-1.0, scalar2=1.0, op0=ALU.mult, op1=ALU.add),
                         reads=["f_sb"], writes=["kk"])
                    if CUT <= 4:
                        return
                    zp, zk = inproj(2)
                    if "vdve" in VAR:
                        S.op("dve", lambda e, zp=zp: e.tensor_copy(v_bfs[p][:], zp[:]), reads=[zk], writes=[f"v_bf{p}"])
                    elif "vnone" in VAR:
                        pass
                    else:
                        S.op("act", lambda e, zp=zp: e.activation(out=v_bfs[p][:], in_=zp[:], func=AF.Identity), reads=[zk], writes=[f"v_bf{p}"])
                    if CUT <= 5:
                        return
                    S.op("pe", lambda e: e.matmul(pD[:], Lst[:], g_sb[:], start=True, stop=True), reads=["Lst", "g_sb"], writes=["pD"])
                    for h in range(4):
                        S.op("pe", lambda e, h=h: e.matmul(pE[:, 2 * h:2 * h + 2], g_sb[:, h * 128:(h + 1) * 128], cones[:],
                                                           start=True, stop=True), reads=["g_sb", "cones"], writes=["pE"])
                    if CUT <= 6:
                        return
                    S.op("act", lambda e: e.activation(out=Eexp[:], in_=pD[:], func=AF.Exp), reads=["pD"], writes=["Eexp"])
                    S.op("act", lambda e: e.activation(out=decs[p][:], in_=pE[:, 0:8], func=AF.Exp), reads=["pE"], writes=[f"dec{p}"])
                    S.op("dve", lambda e: e.tensor_tensor(out=Kd_bfs[p][:], in0=kk[:], in1=Eexp[:], op=ALU.mult),
                         reads=["kk", "Eexp"], writes=[f"Kd_bf{p}"])
                    if not prefix:
                        S.op("act", lambda e: e.activation(out=Eexp[:], in_=pD[:], func=AF.Exp, scale=-1.0), reads=["pD"], writes=["Eexp"])
                        S.op("dve", lambda e: e.tensor_tensor(out=Qm_bfs[p][:], in0=q_sb[:], in1=Eexp[:], op=ALU.mult),
                             reads=["q_sb", "Eexp"], writes=[f"Qm_bf{p}"])
                        yield
                        zp, zk = inproj(3)
                        S.op("act", lambda e, zp=zp: e.activation(out=sg[:], in_=zp[:], func=AF.Sigmoid), reads=[zk], writes=["sg"])
                        S.op("dve", lambda e, zp=zp: e.tensor_tensor(out=ggs[p][:], in0=zp[:], in1=sg[:], op=ALU.mult), reads=[zk, "sg"], writes=[f"gg{p}"])
                        S.op("dve", lambda e: e.tensor_tensor(out=ggs[p][:], in0=ggs[p][:], in1=hg_rep[:], op=ALU.mult),
                             reads=[f"gg{p}", "hg_rep"], writes=[f"gg{p}"])
                        zp, zk = inproj(4)
                        S.op("act", lambda e, zp=zp: e.activation(out=u_sbs[p][:], in_=zp[:], func=AF.Gelu), reads=[zk], writes=[f"u_sb{p}"])
                        yield
                        zp, zk = inproj(5)
                        S.op("act", lambda e, zp=zp: e.activation(out=vv[:], in_=zp[:], func=AF.Gelu), reads=[zk], writes=["vv"])
                        S.op("dve", lambda e: e.bn_stats(bst[:], vv[:]), reads=["vv"], writes=["bst"])
                        S.op("dve", lambda e: e.bn_aggr(bag[:], bst[:]), reads=["bst"], writes=["bag"])
                        rstd_from(bag[:, 1:2], rs[:, 1:2], 1.0, ["bag"], "rs1")
                        S.op("dve", lambda e: e.scalar_tensor_tensor(out=nmr[:], in0=bag[:, 0:1], scalar=-1.0, in1=rs[:, 1:2],
                                                                     op0=ALU.mult, op1=ALU.mult), reads=["bag", "rs1"], writes=["nmr"])
                        S.op("act", lambda e: e.activation(out=vn[:], in_=vv[:], func=AF.Identity, scale=rs[:, 1:2], bias=nmr[:]),
                             reads=["vv", "rs1", "nmr"], writes=["vn"])
                        S.op("dve", lambda e: e.tensor_tensor(out=vn[:], in0=vn[:], in1=lng_rep[:], op=ALU.mult),
                             reads=["vn", "lng_rep"], writes=["vn"])
                        S.op("dve", lambda e: e.tensor_tensor(out=vn_bfs[p][:], in0=vn[:], in1=lnb_rep[:], op=ALU.add),
                             reads=["vn", "lnb_rep"], writes=[f"vn_bf{p}"])
                else:
                    z0, k0 = inproj(0)
                    z1, k1 = inproj(1)
                    S.op("act", lambda e: e.activation(out=q_sb[:], in_=z0[:], func=AF.Identity), reads=[k0], writes=["q_sb"])
                    S.op("act", lambda e: e.activation(out=sg[:], in_=z1[:], func=AF.Sigmoid), reads=[k1], writes=["sg"])
                    z2, k2 = inproj(2)
                    z3, k3 = inproj(3)
                    S.op("dve", lambda e: e.tensor_tensor(out=f_sb[:], in0=sg[:], in1=oml_rep[:], op=ALU.mult),
                         reads=["sg", "oml_rep"], writes=["f_sb"])
                    S.op("dve", lambda e: e.tensor_tensor(out=f_sb[:], in0=f_sb[:], in1=lb_rep[:], op=ALU.add),
                         reads=["f_sb", "lb_rep"], writes=["f_sb"])
                    S.op("act", lambda e: e.activation(out=v_bfs[p][:], in_=z2[:], func=AF.Identity), reads=[k2], writes=[f"v_bf{p}"])
                    S.op("act", lambda e: e.activation(out=sg2[:], in_=z3[:], func=AF.Sigmoid), reads=[k3], writes=["sg2"])
                    S.op("dve", lambda e: e.tensor_tensor(out=ggs[p][:], in0=z3[:], in1=sg2[:], op=ALU.mult), reads=[k3, "sg2"], writes=[f"gg{p}"])
                    S.op("dve", lambda e: e.tensor_scalar(out=kk[:], in0=f_sb[:], scalar1=-1.0, scalar2=1.0, op0=ALU.mult, op1=ALU.add),
                         reads=["f_sb"], writes=["kk"])
                    S.op("dve", lambda e: e.tensor_tensor(out=ggs[p][:], in0=ggs[p][:], in1=hg_rep[:], op=ALU.mult),
                         reads=[f"gg{p}", "hg_rep"], writes=[f"gg{p}"])
                    yield
                    z4, k4 = inproj(4)
                    z5, k5 = inproj(5)
                    S.op("act", lambda e: e.activation(out=u_sbs[p][:], in_=z4[:], func=AF.Gelu), reads=[k4], writes=[f"u_sb{p}"])
                    S.op("act", lambda e: e.activation(out=vv[:], in_=z5[:], func=AF.Gelu), reads=[k5], writes=["vv"])
                    S.op("act", lambda e: e.activation(out=g_sb[:], in_=f_sb[:], func=AF.Ln), reads=["f_sb"], writes=["g_sb"])
                    S.op("dve", lambda e: e.bn_stats(bst[:], vv[:]), reads=["vv"], writes=["bst"])
                    S.op("dve", lambda e: e.bn_aggr(bag[:], bst[:]), reads=["bst"], writes=["bag"])
                    S.op("pe", lambda e: e.matmul(pD[:], Lst[:], g_sb[:], start=True, stop=True), reads=["Lst", "g_sb"], writes=["pD"])
                    for h in range(4):
                        S.op("pe", lambda e, h=h: e.matmul(pE[:, 2 * h:2 * h + 2], g_sb[:, h * 128:(h + 1) * 128], cones[:],
                                                           start=True, stop=True), reads=["g_sb", "cones"], writes=["pE"])
                    rstd_from(bag[:, 1:2], rs[:, 1:2], 1.0, ["bag"], "rs1")
                    S.op("act", lambda e: e.activation(out=Eexp[:], in_=pD[:], func=AF.Exp), reads=["pD"], writes=["Eexp"])
                    S.op("act", lambda e: e.activation(out=decs[p][:], in_=pE[:, 0:8], func=AF.Exp), reads=["pE"], writes=[f"dec{p}"])
                    S.op("dve", lambda e: e.scalar_tensor_tensor(out=nmr[:], in0=bag[:, 0:1], scalar=-1.0, in1=rs[:, 1:2],
                                                                 op0=ALU.mult, op1=ALU.mult), reads=["bag", "rs1"], writes=["nmr"])
                    S.op("dve", lambda e: e.tensor_tensor(out=Kd_bfs[p][:], in0=kk[:], in1=Eexp[:], op=ALU.mult),
                         reads=["kk", "Eexp"], writes=[f"Kd_bf{p}"])
                    S.op("act", lambda e: e.activation(out=Eexp[:], in_=pD[:], func=AF.Exp, scale=-1.0), reads=["pD"], writes=["Eexp"])
                    S.op("act", lambda e: e.activation(out=vn[:], in_=vv[:], func=AF.Identity, scale=rs[:, 1:2], bias=nmr[:]),
                         reads=["vv", "rs1", "nmr"], writes=["vn"])
                    S.op("dve", lambda e: e.tensor_tensor(out=Qm_bfs[p][:], in0=q_sb[:], in1=Eexp[:], op=ALU.mult),
                         reads=["q_sb", "Eexp"], writes=[f"Qm_bf{p}"])
                    S.op("dve", lambda e: e.tensor_tensor(out=vn[:], in0=vn[:], in1=lng_rep[:], op=ALU.mult),
                         reads=["vn", "lng_rep"], writes=["vn"])
                    S.op("dve", lambda e: e.tensor_tensor(out=vn_bfs[p][:], in0=vn[:], in1=lnb_rep[:], op=ALU.add),
                         reads=["vn", "lnb_rep"], writes=[f"vn_bf{p}"])

            def stage2(n, i, prefix):
                p = n % 2
                xi = xin[p]
                xk = f"xin{p}"
                if not prefix:
                    for h in range(4):
                        S.op("pe", lambda e, h=h: e.transpose(pA[:, h, :], Qm_bfs[p][:, h * 128:(h + 1) * 128], ident_bf[:]),
                             reads=[f"Qm_bf{p}", "ident_bf"], writes=["pA"])
                    for h in range(4):
                        S.op("pe", lambda e, h=h: e.transpose(pA[:, 4 + h, :], Kd_bfs[p][:, h * 128:(h + 1) * 128], ident_bf[:]),
                             reads=[f"Kd_bf{p}", "ident_bf"], writes=["pA"])
                    S.op("act", lambda e: e.activation(out=QKT[:], in_=pA[:], func=AF.Identity), reads=["pA"], writes=["QKT"])
                    for c in range(2):
                        cs = slice(c * 64, (c + 1) * 64)
                        for h in range(4):
                            S.op("pe", lambda e, h=h, cs=cs: e.matmul(pE[cs, 64 + h * 64:128 + h * 64], QKT[:, 4 + h, cs], QKT[:, h, cs],
                                                                      start=True, stop=True), reads=["QKT"], writes=["pE"])
                    S.op("dve", lambda e: e.tensor_tensor(out=scm[:], in0=pE[:, 64:320].rearrange("p (h t) -> p h t", h=4),
                                                          in1=cmask[:], op=ALU.mult), reads=["pE", "cmask"], writes=["scm"])
                yield
                for c in range(2):
                    cs = slice(c * 64, (c + 1) * 64)
                    S.op("dve", lambda e, c=c: e.tensor_tensor(out=Sst[:], in0=Sst[:],
                                                               in1=bc(decs[p][:, c::2].unsqueeze(2), [128, 4, 128]), op=ALU.mult),
                         reads=["Sst", f"dec{p}"], writes=["Sst"])
                    if not prefix:
                        S.op("act", lambda e: e.activation(out=Sp_bf[:], in_=Sst[:], func=AF.Identity), reads=["Sst"], writes=["Sp_bf"])
                        for h in range(4):
                            hs = slice(h * 128, (h + 1) * 128)
                            S.op("pe", lambda e, h=h, cs=cs, hs=hs: e.matmul(pF[cs, hs], QKT[:, h, cs], Sp_bf[:, h, :], start=True, stop=False),
                                 reads=["QKT", "Sp_bf"], writes=["pF"])
                            S.op("pe", lambda e, h=h, cs=cs, hs=hs: e.matmul(pF[cs, hs], scm[cs, h, :], v_bfs[p][cs, hs], start=False, stop=True),
                                 reads=["scm", f"v_bf{p}"], writes=["pF"])
                    for h in range(4):
                        hs = slice(h * 128, (h + 1) * 128)
                        S.op("pe", lambda e, cs=cs, hs=hs: e.matmul(pG[:, hs], Kd_bfs[p][cs, hs], v_bfs[p][cs, hs], start=True, stop=True),
                             reads=[f"Kd_bf{p}", f"v_bf{p}"], writes=["pG"])
                    S.op("dve", lambda e: e.tensor_tensor(out=Sst[:].rearrange("p h v -> p (h v)"), in0=Sst[:].rearrange("p h v -> p (h v)"),
                                                          in1=pG[:], op=ALU.add), reads=["Sst", "pG"], writes=["Sst"])
                if prefix:
                    return
                yield
                S.op("act", lambda e: e.activation(out=o_t[:], in_=pF[:], func=AF.Identity), reads=["pF"], writes=["o_t"])
                for h in range(4):
                    S.op("act", lambda e, h=h: e.activation(out=junk[:, h * 128:(h + 1) * 128], in_=o_t[:, h * 128:(h + 1) * 128],
                                                            func=AF.Square, accum_out=ss[:, 2 + h:3 + h]),
                         reads=["o_t"], writes=["junk", f"ssh{h}"])
                rstd_from(ss[:, 2:6], rs[:, 2:6], 128.0, [f"ssh{h}" for h in range(4)], "rsh")
                S.op("dve", lambda e: e.tensor_tensor(out=o_t[:].rearrange("p (h v) -> p h v", h=4), in0=o_t[:].rearrange("p (h v) -> p h v", h=4),
                                                      in1=bc(rs[:, 2:6].unsqueeze(2), [128, 4, 128]), op=ALU.mult),
                     reads=["o_t", "rsh"], writes=["o_t"])
                S.op("dve", lambda e: e.tensor_tensor(out=cat[:, 0:512], in0=o_t[:], in1=ggs[p][:], op=ALU.mult),
                     reads=["o_t", f"gg{p}"], writes=["cat_h"])
                yield
                zp, zk = zbank()
                for g in range(4):
                    gs = slice(g * 128, (g + 1) * 128)
                    S.op("pe", lambda e, g=g, gs=gs, zp=zp: e.matmul(zp[:, gs], wsT_bf[:, g, :], vn_bfs[p][:, gs], start=True, stop=True),
                         reads=["wsT_bf", f"vn_bf{p}"], writes=[zk])
                for g in range(4):
                    gs = slice(g * 128, (g + 1) * 128)
                    S.op("dve", lambda e, g=g, gs=gs, zp=zp: e.scalar_tensor_tensor(out=yg[:, gs], in0=zp[:, gs], scalar=bs_col[:, g:g + 1],
                                                                                   in1=u_sbs[p][:, gs], op0=ALU.add, op1=ALU.mult),
                         reads=[zk, "bs_col", f"u_sb{p}"], writes=["yg"])
                for g in range(4):
                    S.op("act", lambda e, g=g: e.activation(out=junk[:, g * 128:(g + 1) * 128], in_=yg[:, g * 128:(g + 1) * 128],
                                                            func=AF.Square, accum_out=ss[:, 6 + g:7 + g]),
                         reads=["yg"], writes=["junk", f"ssg{g}"])
                rstd_from(ss[:, 6:10], rs[:, 6:10], 128.0, [f"ssg{g}" for g in range(4)], "rsg")
                S.op("dve", lambda e: e.tensor_tensor(out=yg[:].rearrange("p (h v) -> p h v", h=4), in0=yg[:].rearrange("p (h v) -> p h v", h=4),
                                                      in1=bc(rs[:, 6:10].unsqueeze(2), [128, 4, 128]), op=ALU.mult),
                     reads=["yg", "rsg"], writes=["yg"])
                S.op("dve", lambda e: e.tensor_tensor(out=cat[:, 512:1024], in0=yg[:], in1=gng_rep[:], op=ALU.mult),
                     reads=["yg", "gng_rep"], writes=["cat_g"])
                yield
                for kc in range(8):
                    S.op("pe", lambda e, kc=kc: e.transpose(pA[:, kc, :], cat[:, kc * 128:(kc + 1) * 128], ident_bf[:]),
                         reads=["cat_h", "cat_g", "ident_bf"], writes=["pA"])
                S.op("act", lambda e: e.activation(out=catT[:], in_=pA[:], func=AF.Identity), reads=["pA"], writes=["catT"])
                x1 = x1t[p]
                x1k = f"x1t{p}"
                for half in range(2):
                    zp, zk = zbank()
                    hsl = slice(half * 512, (half + 1) * 512)
                    for kc in range(8):
                        S.op("pe", lambda e, kc=kc, zp=zp, hsl=hsl: e.matmul(zp[:], catT[:, kc, :], wout_sb[:, kc, hsl],
                                                                             start=(kc == 0), stop=(kc == 7)),
                             reads=["catT", "wout"], writes=[zk])
                    S.op("dve", lambda e, zp=zp, hsl=hsl: e.tensor_tensor(out=x1[:, hsl], in0=zp[:], in1=gate1_rep[:, hsl], op=ALU.mult),
                         reads=[zk, f"gate_rep0_{half}"], writes=[x1k + f"_{half}"])
                    S.op("dve", lambda e, hsl=hsl: e.tensor_tensor(out=x1[:, hsl], in0=x1[:, hsl], in1=xi[:, hsl], op=ALU.add),
                         reads=[x1k + f"_{half}", xk], writes=[x1k + f"_{half}"])
                x1keys = [x1k + "_0", x1k + "_1"]
                S.op("sp", lambda e: e.dma_start(out=x1s[i * 128:(i + 1) * 128, :], in_=x1[:]), reads=x1keys, writes=[f"x1s{i}"],
                     chan="st_" + x1k)
                yield
                S.op("act", lambda e: e.activation(out=junk[:], in_=x1[:], func=AF.Square, accum_out=ss[:, 10:11]),
                     reads=x1keys, writes=["junk", "ss10"])
                rstd_from(ss[:, 10:11], rs[:, 10:11], D, ["ss10"], "rs10")
                xb = xh2b[p]
                xbk = f"xh2b{p}"
                S.op("dve", lambda e: e.tensor_scalar(out=xh2f[:], in0=x1[:], scalar1=rs[:, 10:11], scalar2=None, op0=ALU.mult),
                     reads=x1keys + ["rs10"], writes=["xh2f"])
                S.op("dve", lambda e: e.tensor_tensor(out=xh2f[:], in0=xh2f[:], in1=G2_rep[:], op=ALU.mult), reads=["xh2f", "G2_rep"], writes=["xh2f"])
                S.op("dve", lambda e: e.tensor_tensor(out=xh2f[:], in0=xh2f[:], in1=SH2_rep[:], op=ALU.add), reads=["xh2f", "gate_rep2_0", "gate_rep2_1"], writes=["xh2f"])
                S.op("act", lambda e: e.activation(out=xb[:], in_=xh2f[:], func=AF.Identity), reads=["xh2f"], writes=[xbk])
                for hf in range(2):
                    for t4 in range(4):
                        kc = hf * 4 + t4
                        S.op("pe", lambda e, kc=kc, t4=t4: e.transpose(pH[:, t4, :], xh2f[:, kc * 128:(kc + 1) * 128], ident_f[:]),
                             reads=["xh2f", "ident_f"], writes=["pH"])
                    ksl = slice(hf * 4, hf * 4 + 4)
                    S.op("dve", lambda e, ksl=ksl: e.tensor_copy(h2T[:, ksl, :], pH[:]), reads=["pH"], writes=[f"h2T{hf}"])
                for kc in range(8):
                    S.op("pe", lambda e, kc=kc: e.matmul(pE[:, 320:352], h2T[:, kc, :], rw[:, kc, :], start=(kc == 0), stop=(kc == 7)),
                         reads=["h2T0", "h2T1", "rw"], writes=["pE"])
                lg_i = lgt_all[:, i, :]
                mx_i = mx8_all[:, i, :]
                gt_i = gates_all[:, i, :]
                ps_i = pos_all[:, i, :]
                S.op("dve", lambda e: e.tensor_tensor(out=lg_i, in0=pE[:, 320:352], in1=rb_rep[:], op=ALU.add),
                     reads=["pE", "rb_rep"], writes=[f"lgt{i}"])
                S.op("dve", lambda e: e.max(mx_i, lg_i), reads=[f"lgt{i}"], writes=[f"mx8{i}"])
                S.op("dve", lambda e: e.tensor_scalar(out=msk[:], in0=lg_i, scalar1=mx8_all[:, i, 3:4], scalar2=None, op0=ALU.is_ge),
                     reads=[f"lgt{i}", f"mx8{i}"], writes=["msk"])
                S.op("dve", lambda e: e.tensor_copy(msk_bf[:], msk[:]), reads=["msk"], writes=["msk_bf"])
                S.op("dve", lambda e: e.tensor_scalar(out=nmx[:], in0=mx8_all[:, i, 0:1], scalar1=-1.0, scalar2=None, op0=ALU.mult),
                     reads=[f"mx8{i}"], writes=["nmx"])
                S.op("act", lambda e: e.activation(out=eexp[:], in_=lg_i, func=AF.Exp, bias=nmx[:]), reads=[f"lgt{i}", "nmx"], writes=["eexp"])
                S.op("dve", lambda e: e.scalar_tensor_tensor(out=eexp[:], in0=eexp[:], scalar=1.0, in1=msk[:], op0=ALU.mult, op1=ALU.mult, accum_out=gsum[:]),
                     reads=["eexp", "msk"], writes=["eexp", "gsum"])
                S.op("dve", lambda e: e.reciprocal(gsum[:], gsum[:]), reads=["gsum"], writes=["gsum"])
                S.op("dve", lambda e: e.tensor_scalar(out=gt_i, in0=eexp[:], scalar1=gsum[:, 0:1], scalar2=None, op0=ALU.mult),
                     reads=["eexp", "gsum"], writes=[f"gates{i}"])
                S.op("pe", lambda e: e.matmul(pE[:, 352:384], Ust_bf[:], msk_bf[:], start=True, stop=True), reads=["Ust", "msk_bf"], writes=["pE"])
                S.op("pe", lambda e: e.matmul(pE[:, 384:416], ones_bf[:], msk_bf[:], start=True, stop=True), reads=["ones_bf", "msk_bf"], writes=["pE"])
                S.op("dve", lambda e: e.tensor_tensor(out=ps_i, in0=pE[:, 352:384], in1=base_rep[:], op=ALU.add),
                     reads=["pE", "base_rep"], writes=[f"pos{i}"])
                S.op("dve", lambda e: e.tensor_tensor(out=base_rep[:], in0=base_rep[:], in1=pE[:, 384:416], op=ALU.add),
                     reads=["pE", "base_rep"], writes=["base_rep"])
                S.op("sp", lambda e: e.dma_start(out=XH[i * 128:(i + 1) * 128, :], in_=xb[:]), reads=[xbk], writes=[f"XH{i}"], chan="st_" + xbk)

            jobs = [(i, True) for i in range(NPRE)] + [(i, False) for i in range(NMAIN)]

            def drive(gens):
                live = list(gens)
                while live:
                    for g in list(live):
                        try:
                            next(g)
                        except StopIteration:
                            live.remove(g)

            def stage2_gen(n):
                i, pre = jobs[n]
                if (not pre) and i == 0:
                    S.op("dve", lambda e: e.tensor_scalar(out=Sst[:], in0=Sst[:], scalar1=pmask[:, 0:1], scalar2=None, op0=ALU.mult),
                         reads=["Sst", "pmask"], writes=["Sst"])
                return stage2(n, i, pre)

            zper = (len(zfill) + max(len(jobs), 1) - 1) // max(len(jobs), 1)

            def zero_some():
                for _ in range(zper):
                    if zfill:
                        z0 = zfill.pop(0)
                        S.op("sp", lambda e, z0=z0: e.dma_start(out=XSv[:, z0:z0 + 1024], in_=zt[:]), reads=["zt"], writes=[f"XSz{z0}"], chan="st_zt")

            if jobs:
                drive([stage1(0, *jobs[0])])
            for n in range(len(jobs)):
                zero_some()
                late_some(len(late) if not jobs[n][1] else 1)
                gens = []
                if n + 1 < len(jobs):
                    gens.append(stage1(n + 1, *jobs[n + 1]))
                gens.append(stage2_gen(n))
                drive(gens)
            while zfill:
                zero_some() if zper else zfill.clear()
            late_some(len(late))
            S.flush()

        with ExitStack() as ED:
            nbe = sb(ED, "nbe", [128, NE])
            tmp32 = sb(ED, "tmp32", [128, NE])
            padded = sb(ED, "padded", [128, NE])
            ones32 = sb(ED, "ones32", [128, NE])
            pad_end = sb(ED, "pad_end", [128, NE])
            start_pad = sb(ED, "start_pad", [128, NE])
            kblk = sb(ED, "kblk", [128, NB])
            cmp = sb(ED, "cmp", [128, NB, NE])
            be_rep = sb(ED, "be_rep", [128, NB])
            kcp = sb(ED, "kcp", [128, 8])
            pcol = sb(ED, "pcol", [128, 1])
            widx_f = sb(ED, "widx_f", [128, NB, 8])
            bidx_f = sb(ED, "bidx_f", [128, NB])
            destf = sb(ED, "destf", [128, NE])
            oh = sb(ED, "oh", [128, NE])
            dk = sb(ED, "dk", [128, 4])
            junk32 = sb(ED, "junk32", [128, NE])
            xb2 = [sb(ED, f"xb2_{i}", [128, D], BF16) for i in range(2)]

            S.op("dve", lambda e: e.tensor_scalar(out=nbe[:], in0=base_rep[:], scalar1=0.5, scalar2=None, op0=ALU.is_gt), writes=["nbe"])
            for j in range(1, MAXBE):
                S.op("dve", lambda e, j=j: e.tensor_scalar(out=tmp32[:], in0=base_rep[:], scalar1=float(j * CAP) + 0.5, scalar2=None, op0=ALU.is_gt),
                     writes=["tmp32"])
                S.op("dve", lambda e: e.tensor_tensor(out=nbe[:], in0=nbe[:], in1=tmp32[:], op=ALU.add), reads=["nbe", "tmp32"], writes=["nbe"])
            S.op("dve", lambda e: e.tensor_scalar(out=padded[:], in0=nbe[:], scalar1=float(CAP), scalar2=None, op0=ALU.mult), reads=["nbe"], writes=["padded"])
            S.op("pool", lambda e: e.memset(ones32[:], 1.0), writes=["ones32"])
            nlive_f = sb(ED, "nlive_f", [128, 1])
            S.op("dve", lambda e: e.tensor_reduce(out=nlive_f[:], in_=nbe[:], axis=mybir.AxisListType.X, op=ALU.add), reads=["nbe"], writes=["nlive_f"])
            S.op("dve", lambda e: e.tensor_copy(nlive_i[:], nlive_f[:]), reads=["nlive_f"], writes=["nlive_i"])
            S.op("dve", lambda e: e.tensor_tensor_scan(out=pad_end[:], data0=ones32[:], data1=padded[:], initial=0.0, op0=ALU.mult, op1=ALU.add),
                 reads=["ones32", "padded"], writes=["pad_end"])
            S.op("dve", lambda e: e.tensor_tensor(out=start_pad[:], in0=pad_end[:], in1=padded[:], op=ALU.subtract),
                 reads=["pad_end", "padded"], writes=["start_pad"])
            S.op("pool", lambda e: e.iota(kblk[:], pattern=[[CAP, NB]], base=0, channel_multiplier=0, allow_small_or_imprecise_dtypes=True), writes=["kblk"])
            S.op("dve", lambda e: e.tensor_tensor(out=cmp[:], in0=bc(pad_end[:, :].unsqueeze(1), [128, NB, NE]),
                                                  in1=bc(kblk[:, :].unsqueeze(2), [128, NB, NE]), op=ALU.is_le),
                 reads=["pad_end", "kblk"], writes=["cmp"])
            S.op("dve", lambda e: e.tensor_reduce(out=be_rep[:], in_=cmp[:], axis=mybir.AxisListType.X, op=ALU.add), reads=["cmp"], writes=["be_rep"])
            S.op("dve", lambda e: e.tensor_scalar(out=be_rep[:], in0=be_rep[:], scalar1=float(NE - 1), scalar2=None, op0=ALU.min), reads=["be_rep"], writes=["be_rep"])
            S.op("pool", lambda e: e.iota(kcp[:], pattern=[[128, 8]], base=0, channel_multiplier=1, allow_small_or_imprecise_dtypes=True), writes=["kcp"])
            S.op("pool", lambda e: e.iota(pcol[:], pattern=[[0, 1]], base=0, channel_multiplier=1, allow_small_or_imprecise_dtypes=True), writes=["pcol"])
            S.op("dve", lambda e: e.tensor_scalar(out=bidx_f[:], in0=be_rep[:], scalar1=float(D), scalar2=None, op0=ALU.mult), reads=["be_rep"], writes=["bidx_f"])
            S.op("dve", lambda e: e.tensor_tensor(out=widx_f[:], in0=bc(bidx_f[:, :].unsqueeze(2), [128, NB, 8]),
                                                  in1=bc(kcp[:, :].unsqueeze(1), [128, NB, 8]), op=ALU.add),
                 reads=["bidx_f", "kcp"], writes=["widx_f"])
            S.op("dve", lambda e: e.tensor_copy(widx[:], widx_f[:]), reads=["widx_f"], writes=["widx"])
            S.op("dve", lambda e: e.tensor_scalar(out=bidx_f[:], in0=be_rep[:], scalar1=128.0, scalar2=pcol[:, 0:1], op0=ALU.mult, op1=ALU.add),
                 reads=["be_rep", "pcol", "widx_f"], writes=["bidx_f"])
            S.op("dve", lambda e: e.tensor_copy(bidx[:], bidx_f[:]), reads=["bidx_f"], writes=["bidx"])
            S.op("dve", lambda e: e.tensor_scalar(out=sel[:], in0=be_rep[:], scalar1=pcol[:, 0:1], scalar2=None, op0=ALU.is_equal),
                 reads=["be_rep", "pcol"], writes=["sel"])
            for i in range(NMAIN):
                xb = xb2[i % 2]
                xbk = f"xb2_{i % 2}"
                S.op("sp", lambda e, i=i, xb=xb: e.dma_start(out=xb[:], in_=XH[i * 128:(i + 1) * 128, :]), writes=[xbk], chan="ld_" + xbk)
                S.op("dve", lambda e, i=i: e.tensor_tensor(out=destf[:], in0=pos_all[:, i, :], in1=start_pad[:], op=ALU.add),
                     reads=["start_pad"], writes=["destf"])
                for k in range(4):
                    S.op("dve", lambda e, i=i, k=k: e.tensor_scalar(out=oh[:], in0=lgt_all[:, i, :], scalar1=mx8_all[:, i, k:k + 1], scalar2=None, op0=ALU.is_equal),
                         writes=["oh"])
                    S.op("dve", lambda e, k=k: e.scalar_tensor_tensor(out=junk32[:], in0=oh[:], scalar=1.0, in1=destf[:], op0=ALU.mult, op1=ALU.mult,
                                                                      accum_out=dk[:, k:k + 1]), reads=["oh", "destf"], writes=["junk32", f"dk{k}"])
                    S.op("dve", lambda e, i=i, k=k: e.scalar_tensor_tensor(out=junk32[:], in0=oh[:], scalar=1.0, in1=gates_all[:, i, :], op0=ALU.mult, op1=ALU.mult,
                                                                           accum_out=gate_all[:, i, k:k + 1]), reads=["oh"], writes=["junk32", f"gate_all{i}_{k}"])
                S.op("dve", lambda e, i=i: e.tensor_copy(dest_all[:, i, :], dk[:]), reads=[f"dk{k}" for k in range(4)], writes=[f"dest_all{i}"])
                for k in range(4):
                    S.op("pool", lambda e, i=i, k=k, xb=xb: e.indirect_dma_start(
                        out=XS[:, :], out_offset=bass.IndirectOffsetOnAxis(ap=dest_all[:, i, k:k + 1], axis=0),
                        in_=xb[:, :], in_offset=None, bounds_check=S.pool_bound, oob_is_err=False),
                        reads=[xbk, f"dest_all{i}"], writes=[f"XS_{i}_{k}"], chan="sc_" + xbk)
            S.flush()

        with ExitStack() as E2:
            wg_sb = [sb(E2, f"wg{i}", [128, 8, 2 * D], BF16) for i in range(2)]
            wd_sb = [sb(E2, f"wd{i}", [128, 8, D], BF16) for i in range(2)]
            xg = [sb(E2, f"xg{i}", [128, NJ, D], BF16) for i in range(2)]
            bgc = [sb(E2, f"bgc{i}", [128, 16]) for i in range(2)]
            sel_full = sb(E2, "sel_full", [32, NB, 128], BF16)
            bd_f = sb(E2, "bd_f", [32, D])
            bd_bf = sb(E2, "bd_bf", [32, D], BF16)
            XTs = [sb(E2, f"XT{i}", [128, 8, CAP], BF16) for i in range(2)]
            bgc7 = [sb(E2, f"bgc7_{i}", [128, 8]) for i in range(2)]
            AT = sb(E2, "AT", [128, 8, CAP], BF16)
            gmin = [sb(E2, f"gmin{i}", [128, CAP]) for i in range(2)]
            sgm = [sb(E2, f"sgm{i}", [128, CAP]) for i in range(2)]
            up1 = [sb(E2, f"up1{i}", [128, CAP]) for i in range(2)]
            yst = [sb(E2, f"yst{i}", [128, NJ, D]) for i in range(2)]
            qA = ps(E2, "qA", [128, 8, 128], BF16)
            qg = [ps(E2, f"qg{i}", [128, 512]) for i in range(2)]
            qu = [ps(E2, f"qu{i}", [128, 512]) for i in range(2)]
            qy = [ps(E2, f"qy{i}", [128, 512]) for i in range(2)]

            S.op("sp", lambda e: e.dma_start(out=bd_f[:], in_=bdn), writes=["bd_f"], chan="ld_bd_f")
            S.op("dve", lambda e: e.tensor_copy(bd_bf[:], bd_f[:]), reads=["bd_f"], writes=["bd_bf"])
            S.op("dve", lambda e: e.tensor_copy(sel_full[:], bc(sel[0:32, :].unsqueeze(2), [32, NB, 128])), writes=["sel_full"])

            def prefetch(k):
                sl = k % 2
                for kc in range(8):
                    S.op("pool", lambda e, kc=kc: e.indirect_dma_start(
                        out=wg_sb[sl][:, kc, :], out_offset=None, in_=wgu[:, :],
                        in_offset=bass.IndirectOffsetOnAxis(ap=widx[:, k, kc:kc + 1], axis=0),
                        bounds_check=S.pool_wbound, oob_is_err=False), writes=[f"wg{sl}_{kc}"], chan=f"ld_wg{sl}")
                for kc in range(8):
                    S.op("pool", lambda e, kc=kc: e.indirect_dma_start(
                        out=wd_sb[sl][:, kc, :], out_offset=None, in_=wdn[:, :],
                        in_offset=bass.IndirectOffsetOnAxis(ap=widx[:, k, kc:kc + 1], axis=0),
                        bounds_check=S.pool_wbound, oob_is_err=False), writes=[f"wd{sl}_{kc}"], chan=f"ld_wd{sl}")
                S.op("pool", lambda e: e.indirect_dma_start(
                    out=bgc[sl][:, :], out_offset=None, in_=bgu_tab[:, :],
                    in_offset=bass.IndirectOffsetOnAxis(ap=bidx[:, k:k + 1], axis=0),
                    bounds_check=S.pool_wbound, oob_is_err=False), writes=[f"bgc{sl}"], chan=f"ld_bgc{sl}")
                S.op("sp", lambda e: e.dma_start(out=xg[sl][:], in_=XS[k * CAP:(k + 1) * CAP, :].rearrange("(j p) d -> p j d", p=128)),
                     writes=[f"xg{sl}"], chan=f"ld_xg{sl}")

            if NB_RUN > 0:
                prefetch(0)

            def xprep(k):
                sl = k % 2
                XT = XTs[sl]
                for j in range(NJ):
                    for kc in range(8):
                        S.op("pe", lambda e, j=j, kc=kc: e.transpose(qA[:, kc, :], xg[sl][:, j, kc * 128:(kc + 1) * 128], ident_bf[:]),
                             reads=[f"xg{sl}"], writes=["qA"])
                    S.op("act", lambda e, j=j: e.activation(out=XT[:, :, j * 128:(j + 1) * 128], in_=qA[:], func=AF.Identity),
                         reads=["qA"], writes=[f"XT{sl}_{j}"])
                S.op("dve", lambda e: e.tensor_scalar(out=bgc7[sl][:], in0=bgc[sl][:, 8:16], scalar1=7.0, scalar2=None, op0=ALU.add),
                     reads=[f"bgc{sl}"], writes=[f"bgc7_{sl}"])

            def block(k):
                sl = k % 2
                XT = XTs[sl]
                if k + 1 < NB_RUN:
                    prefetch(k + 1)
                xtk = [f"XT{sl}_{j}" for j in range(NJ)]
                wgk = [f"wg{sl}_{kc}" for kc in range(8)]
                wdk = [f"wd{sl}_{kc}" for kc in range(8)]
                for fc in range(8):
                    b = fc % 2
                    for kc in range(8):
                        S.op("pe", lambda e, fc=fc, kc=kc, b=b: e.matmul(qg[b][:, 0:CAP], wg_sb[sl][:, kc, fc * 128:(fc + 1) * 128], XT[:, kc, :],
                                                                         start=(kc == 0), stop=(kc == 7)),
                             reads=xtk + wgk, writes=[f"qg{b}"])
                    for kc in range(8):
                        S.op("pe", lambda e, fc=fc, kc=kc, b=b: e.matmul(qu[b][:, 0:CAP], wg_sb[sl][:, kc, D + fc * 128:D + (fc + 1) * 128], XT[:, kc, :],
                                                                         start=(kc == 0), stop=(kc == 7)),
                             reads=xtk + wgk, writes=[f"qu{b}"])
                    S.op("dve", lambda e, fc=fc, b=b: e.tensor_scalar(out=gmin[b][:], in0=qg[b][:, 0:CAP], scalar1=bgc[sl][:, fc:fc + 1], scalar2=7.0,
                                                                      op0=ALU.add, op1=ALU.min), reads=[f"qg{b}", f"bgc{sl}"], writes=[f"gmin{b}"])
                    S.op("act", lambda e, b=b: e.activation(out=sgm[b][:], in_=gmin[b][:], func=AF.Silu, scale=1.702),
                         reads=[f"gmin{b}"], writes=[f"sgm{b}"])
                    S.op("act", lambda e, fc=fc, b=b: e.activation(out=up1[b][:], in_=qu[b][:, 0:CAP], func=AF.Relu, bias=bgc7[sl][:, fc:fc + 1]),
                         reads=[f"qu{b}", f"bgc7_{sl}"], writes=[f"up1{b}"])
                    S.op("dve", lambda e, b=b: e.tensor_scalar(out=up1[b][:], in0=up1[b][:], scalar1=14.0, scalar2=-6.0, op0=ALU.min, op1=ALU.add),
                         reads=[f"up1{b}"], writes=[f"up1{b}"])
                    S.op("dve", lambda e, fc=fc, b=b: e.scalar_tensor_tensor(out=AT[:, fc, :], in0=sgm[b][:], scalar=1.0 / 1.702, in1=up1[b][:],
                                                                             op0=ALU.mult, op1=ALU.mult),
                         reads=[f"up1{b}", f"sgm{b}"], writes=[f"AT{fc}"])
                if k + 1 < NB_RUN:
                    xprep(k + 1)
                atk = [f"AT{fc}" for fc in range(8)]
                ys = yst[sl]
                n = 0
                for j in range(NJ):
                    for half in range(2):
                        b = n % 2
                        n += 1
                        hsl = slice(half * 512, (half + 1) * 512)
                        for fc in range(8):
                            S.op("pe", lambda e, j=j, fc=fc, b=b, hsl=hsl: e.matmul(qy[b][:], AT[:, fc, j * 128:(j + 1) * 128], wd_sb[sl][:, fc, hsl],
                                                                                   start=(fc == 0), stop=False),
                                 reads=atk + wdk, writes=[f"qy{b}"])
                        S.op("pe", lambda e, b=b, hsl=hsl: e.matmul(qy[b][:], sel_full[:, k, :], bd_bf[:, hsl], start=False, stop=True),
                             reads=["bd_bf", "sel_full"], writes=[f"qy{b}"])
                        S.op("act", lambda e, j=j, b=b, hsl=hsl: e.activation(out=ys[:, j, hsl], in_=qy[b][:], func=AF.Identity),
                             reads=[f"qy{b}"], writes=[f"yst{sl}_{j}_{half}"])
                g_save, S.grp = S.grp, None
                S.op("sp", lambda e: e.dma_start(out=YS[k * CAP:(k + 1) * CAP, :].rearrange("(j p) d -> p j d", p=128), in_=ys[:]),
                     reads=[f"yst{sl}_{j}_{h}" for j in range(NJ) for h in range(2)], writes=[f"YS{k}"], chan=f"st_yst{sl}")
                S.grp = g_save

            for i2 in range(2):
                S.op("pool", lambda e, i2=i2: e.memset(yst[i2][:], 0.0), writes=[f"yst{i2}_{j}_{h}" for j in range(NJ) for h in range(2)])
            if NB_RUN > 0:
                xprep(0)
            S.grp_ap = nlive_i[0:1, 0:1]
            for k in range(NB_RUN):
                S.grp = k if SKIP else None
                block(k)
            S.grp = None
            S.flush()

        with ExitStack() as E3:
            x1c = [sb(E3, f"x1c{i}", [128, D]) for i in range(2)]
            yk = [[sb(E3, f"yk{i}_{k}", [128, D]) for k in range(4)] for i in range(2)]
            acc = [sb(E3, f"acc{i}", [128, D]) for i in range(2)]
            acc2 = [sb(E3, f"accb{i}", [128, D]) for i in range(2)]
            ot = [sb(E3, f"ot{i}", [128, D]) for i in range(2)]
            cjunk = sb(E3, "cjunk", [128, D], BF16)
            css = sb(E3, "css", [128, 2])
            crs = sb(E3, "crs", [128, 2])
            for i2 in range(2):
                for k in range(4):
                    S.op("pool", lambda e, i2=i2, k=k: e.memset(yk[i2][k][:], 0.0), writes=[f"yk{i2}_{k}"])
            for i in range(NMAIN if COMB else 0):
                b = i % 2
                S.op("sp", lambda e, i=i, b=b: e.dma_start(out=x1c[b][:], in_=x1s[i * 128:(i + 1) * 128, :]), writes=[f"x1c{b}"], chan=f"ld_x1c{b}")
                for k in range(4):
                    S.op("pool", lambda e, i=i, b=b, k=k: e.indirect_dma_start(
                        out=yk[b][k][:, :], out_offset=None, in_=YS[:, :],
                        in_offset=bass.IndirectOffsetOnAxis(ap=dest_all[:, i, k:k + 1], axis=0),
                        bounds_check=S.pool_bound, oob_is_err=False), writes=[f"yk{b}_{k}"], chan=f"ld_yk{b}_{k}")
                S.op("act", lambda e, i=i, b=b: e.activation(out=acc[b][:], in_=yk[b][0][:], func=AF.Identity, scale=gate_all[:, i, 0:1]),
                     reads=[f"yk{b}_0"], writes=[f"acc{b}"])
                for k in range(1, 4):
                    S.op("dve", lambda e, i=i, b=b, k=k: e.scalar_tensor_tensor(out=acc[b][:], in0=yk[b][k][:], scalar=gate_all[:, i, k:k + 1], in1=acc[b][:],
                                                                               op0=ALU.mult, op1=ALU.add), reads=[f"yk{b}_{k}", f"acc{b}"], writes=[f"acc{b}"])
                S.op("pool", lambda e, b=b: e.tensor_tensor(out=acc2[b][:], in0=acc[b][:], in1=gate2_rep[:], op=ALU.mult), reads=[f"acc{b}"], writes=[f"accb{b}"])
                S.op("dve", lambda e, b=b: e.tensor_tensor(out=acc2[b][:], in0=acc2[b][:], in1=x1c[b][:], op=ALU.add), reads=[f"accb{b}", f"x1c{b}"], writes=[f"accb{b}"])
                S.op("act", lambda e, b=b: e.activation(out=cjunk[:], in_=acc2[b][:], func=AF.Square, accum_out=css[:, b:b + 1]), reads=[f"accb{b}"], writes=["cjunk", f"css{b}"])
                S.op("act", lambda e, b=b: e.activation(out=crs[:, b:b + 1], in_=css[:, b:b + 1], func=AF.Ln, scale=1.0 / D, bias=eps_t[:]), reads=[f"css{b}"], writes=[f"crs{b}"])
                S.op("act", lambda e, b=b: e.activation(out=crs[:, b:b + 1], in_=crs[:, b:b + 1], func=AF.Exp, scale=-0.5), reads=[f"crs{b}"], writes=[f"crs{b}"])
                S.op("dve", lambda e, b=b: e.scalar_tensor_tensor(out=ot[b][:], in0=acc2[b][:], scalar=crs[:, b:b + 1], in1=fg_rep[:], op0=ALU.mult, op1=ALU.mult),
                     reads=[f"accb{b}", f"crs{b}"], writes=[f"ot{b}"])
                S.op("sp", lambda e, i=i, b=b: e.dma_start(out=out_d[i * 128:(i + 1) * 128, :], in_=ot[b][:]), reads=[f"ot{b}"], writes=[f"out{i}"],
                     chan=f"st_ot{b}")
            S.flush()
    return nc


def make_in_maps(x, c, ada_w, ada_b, norm_mix_g, w_in, lb_params, hgrn_norm_g, gmlp_ln_g, gmlp_ln_b,
                 gmlp_ws, gmlp_bs, gmlp_norm_g, w_out, norm_ffn_g, router_w, router_b, w_gate_up,
                 b_gate_up, w_down, b_down, final_g):
    f = lambda a: np.ascontiguousarray(np.asarray(a, dtype=np.float32))
    x = f(x)
    c = f(c)
    col = lambda v: f(np.asarray(v, np.float32).reshape(-1, 128).T)
    shared = dict(
        ada_w=f(ada_w[0]), ada_b_col=col(ada_b[0]), ada_b=f(ada_b[0]),
        g1_col=col(norm_mix_g[0]), g2_row=f(norm_ffn_g[0]),
        w_in=f(w_in[0]), lbp=f(lb_params), hg=f(hgrn_norm_g[0]), lng=f(gmlp_ln_g[0]), lnb=f(gmlp_ln_b[0]),
        gng=f(gmlp_norm_g[0]),
        wsT=f(np.transpose(np.asarray(gmlp_ws[0], np.float32), (2, 0, 1))),
        bs_col=f(np.asarray(gmlp_bs[0], np.float32).T),
        w_out=f(w_out[0]),
        rw=f(np.asarray(router_w[0], np.float32).reshape(8, 128, NE).transpose(1, 0, 2)),
        rb=f(router_b[0]),
        wgu=f(np.asarray(w_gate_up[0], np.float32).reshape(NE * D, 2 * D)),
        bgu_tab=f(np.asarray(b_gate_up[0], np.float32).reshape(NE, 16, 128).transpose(0, 2, 1).reshape(NE * 128, 16)),
        wdn=f(np.asarray(w_down[0], np.float32).reshape(NE * D, D)), bdn=f(b_down[0]), fg=f(final_g),
    )
    maps = []
    for core in range(8):
        b, half = core // 2, core % 2
        m = dict(shared)
        m["x_main"] = f(x[b, half * TOK:(half + 1) * TOK])
        m["x_pre"] = f(x[b, 0:TOK])
        m["pmask"] = np.full((128, 1), float(half), np.float32)
        m["c_col"] = col(c[b])
        maps.append(m)
    return maps


def kernel(**inputs):
    maps = make_in_maps(**inputs)
    nc = build()
    res = run_bass_kernel_spmd(nc, maps, core_ids=list(range(8)))
    out = np.empty((4, 2 * TOK, D), np.float32)
    for core in range(8):
        b, half = core // 2, core % 2
        out[b, half * TOK:(half + 1) * TOK] = res.results[core]["out"]
    if DBG:
        kernel.dbg = [r for r in res.results]
    return out
```

```python
import os
from contextlib import ExitStack

import numpy as np
import concourse.bass as bass
import concourse.mybir as mybir
from concourse.bass_utils import run_bass_kernel_spmd

F32 = mybir.dt.float32
BF16 = mybir.dt.bfloat16
I32 = mybir.dt.int32
AF = mybir.ActivationFunctionType
ALU = mybir.AluOpType

D = 1024
NT = 16
TOK = NT * 128
NE = 32
CAP = 384
NJ = CAP // 128
NB = (TOK * 4 + NE * (CAP - 1) + CAP - 1) // CAP
NROWS = NB * CAP
MAXBE = (TOK + CAP - 1) // CAP
EPS = 1e-6
BIGOFF = 1.0e6
DBG = bool(int(os.environ.get("MK_DBG", "0")))
NEXP_RUN = int(os.environ.get("MK_NEXP", str(NE)))
NPRE = int(os.environ.get("MK_NPRE", str(NT)))
NMAIN = int(os.environ.get("MK_NMAIN", str(NT)))
COMB = int(os.environ.get("MK_COMB", "1"))
CUT = int(os.environ.get("MK_CUT", "99"))
SKIP = int(os.environ.get("MK_SKIP", "1"))
VAR = os.environ.get("MK_VAR", "")
NEW = NE
NB_RUN = int(os.environ.get("MK_NB", str(NB)))


class Sched:
    ENGS = ("pe", "act", "dve", "pool", "sp")

    def __init__(self, nc, es):
        self.nc, self.es = nc, es
        self.rec = {e: [] for e in self.ENGS}
        self.chan = {}
        self.sems = {}
        self.lastw, self.readers = {}, {}
        self.known = {e: {} for e in self.ENGS}
        self.nsem = 0
        self.dma_final = {}
        self.dma_cur = {}
        self.dma_base = {}
        self.dma_open = {}
        self.grp = None
        self.cur_grp = {e: None for e in self.ENGS}
        self.grp_ap = None

    def _signal(self, ch, inc):
        c = self.chan.get(ch)
        if c is None or c[1] + inc > 30000:
            name = f"s{self.nsem}"
            self.nsem += 1
            self.sems[name] = self.es.enter_context(self.nc.semaphore(name))
            c = [name, 0]
            self.chan[ch] = c
        c[1] += inc
        return (c[0], c[1])

    def op(self, eng, fn, reads=(), writes=(), chan=None):
        if self.grp != self.cur_grp[eng]:
            self.cur_grp[eng] = self.grp
            self.known[eng] = {}
        deps = {}
        closing = set()

        def add(sv):
            if sv is None:
                return
            v = sv[1]
            if sv[0] in self.dma_cur:
                base = self.dma_base.get(sv[0], 0)
                if v <= base:
                    v = base
                else:
                    v = self.dma_cur[sv[0]]
                    closing.add(sv[0])
            if v > deps.get(sv[0], 0):
                deps[sv[0]] = v

        for k in reads:
            add(self.lastw.get(k))
        for k in writes:
            add(self.lastw.get(k))
            for r in self.readers.get(k, ()):
                add(r)
        own = self.chan.get(eng)
        waits = []
        for s, v in deps.items():
            if eng == "pe" and own is not None and s == own[0]:
                continue
            if self.known[eng].get(s, 0) >= v:
                continue
            self.known[eng][s] = v
            waits.append((s, v))
        sig = self._signal(chan if chan else eng, 16 if chan else 1)
        for s in closing:
            self.dma_open[s] = False
        if chan:
            if self.grp is None:
                self.dma_final[chan] = sig
            if not self.dma_open.get(sig[0], False):
                self.dma_open[sig[0]] = True
                self.dma_base[sig[0]] = sig[1] - 16
            self.dma_cur[sig[0]] = sig[1]
        self.rec[eng].append((waits, fn, sig, 16 if chan else 1, self.grp, bool(chan)))
        for k in reads:
            self.readers.setdefault(k, []).append(sig)
        for k in writes:
            self.lastw[k] = sig
            self.readers[k] = []

    def flush(self):
        for ch, sig in self.dma_final.items():
            if self.known["sp"].get(sig[0], 0) < sig[1]:
                self.known["sp"][sig[0]] = sig[1]
                self.rec["sp"].append(([sig], None, None, 0, None, False))
        self.dma_final = {}
        rec, sems = self.rec, self.sems

        def emit(engname):
            def emit_op(e, o):
                waits, fn, sig, inc = o[0], o[1], o[2], o[3]
                for sn, v in waits:
                    e.wait_ge(sems[sn], v)
                if fn is not None:
                    fn(e).then_inc(sems[sig[0]], inc)

            def body(e):
                if engname == "pool":
                    self.pool_bound = e.to_reg(NROWS - 1)
                    self.pool_wbound = e.to_reg(NE * D - 1)
                ops = rec[engname]
                reg = None
                if any(o[4] is not None for o in ops):
                    reg = e.alloc_register("nlive_" + engname)
                    e.reg_load(reg, self.grp_ap)
                i = 0
                while i < len(ops):
                    g = ops[i][4]
                    if g is None:
                        emit_op(e, ops[i])
                        i += 1
                        continue
                    j = i
                    while j < len(ops) and ops[j][4] == g:
                        j += 1
                    tot, base, dlast = {}, {}, {}
                    for o in ops[i:j]:
                        if o[1] is None:
                            continue
                        if o[5]:
                            dlast[o[2][0]] = o[2][1]
                        else:
                            base.setdefault(o[2][0], o[2][1] - o[3])
                            tot[o[2][0]] = tot.get(o[2][0], 0) + o[3]
                    with e.If_lt(reg, g + 1):
                        for sn, t in tot.items():
                            if base[sn] > 0:
                                e.wait_ge(sems[sn], base[sn])
                            e.sem_inc(sems[sn], t)
                    with e.Else():
                        for o in ops[i:j]:
                            emit_op(e, o)
                        for sn, v in dlast.items():
                            e.wait_ge(sems[sn], v)
                    i = j
            return body

        with self.nc.Block() as block:
            block.tensor(emit("pe"))
            block.scalar(emit("act"))
            block.vector(emit("dve"))
            block.gpsimd(emit("pool"))
            block.sync(emit("sp"))
        self.rec = {e: [] for e in self.ENGS}
        self.lastw, self.readers = {}, {}
        self.chan = {}
        self.known = {e: {} for e in self.ENGS}
        self.grp = None
        self.cur_grp = {e: None for e in self.ENGS}


def build():
    nc = bass.Bass("TRN2", target_bir_lowering=False)

    def din(name, shape, dt=F32):
        return nc.dram_tensor(name, list(shape), dt, kind="ExternalInput").ap()

    x_main = din("x_main", [TOK, D])
    x_pre = din("x_pre", [TOK, D])
    pmask_d = din("pmask", [128, 1])
    c_col_d = din("c_col", [128, 8])
    ada_w = din("ada_w", [D, 6 * D])
    ada_b_col_d = din("ada_b_col", [128, 48])
    ada_b_d = din("ada_b", [6 * D])
    g1_col_d = din("g1_col", [128, 8])
    g2_row_d = din("g2_row", [D])
    w_in = din("w_in", [D, 3072])
    lbp_d = din("lbp", [2, 512])
    hg_d = din("hg", [512])
    lng_d = din("lng", [512])
    lnb_d = din("lnb", [512])
    gng_d = din("gng", [512])
    wsT_d = din("wsT", [128, 4, 128])
    bs_col_d = din("bs_col", [128, 4])
    w_out = din("w_out", [D, D])
    rw_d = din("rw", [128, 8, 32])
    rb_d = din("rb", [32])
    wgu = din("wgu", [NE * D, 2 * D])
    bgu_tab = din("bgu_tab", [NE * 128, 16])
    wdn = din("wdn", [NE * D, D])
    bdn = din("bdn", [NE, D])
    fg_d = din("fg", [D])
    out_d = nc.dram_tensor("out", [TOK, D], F32, kind="ExternalOutput").ap()
    x1s = nc.dram_tensor("x1s", [TOK, D], F32, kind="ExternalOutput" if DBG else "Internal").ap()
    XS = nc.dram_tensor("XS", [NROWS, D], BF16, kind="Internal").ap()
    YS = nc.dram_tensor("YS", [NROWS, D], F32, kind="Internal").ap()
    XH = nc.dram_tensor("XH", [TOK, D], BF16, kind="Internal").ap()

    with ExitStack() as ES:
        S = Sched(nc, ES)

        def sb(es, name, shape, dt=F32):
            return es.enter_context(nc.sbuf_tensor("sb_" + name, list(shape), dt))

        def ps(es, name, shape, dt=F32):
            return es.enter_context(nc.psum_tensor("ps_" + name, list(shape), dt))

        def bc(ap, shape):
            return ap.to_broadcast(list(shape))

        ident_bf = sb(ES, "ident_bf", [128, 128], BF16)
        ident_f = sb(ES, "ident_f", [128, 128])
        modT = sb(ES, "modT", [128, 48])
        G1T = sb(ES, "G1T", [128, 8])
        gate2_rep = sb(ES, "gate2_rep", [128, D])
        fg_rep = sb(ES, "fg_rep", [128, D])
        lgt_all = sb(ES, "lgt_all", [128, NT, NE])
        mx8_all = sb(ES, "mx8_all", [128, NT, 8])
        gates_all = sb(ES, "gates_all", [128, NT, NE])
        pos_all = sb(ES, "pos_all", [128, NT, NE])
        base_rep = sb(ES, "base_rep", [128, NE])
        widx = sb(ES, "widx", [128, NB, 8], I32)
        bidx = sb(ES, "bidx", [128, NB], I32)
        sel = sb(ES, "sel", [128, NB])
        nlive_i = sb(ES, "nlive_i", [128, 1], I32)
        dest_all = sb(ES, "dest_all", [128, NT, 4], I32)
        gate_all = sb(ES, "gate_all", [128, NT, 4])
        eps_t = sb(ES, "eps_t", [128, 1])
        pmask = sb(ES, "pmask", [128, 1])

        with ExitStack() as E1:
            gate1_rep = sb(E1, "gate1_rep", [128, D])
            SH2_rep = sb(E1, "SH2_rep", [128, D])
            G2_rep = sb(E1, "G2_rep", [128, D])
            lb_rep = sb(E1, "lb_rep", [128, 512])
            oml_rep = sb(E1, "oml_rep", [128, 512])
            hg_rep = sb(E1, "hg_rep", [128, 512])
            lng_rep = sb(E1, "lng_rep", [128, 512])
            lnb_rep = sb(E1, "lnb_rep", [128, 512])
            gng_rep = sb(E1, "gng_rep", [128, 512])
            wsT_f = sb(E1, "wsT_f", [128, 4, 128])
            wsT_bf = sb(E1, "wsT_bf", [128, 4, 128], BF16)
            bs_col = sb(E1, "bs_col", [128, 4])
            Lst = sb(E1, "Lst", [128, 128])
            cones = sb(E1, "cones", [128, 2])
            cmask = sb(E1, "cmask", [128, 4, 64])
            Ust_bf = sb(E1, "Ust_bf", [128, 128], BF16)
            ones_bf = sb(E1, "ones_bf", [128, 128], BF16)
            ecol = sb(E1, "ecol", [128, NE])
            rw = sb(E1, "rw", [128, 8, 32])
            rb_rep = sb(E1, "rb_rep", [128, 32])
            c_col = sb(E1, "c_col", [128, 8])
            c_sig = sb(E1, "c_sig", [128, 8])
            c_act = sb(E1, "c_act", [128, 8])
            c_rep = sb(E1, "c_rep", [128, 8, 128], BF16)
            ada_b_col = sb(E1, "ada_b_col", [128, 48])
            g1_col = sb(E1, "g1_col", [128, 8])
            adaw_sb = [sb(E1, f"adaw{i}", [128, 8, 256], BF16) for i in range(2)]
            mod_rep = sb(E1, "mod_rep", [128, 256])
            win_sb = sb(E1, "win_sb", [128, 8, 3072], BF16)
            wout_sb = sb(E1, "wout_sb", [128, 8, D], BF16)
            Sst = sb(E1, "Sst", [128, 4, 128])
            Sp_bf = sb(E1, "Sp_bf", [128, 4, 128], BF16)
            xin = [sb(E1, f"xin{i}", [128, D]) for i in range(2)]
            junk = sb(E1, "junk", [128, D], BF16)
            ss = sb(E1, "ss", [128, 16])
            rs = sb(E1, "rs", [128, 16])
            xhat = sb(E1, "xhat", [128, D], BF16)
            hT = sb(E1, "hT", [128, 8, 128], BF16)
            q_sb = sb(E1, "q_sb", [128, 512])
            sg = sb(E1, "sg", [128, 512])
            sg2 = sb(E1, "sg2", [128, 512], BF16)
            f_sb = sb(E1, "f_sb", [128, 512])
            g_sb = sb(E1, "g_sb", [128, 512])
            kk = sb(E1, "kk", [128, 512])
            Eexp = sb(E1, "Eexp", [128, 512])
            decs = [sb(E1, f"dec{i}", [128, 8]) for i in range(2)]
            Kd_bfs = [sb(E1, f"Kd_bf{i}", [128, 512], BF16) for i in range(2)]
            Qm_bfs = [sb(E1, f"Qm_bf{i}", [128, 512], BF16) for i in range(2)]
            v_bfs = [sb(E1, f"v_bf{i}", [128, 512], BF16) for i in range(2)]
            ggs = [sb(E1, f"gg{i}", [128, 512]) for i in range(2)]
            u_sbs = [sb(E1, f"u_sb{i}", [128, 512]) for i in range(2)]
            vv = sb(E1, "vv", [128, 512])
            bst = sb(E1, "bst", [128, 6])
            bag = sb(E1, "bag", [128, 2])
            nmr = sb(E1, "nmr", [128, 1])
            vn = sb(E1, "vn", [128, 512])
            vn_bfs = [sb(E1, f"vn_bf{i}", [128, 512], BF16) for i in range(2)]
            QKT = sb(E1, "QKT", [128, 8, 128], BF16)
            scm = sb(E1, "scm", [128, 4, 64], BF16)
            o_t = sb(E1, "o_t", [128, 512])
            yg = sb(E1, "yg", [128, 512])
            cat = sb(E1, "cat", [128, D], BF16)
            catT = sb(E1, "catT", [128, 8, 128], BF16)
            x1t = [sb(E1, f"x1t{i}", [128, D]) for i in range(2)]
            xh2f = sb(E1, "xh2f", [128, D])
            xh2b = [sb(E1, f"xh2b{i}", [128, D], BF16) for i in range(2)]
            h2T = sb(E1, "h2T", [128, 8, 128])
            nmx = sb(E1, "nmx", [128, 1])
            msk = sb(E1, "msk", [128, NE])
            msk_bf = sb(E1, "msk_bf", [128, NE], BF16)
            eexp = sb(E1, "eexp", [128, NE])
            gsum = sb(E1, "gsum", [128, 1])
            pA = ps(E1, "pA", [128, 8, 128], BF16)
            pB = ps(E1, "pB", [128, 512])
            pC = ps(E1, "pC", [128, 512])
            pD = ps(E1, "pD", [128, 512])
            pE = ps(E1, "pE", [128, 512])
            pF = ps(E1, "pF", [128, 512])
            pG = ps(E1, "pG", [128, 512])
            pH = ps(E1, "pH", [128, 4, 128])
            zb = [pB, pC]
            zbk = ["pB", "pC"]
            zctr = [0]

            def zbank():
                i = 0 if "onebank" in VAR else zctr[0] % 2
                zctr[0] += 1
                return zb[i], zbk[i]

            S.op("pool", lambda e: e.memset(ident_f[:], 0.0), writes=["ident_f"])
            S.op("pool", lambda e: e.affine_select(out=ident_f[:], in_=ident_f[:], pattern=[[-1, 128]],
                                                    compare_op=ALU.not_equal, fill=1.0, base=0, channel_multiplier=1),
                 reads=["ident_f"], writes=["ident_f"])
            S.op("pool", lambda e: e.tensor_copy(ident_bf[:], ident_f[:]), reads=["ident_f"], writes=["ident_bf"])
            S.op("pool", lambda e: e.memset(eps_t[:], EPS), writes=["eps_t"])
            S.op("pool", lambda e: e.memset(Lst[:], 1.0), writes=["Lst"])
            S.op("pool", lambda e: e.affine_select(out=Lst[:], in_=Lst[:], pattern=[[-1, 128]], compare_op=ALU.is_gt,
                                                    fill=0.0, base=0, channel_multiplier=1), reads=["Lst"], writes=["Lst"])
            S.op("pool", lambda e: e.memset(Lst[64:128, 0:64], 0.0), reads=["Lst"], writes=["Lst"])
            S.op("pool", lambda e: e.memset(cones[:], 0.0), writes=["cones"])
            S.op("pool", lambda e: e.memset(cones[0:64, 0:1], 1.0), reads=["cones"], writes=["cones"])
            S.op("pool", lambda e: e.memset(cones[64:128, 1:2], 1.0), reads=["cones"], writes=["cones"])
            S.op("pool", lambda e: e.memset(cmask[:], 1.0), writes=["cmask"])
            for hf in range(2):
                S.op("pool", lambda e, hf=hf: e.affine_select(
                    out=cmask[hf * 64:(hf + 1) * 64], in_=cmask[hf * 64:(hf + 1) * 64], pattern=[[0, 4], [1, 64]],
                    compare_op=ALU.is_ge, fill=0.0, base=0, channel_multiplier=-1), reads=["cmask"], writes=["cmask"])
            S.op("pool", lambda e: e.memset(Ust_bf[:], 1.0), writes=["Ust"])
            S.op("pool", lambda e: e.affine_select(out=Ust_bf[:], in_=Ust_bf[:], pattern=[[1, 128]], compare_op=ALU.is_gt,
                                                    fill=0.0, base=0, channel_multiplier=-1), reads=["Ust"], writes=["Ust"])
            S.op("pool", lambda e: e.memset(ones_bf[:], 1.0), writes=["ones_bf"])
            S.op("pool", lambda e: e.iota(ecol[:], pattern=[[CAP, NE]], base=0, channel_multiplier=0,
                                          allow_small_or_imprecise_dtypes=True), writes=["ecol"])
            S.op("pool", lambda e: e.memset(base_rep[:], 0.0), writes=["base_rep"])
            S.op("pool", lambda e: e.memset(Sst[:], 0.0), writes=["Sst"])

            zt = sb(E1, "zt", [128, 1024], BF16)
            S.op("pool", lambda e: e.memset(zt[:], 0.0), writes=["zt"])
            XSv = XS.rearrange("(p r) d -> p (r d)", p=128)
            zfill = [("x", z0) for z0 in range(0, (NROWS // 128) * D, 1024)]
            YSv = YS.rearrange("(p r) d -> p (r d)", p=128)
            zfill += [("y", z0) for z0 in range(0, (NROWS // 128) * D, 512)]
            zt32 = zt[:].bitcast(F32)
            def ld(dst, src, key):
                S.op("sp", lambda e: e.dma_start(out=dst, in_=src), writes=[key], chan="ld_" + key)

            ld(pmask[:], pmask_d, "pmask")
            ld(c_col[:], c_col_d, "c_col")
            ld(ada_b_col[:], ada_b_col_d, "ada_b_col")
            ld(gate1_rep[:], ada_b_d[2 * D:3 * D].partition_broadcast(128), "gate_rep0")
            ld(gate2_rep[:], ada_b_d[5 * D:6 * D].partition_broadcast(128), "gate_rep1")
            ld(SH2_rep[:], ada_b_d[3 * D:4 * D].partition_broadcast(128), "gate_rep2")
            ld(xh2f[:], ada_b_d[4 * D:5 * D].partition_broadcast(128), "gate_rep3")
            ld(G2_rep[:], g2_row_d.partition_broadcast(128), "g2row")
            ld(g1_col[:], g1_col_d, "g1_col")
            ld(lb_rep[:], lbp_d[0].partition_broadcast(128), "lbp0")
            ld(oml_rep[:], lbp_d[1].partition_broadcast(128), "lbp1")
            ld(hg_rep[:], hg_d.partition_broadcast(128), "hg_rep")
            ld(lng_rep[:], lng_d.partition_broadcast(128), "lng_rep")
            ld(lnb_rep[:], lnb_d.partition_broadcast(128), "lnb_rep")
            ld(gng_rep[:], gng_d.partition_broadcast(128), "gng_rep")
            ld(wsT_f[:], wsT_d, "wsT_f")
            ld(bs_col[:], bs_col_d, "bs_col")
            ld(rw[:], rw_d, "rw")
            ld(rb_rep[:], rb_d.partition_broadcast(128), "rb_rep")
            ld(fg_rep[:], fg_d.partition_broadcast(128), "fg_rep")

            S.op("act", lambda e: e.activation(out=c_sig[:], in_=c_col[:], func=AF.Sigmoid), reads=["c_col"], writes=["c_sig"])
            S.op("dve", lambda e: e.tensor_tensor(out=c_act[:], in0=c_col[:], in1=c_sig[:], op=ALU.mult),
                 reads=["c_col", "c_sig"], writes=["c_act"])
            S.op("dve", lambda e: e.tensor_copy(c_rep[:], bc(c_act[:, :].unsqueeze(2), [128, 8, 128])),
                 reads=["c_act"], writes=["c_rep"])
            S.op("dve", lambda e: e.tensor_tensor(out=lb_rep[:], in0=lb_rep[:], in1=oml_rep[:], op=ALU.subtract),
                 reads=["lbp0", "lbp1"], writes=["lbd", "lbp0"])
            S.op("act", lambda e: e.activation(out=oml_rep[:], in_=lb_rep[:], func=AF.Sigmoid, scale=-1.0),
                 reads=["lbd", "lbp1"], writes=["oml_rep", "lbp1"])
            S.op("act", lambda e: e.activation(out=lb_rep[:], in_=lb_rep[:], func=AF.Sigmoid),
                 reads=["lbd", "oml_rep"], writes=["lbd", "lb_rep"])
            S.op("pool", lambda e: e.affine_select(out=wsT_bf[:], in_=wsT_f[:], pattern=[[0, 4], [1, 128]],
                                                    compare_op=ALU.is_ge, fill=0.0, base=0, channel_multiplier=-1),
                 reads=["wsT_f"], writes=["wsT_bf"])

            CW = 256

            def mod_chunk(jj):
                slot = jj % 2
                grp, off = (jj * CW) // D, (jj * CW) % D
                S.op("pool", lambda e, jj=jj, slot=slot: e.dma_start(
                    out=adaw_sb[slot][:], in_=ada_w[:, jj * CW:(jj + 1) * CW].rearrange("(kc p) n -> p kc n", p=128)),
                    writes=[f"adaw{slot}"], chan=f"ld_adaw{slot}")
                for kc in range(8):
                    S.op("pe", lambda e, kc=kc, slot=slot: e.matmul(pD[:, 0:CW], c_rep[:, kc, :], adaw_sb[slot][:, kc, :],
                                                                   start=(kc == 0), stop=(kc == 7)),
                         reads=["c_rep", f"adaw{slot}"], writes=["pD"])
                if grp >= 2:
                    which = {2: 0, 5: 1, 3: 2, 4: 3}[grp]
                    dst = [gate1_rep, gate2_rep, SH2_rep, xh2f][which]
                    S.op("dve", lambda e, dst=dst, off=off: e.tensor_tensor(out=dst[:, off:off + CW], in0=pD[:, 0:CW], in1=dst[:, off:off + CW], op=ALU.add),
                         reads=["pD", f"gate_rep{which}"], writes=[f"gate_rep{which}_{off // 512}"])
                else:
                    S.op("dve", lambda e: e.tensor_copy(mod_rep[:], pD[:, 0:CW]), reads=["pD"], writes=["mod_rep"])
                    for t4 in range(CW // 128):
                        S.op("pe", lambda e, t4=t4: e.transpose(pH[:, t4, :], mod_rep[:, t4 * 128:(t4 + 1) * 128], ident_f[:]),
                             reads=["mod_rep", "ident_f"], writes=["pH"])
                    c0 = jj * (CW // 128)
                    S.op("dve", lambda e, c0=c0: e.tensor_tensor(out=modT[:, c0:c0 + CW // 128], in0=pH[:, 0:CW // 128, 0],
                                                                 in1=ada_b_col[:, c0:c0 + CW // 128], op=ALU.add),
                         reads=["pH", "ada_b_col"], writes=[f"modT{jj}"])
            NEARLY = 2 * D // CW
            for jj in range(NEARLY):
                mod_chunk(jj)
            for kc in range(8):
                for hh in range(3):
                    S.op("pool", lambda e, kc=kc, hh=hh: e.dma_start(out=win_sb[:, kc, hh * 1024:(hh + 1) * 1024],
                                                                     in_=w_in[kc * 128:(kc + 1) * 128, hh * 1024:(hh + 1) * 1024]),
                         writes=[f"win_{kc}_{hh}"], chan="ld_win")
            S.op("pool", lambda e: e.dma_start(out=wout_sb[:], in_=w_out.rearrange("(kc p) n -> p kc n", p=128)),
                 writes=["wout"], chan="ld_wout")

            mk = [f"modT{jj}" for jj in range(8)]
            S.op("dve", lambda e: e.scalar_tensor_tensor(out=G1T[:], in0=modT[:, 8:16], scalar=1.0, in1=g1_col[:],
                                                         op0=ALU.add, op1=ALU.mult), reads=mk + ["g1_col"], writes=["G1T"])
            late = list(range(NEARLY, 6 * D // CW))

            def late_some(nmax):
                for _ in range(nmax):
                    if late:
                        mod_chunk(late.pop(0))
                        if not late:
                                    S.op("dve", lambda e: e.scalar_tensor_tensor(out=G2_rep[:], in0=xh2f[:], scalar=1.0, in1=G2_rep[:],
                                                                                 op0=ALU.add, op1=ALU.mult), reads=["gate_rep3_0", "gate_rep3_1", "g2row"], writes=["G2_rep", "xh2f"])

            SH1 = modT[:, 0:8]
            winall = [f"win_{kc}_{hh}" for kc in range(8) for hh in range(3)]

            def rstd_from(ssap, rsap, n, keys_r, key_w):
                S.op("act", lambda e: e.activation(out=rsap, in_=ssap, func=AF.Ln, scale=1.0 / n, bias=eps_t[:]),
                     reads=keys_r + ["eps_t"], writes=[key_w])
                S.op("act", lambda e: e.activation(out=rsap, in_=rsap, func=AF.Exp, scale=-0.5),
                     reads=[key_w], writes=[key_w])

            def stage1(n, i, prefix):
                p = n % 2
                xsrc = x_pre if prefix else x_main
                xi = xin[p]
                xk = f"xin{p}"
                S.op("sp", lambda e: e.dma_start(out=xi[:], in_=xsrc[i * 128:(i + 1) * 128, :]), writes=[xk], chan="ld_" + xk)
                S.op("act", lambda e: e.activation(out=junk[:], in_=xi[:], func=AF.Square, accum_out=ss[:, 0:1]),
                     reads=[xk], writes=["junk", "ss0"])
                if CUT <= 1:
                    return
                rstd_from(ss[:, 0:1], rs[:, 0:1], D, ["ss0"], "rs0")
                if CUT <= 2:
                    return
                S.op("dve", lambda e: e.tensor_scalar(out=xhat[:], in0=xi[:], scalar1=rs[:, 0:1], scalar2=None, op0=ALU.mult),
                     reads=[xk, "rs0"], writes=["xhat"])
                for kc in range(8):
                    S.op("pe", lambda e, kc=kc: e.transpose(pA[:, kc, :], xhat[:, kc * 128:(kc + 1) * 128], ident_bf[:]),
                         reads=["xhat", "ident_bf"], writes=["pA"])
                for kc in range(8):
                    S.op("act", lambda e, kc=kc: e.activation(out=hT[:, kc, :], in_=pA[:, kc, :], func=AF.Identity,
                                                              scale=G1T[:, kc:kc + 1], bias=modT[:, kc:kc + 1]),
                         reads=["pA", "G1T"] + mk, writes=["hT"])
                yield

                if CUT <= 3:
                    return

                def inproj(nb):
                    zp, zk = zbank()
                    for kc in range(8):
                        S.op("pe", lambda e, kc=kc, zp=zp: e.matmul(zp[:], hT[:, kc, :], win_sb[:, kc, nb * 512:(nb + 1) * 512],
                                                             start=(kc == 0), stop=(kc == 7)),
                             reads=["hT"] + winall, writes=[zk])
                    return zp, zk

                if prefix:
                    if not prefix:
                        zp, zk = inproj(0)
                        S.op("act", lambda e, zp=zp: e.activation(out=q_sb[:], in_=zp[:], func=AF.Identity), reads=[zk], writes=["q_sb"])
                    zp, zk = inproj(1)
                    S.op("act", lambda e, zp=zp: e.activation(out=sg[:], in_=zp[:], func=AF.Sigmoid), reads=[zk], writes=["sg"])
                    S.op("dve", lambda e: e.tensor_tensor(out=f_sb[:], in0=sg[:], in1=oml_rep[:], op=ALU.mult),
                         reads=["sg", "oml_rep"], writes=["f_sb"])
                    S.op("dve", lambda e: e.tensor_tensor(out=f_sb[:], in0=f_sb[:], in1=lb_rep[:], op=ALU.add),
                         reads=["f_sb", "lb_rep"], writes=["f_sb"])
                    S.op("act", lambda e: e.activation(out=g_sb[:], in_=f_sb[:], func=AF.Ln), reads=["f_sb"], writes=["g_sb"])
                    S.op("dve", lambda e: e.tensor_scalar(out=kk[:], in0=f_sb[:], scalar1=-1.0, scalar2=1.0, op0=ALU.mult, op1=ALU.add),
                         reads=["f_sb"], writes=["kk"])
                    if CUT <= 4:
                        return
                    zp, zk = inproj(2)
                    if "vdve" in VAR:
                        S.op("dve", lambda e, zp=zp: e.tensor_copy(v_bfs[p][:], zp[:]), reads=[zk], writes=[f"v_bf{p}"])
                    elif "vnone" in VAR:
                        pass
                    else:
                        S.op("act", lambda e, zp=zp: e.activation(out=v_bfs[p][:], in_=zp[:], func=AF.Identity), reads=[zk], writes=[f"v_bf{p}"])
                    if CUT <= 5:
                        return
                    S.op("pe", lambda e: e.matmul(pD[:], Lst[:], g_sb[:], start=True, stop=True), reads=["Lst", "g_sb"], writes=["pD"])
                    for h in range(4):
                        S.op("pe", lambda e, h=h: e.matmul(pE[:, 2 * h:2 * h + 2], g_sb[:, h * 128:(h + 1) * 128], cones[:],
                                                           start=True, stop=True), reads=["g_sb", "cones"], writes=["pE"])
                    if CUT <= 6:
                        return
                    S.op("act", lambda e: e.activation(out=Eexp[:], in_=pD[:], func=AF.Exp), reads=["pD"], writes=["Eexp"])
                    S.op("act", lambda e: e.activation(out=decs[p][:], in_=pE[:, 0:8], func=AF.Exp), reads=["pE"], writes=[f"dec{p}"])
                    S.op("dve", lambda e: e.tensor_tensor(out=Kd_bfs[p][:], in0=kk[:], in1=Eexp[:], op=ALU.mult),
                         reads=["kk", "Eexp"], writes=[f"Kd_bf{p}"])
                    if not prefix:
                        S.op("act", lambda e: e.activation(out=Eexp[:], in_=pD[:], func=AF.Exp, scale=-1.0), reads=["pD"], writes=["Eexp"])
                        S.op("dve", lambda e: e.tensor_tensor(out=Qm_bfs[p][:], in0=q_sb[:], in1=Eexp[:], op=ALU.mult),
                             reads=["q_sb", "Eexp"], writes=[f"Qm_bf{p}"])
                        yield
                        zp, zk = inproj(3)
                        S.op("act", lambda e, zp=zp: e.activation(out=sg[:], in_=zp[:], func=AF.Sigmoid), reads=[zk], writes=["sg"])
                        S.op("dve", lambda e, zp=zp: e.tensor_tensor(out=ggs[p][:], in0=zp[:], in1=sg[:], op=ALU.mult), reads=[zk, "sg"], writes=[f"gg{p}"])
                        S.op("dve", lambda e: e.tensor_tensor(out=ggs[p][:], in0=ggs[p][:], in1=hg_rep[:], op=ALU.mult),
                             reads=[f"gg{p}", "hg_rep"], writes=[f"gg{p}"])
                        zp, zk = inproj(4)
                        S.op("act", lambda e, zp=zp: e.activation(out=u_sbs[p][:], in_=zp[:], func=AF.Gelu), reads=[zk], writes=[f"u_sb{p}"])
                        yield
                        zp, zk = inproj(5)
                        S.op("act", lambda e, zp=zp: e.activation(out=vv[:], in_=zp[:], func=AF.Gelu), reads=[zk], writes=["vv"])
                        S.op("dve", lambda e: e.bn_stats(bst[:], vv[:]), reads=["vv"], writes=["bst"])
                        S.op("dve", lambda e: e.bn_aggr(bag[:], bst[:]), reads=["bst"], writes=["bag"])
                        rstd_from(bag[:, 1:2], rs[:, 1:2], 1.0, ["bag"], "rs1")
                        S.op("dve", lambda e: e.scalar_tensor_tensor(out=nmr[:], in0=bag[:, 0:1], scalar=-1.0, in1=rs[:, 1:2],
                                                                     op0=ALU.mult, op1=ALU.mult), reads=["bag", "rs1"], writes=["nmr"])
                        S.op("act", lambda e: e.activation(out=vn[:], in_=vv[:], func=AF.Identity, scale=rs[:, 1:2], bias=nmr[:]),
                             reads=["vv", "rs1", "nmr"], writes=["vn"])
                        S.op("dve", lambda e: e.tensor_tensor(out=vn[:], in0=vn[:], in1=lng_rep[:], op=ALU.mult),
                             reads=["vn", "lng_rep"], writes=["vn"])
                        S.op("dve", lambda e: e.tensor_tensor(out=vn_bfs[p][:], in0=vn[:], in1=lnb_rep[:], op=ALU.add),
                             reads=["vn", "lnb_rep"], writes=[f"vn_bf{p}"])
                else:
                    z0, k0 = inproj(0)
                    z1, k1 = inproj(1)
                    S.op("act", lambda e: e.activation(out=q_sb[:], in_=z0[:], func=AF.Identity), reads=[k0], writes=["q_sb"])
                    S.op("act", lambda e: e.activation(out=sg[:], in_=z1[:], func=AF.Sigmoid), reads=[k1], writes=["sg"])
                    z2, k2 = inproj(2)
                    z3, k3 = inproj(3)
                    S.op("dve", lambda e: e.tensor_tensor(out=f_sb[:], in0=sg[:], in1=oml_rep[:], op=ALU.mult),
                         reads=["sg", "oml_rep"], writes=["f_sb"])
                    S.op("dve", lambda e: e.tensor_tensor(out=f_sb[:], in0=f_sb[:], in1=lb_rep[:], op=ALU.add),
                         reads=["f_sb", "lb_rep"], writes=["f_sb"])
                    S.op("act", lambda e: e.activation(out=v_bfs[p][:], in_=z2[:], func=AF.Identity), reads=[k2], writes=[f"v_bf{p}"])
                    S.op("act", lambda e: e.activation(out=sg2[:], in_=z3[:], func=AF.Sigmoid), reads=[k3], writes=["sg2"])
                    S.op("dve", lambda e: e.tensor_tensor(out=ggs[p][:], in0=z3[:], in1=sg2[:], op=ALU.mult), reads=[k3, "sg2"], writes=[f"gg{p}"])
                    S.op("dve", lambda e: e.tensor_scalar(out=kk[:], in0=f_sb[:], scalar1=-1.0, scalar2=1.0, op0=ALU.mult, op1=ALU.add),
                         reads=["f_sb"], writes=["kk"])
                    S.op("dve", lambda e: e.tensor_tensor(out=ggs[p][:], in0=ggs[p][:], in1=hg_rep[:], op=ALU.mult),
                         reads=[f"gg{p}", "hg_rep"], writes=[f"gg{p}"])
                    yield
                    z4, k4 = inproj(4)
                    z5, k5 = inproj(5)
                    S.op("act", lambda e: e.activation(out=u_sbs[p][:], in_=z4[:], func=AF.Gelu), reads=[k4], writes=[f"u_sb{p}"])
                    S.op("act", lambda e: e.activation(out=vv[:], in_=z5[:], func=AF.Gelu), reads=[k5], writes=["vv"])
                    S.op("act", lambda e: e.activation(out=g_sb[:], in_=f_sb[:], func=AF.Ln), reads=["f_sb"], writes=["g_sb"])
                    S.op("dve", lambda e: e.bn_stats(bst[:], vv[:]), reads=["vv"], writes=["bst"])
                    S.op("dve", lambda e: e.bn_aggr(bag[:], bst[:]), reads=["bst"], writes=["bag"])
                    S.op("pe", lambda e: e.matmul(pD[:], Lst[:], g_sb[:], start=True, stop=True), reads=["Lst", "g_sb"], writes=["pD"])
                    for h in range(4):
                        S.op("pe", lambda e, h=h: e.matmul(pE[:, 2 * h:2 * h + 2], g_sb[:, h * 128:(h + 1) * 128], cones[:],
                                                           start=True, stop=True), reads=["g_sb", "cones"], writes=["pE"])
                    rstd_from(bag[:, 1:2], rs[:, 1:2], 1.0, ["bag"], "rs1")
                    S.op("act", lambda e: e.activation(out=Eexp[:], in_=pD[:], func=AF.Exp), reads=["pD"], writes=["Eexp"])
                    S.op("act", lambda e: e.activation(out=decs[p][:], in_=pE[:, 0:8], func=AF.Exp), reads=["pE"], writes=[f"dec{p}"])
                    S.op("dve", lambda e: e.scalar_tensor_tensor(out=nmr[:], in0=bag[:, 0:1], scalar=-1.0, in1=rs[:, 1:2],
                                                                 op0=ALU.mult, op1=ALU.mult), reads=["bag", "rs1"], writes=["nmr"])
                    S.op("dve", lambda e: e.tensor_tensor(out=Kd_bfs[p][:], in0=kk[:], in1=Eexp[:], op=ALU.mult),
                         reads=["kk", "Eexp"], writes=[f"Kd_bf{p}"])
                    S.op("act", lambda e: e.activation(out=Eexp[:], in_=pD[:], func=AF.Exp, scale=-1.0), reads=["pD"], writes=["Eexp"])
                    S.op("act", lambda e: e.activation(out=vn[:], in_=vv[:], func=AF.Identity, scale=rs[:, 1:2], bias=nmr[:]),
                         reads=["vv", "rs1", "nmr"], writes=["vn"])
                    S.op("dve", lambda e: e.tensor_tensor(out=Qm_bfs[p][:], in0=q_sb[:], in1=Eexp[:], op=ALU.mult),
                         reads=["q_sb", "Eexp"], writes=[f"Qm_bf{p}"])
                    S.op("dve", lambda e: e.tensor_tensor(out=vn[:], in0=vn[:], in1=lng_rep[:], op=ALU.mult),
                         reads=["vn", "lng_rep"], writes=["vn"])
                    S.op("dve", lambda e: e.tensor_tensor(out=vn_bfs[p][:], in0=vn[:], in1=lnb_rep[:], op=ALU.add),
                         reads=["vn", "lnb_rep"], writes=[f"vn_bf{p}"])

            def stage2(n, i, prefix):
                p = n % 2
                xi = xin[p]
                xk = f"xin{p}"
                if not prefix:
                    for h in range(4):
                        S.op("pe", lambda e, h=h: e.transpose(pA[:, h, :], Qm_bfs[p][:, h * 128:(h + 1) * 128], ident_bf[:]),
                             reads=[f"Qm_bf{p}", "ident_bf"], writes=["pA"])
                    for h in range(4):
                        S.op("pe", lambda e, h=h: e.transpose(pA[:, 4 + h, :], Kd_bfs[p][:, h * 128:(h + 1) * 128], ident_bf[:]),
                             reads=[f"Kd_bf{p}", "ident_bf"], writes=["pA"])
                    S.op("act", lambda e: e.activation(out=QKT[:], in_=pA[:], func=AF.Identity), reads=["pA"], writes=["QKT"])
                    for c in range(2):
                        cs = slice(c * 64, (c + 1) * 64)
                        for h in range(4):
                            S.op("pe", lambda e, h=h, cs=cs: e.matmul(pE[cs, 64 + h * 64:128 + h * 64], QKT[:, 4 + h, cs], QKT[:, h, cs],
                                                                      start=True, stop=True), reads=["QKT"], writes=["pE"])
                    S.op("dve", lambda e: e.tensor_tensor(out=scm[:], in0=pE[:, 64:320].rearrange("p (h t) -> p h t", h=4),
                                                          in1=cmask[:], op=ALU.mult), reads=["pE", "cmask"], writes=["scm"])
                yield
                for c in range(2):
                    cs = slice(c * 64, (c + 1) * 64)
                    S.op("dve", lambda e, c=c: e.tensor_tensor(out=Sst[:], in0=Sst[:],
                                                               in1=bc(decs[p][:, c::2].unsqueeze(2), [128, 4, 128]), op=ALU.mult),
                         reads=["Sst", f"dec{p}"], writes=["Sst"])
                    if not prefix:
                        S.op("act", lambda e: e.activation(out=Sp_bf[:], in_=Sst[:], func=AF.Identity), reads=["Sst"], writes=["Sp_bf"])
                        for h in range(4):
                            hs = slice(h * 128, (h + 1) * 128)
                            S.op("pe", lambda e, h=h, cs=cs, hs=hs: e.matmul(pF[cs, hs], QKT[:, h, cs], Sp_bf[:, h, :], start=True, stop=False),
                                 reads=["QKT", "Sp_bf"], writes=["pF"])
                            S.op("pe", lambda e, h=h, cs=cs, hs=hs: e.matmul(pF[cs, hs], scm[cs, h, :], v_bfs[p][cs, hs], start=False, stop=True),
                                 reads=["scm", f"v_bf{p}"], writes=["pF"])
                    for h in range(4):
                        hs = slice(h * 128, (h + 1) * 128)
                        S.op("pe", lambda e, cs=cs, hs=hs: e.matmul(pG[:, hs], Kd_bfs[p][cs, hs], v_bfs[p][cs, hs], start=True, stop=True),
                             reads=[f"Kd_bf{p}", f"v_bf{p}"], writes=["pG"])
                    S.op("dve", lambda e: e.tensor_tensor(out=Sst[:].rearrange("p h v -> p (h v)"), in0=Sst[:].rearrange("p h v -> p (h v)"),
                                                          in1=pG[:], op=ALU.add), reads=["Sst", "pG"], writes=["Sst"])
                if prefix:
                    return
                yield
                S.op("act", lambda e: e.activation(out=o_t[:], in_=pF[:], func=AF.Identity), reads=["pF"], writes=["o_t"])
                for h in range(4):
                    S.op("act", lambda e, h=h: e.activation(out=junk[:, h * 128:(h + 1) * 128], in_=o_t[:, h * 128:(h + 1) * 128],
                                                            func=AF.Square, accum_out=ss[:, 2 + h:3 + h]),
                         reads=["o_t"], writes=["junk", f"ssh{h}"])
                rstd_from(ss[:, 2:6], rs[:, 2:6], 128.0, [f"ssh{h}" for h in range(4)], "rsh")
                S.op("dve", lambda e: e.tensor_tensor(out=o_t[:].rearrange("p (h v) -> p h v", h=4), in0=o_t[:].rearrange("p (h v) -> p h v", h=4),
                                                      in1=bc(rs[:, 2:6].unsqueeze(2), [128, 4, 128]), op=ALU.mult),
                     reads=["o_t", "rsh"], writes=["o_t"])
                S.op("dve", lambda e: e.tensor_tensor(out=cat[:, 0:512], in0=o_t[:], in1=ggs[p][:], op=ALU.mult),
                     reads=["o_t", f"gg{p}"], writes=["cat_h"])
                yield
                zp, zk = zbank()
                for g in range(4):
                    gs = slice(g * 128, (g + 1) * 128)
                    S.op("pe", lambda e, g=g, gs=gs, zp=zp: e.matmul(zp[:, gs], wsT_bf[:, g, :], vn_bfs[p][:, gs], start=True, stop=True),
                         reads=["wsT_bf", f"vn_bf{p}"], writes=[zk])
                for g in range(4):
                    gs = slice(g * 128, (g + 1) * 128)
                    S.op("dve", lambda e, g=g, gs=gs, zp=zp: e.scalar_tensor_tensor(out=yg[:, gs], in0=zp[:, gs], scalar=bs_col[:, g:g + 1],
                                                                                   in1=u_sbs[p][:, gs], op0=ALU.add, op1=ALU.mult),
                         reads=[zk, "bs_col", f"u_sb{p}"], writes=["yg"])
                for g in range(4):
                    S.op("act", lambda e, g=g: e.activation(out=junk[:, g * 128:(g + 1) * 128], in_=yg[:, g * 128:(g + 1) * 128],
                                                            func=AF.Square, accum_out=ss[:, 6 + g:7 + g]),
                         reads=["yg"], writes=["junk", f"ssg{g}"])
                rstd_from(ss[:, 6:10], rs[:, 6:10], 128.0, [f"ssg{g}" for g in range(4)], "rsg")
                S.op("dve", lambda e: e.tensor_tensor(out=yg[:].rearrange("p (h v) -> p h v", h=4), in0=yg[:].rearrange("p (h v) -> p h v", h=4),
                                                      in1=bc(rs[:, 6:10].unsqueeze(2), [128, 4, 128]), op=ALU.mult),
                     reads=["yg", "rsg"], writes=["yg"])
                S.op("dve", lambda e: e.tensor_tensor(out=cat[:, 512:1024], in0=yg[:], in1=gng_rep[:], op=ALU.mult),
                     reads=["yg", "gng_rep"], writes=["cat_g"])
                yield
                for kc in range(8):
                    S.op("pe", lambda e, kc=kc: e.transpose(pA[:, kc, :], cat[:, kc * 128:(kc + 1) * 128], ident_bf[:]),
                         reads=["cat_h", "cat_g", "ident_bf"], writes=["pA"])
                S.op("act", lambda e: e.activation(out=catT[:], in_=pA[:], func=AF.Identity), reads=["pA"], writes=["catT"])
                x1 = x1t[p]
                x1k = f"x1t{p}"
                for half in range(2):
                    zp, zk = zbank()
                    hsl = slice(half * 512, (half + 1) * 512)
                    for kc in range(8):
                        S.op("pe", lambda e, kc=kc, zp=zp, hsl=hsl: e.matmul(zp[:], catT[:, kc, :], wout_sb[:, kc, hsl],
                                                                             start=(kc == 0), stop=(kc == 7)),
                             reads=["catT", "wout"], writes=[zk])
                    S.op("dve", lambda e, zp=zp, hsl=hsl: e.tensor_tensor(out=x1[:, hsl], in0=zp[:], in1=gate1_rep[:, hsl], op=ALU.mult),
                         reads=[zk, f"gate_rep0_{half}"], writes=[x1k + f"_{half}"])
                    S.op("dve", lambda e, hsl=hsl: e.tensor_tensor(out=x1[:, hsl], in0=x1[:, hsl], in1=xi[:, hsl], op=ALU.add),
                         reads=[x1k + f"_{half}", xk], writes=[x1k + f"_{half}"])
                x1keys = [x1k + "_0", x1k + "_1"]
                S.op("sp", lambda e: e.dma_start(out=x1s[i * 128:(i + 1) * 128, :], in_=x1[:]), reads=x1keys, writes=[f"x1s{i}"],
                     chan="st_" + x1k)
                yield
                S.op("act", lambda e: e.activation(out=junk[:], in_=x1[:], func=AF.Square, accum_out=ss[:, 10:11]),
                     reads=x1keys, writes=["junk", "ss10"])
                rstd_from(ss[:, 10:11], rs[:, 10:11], D, ["ss10"], "rs10")
                xb = xh2b[p]
                xbk = f"xh2b{p}"
                S.op("dve", lambda e: e.tensor_scalar(out=xh2f[:], in0=x1[:], scalar1=rs[:, 10:11], scalar2=None, op0=ALU.mult),
                     reads=x1keys + ["rs10"], writes=["xh2f"])
                S.op("dve", lambda e: e.tensor_tensor(out=xh2f[:], in0=xh2f[:], in1=G2_rep[:], op=ALU.mult), reads=["xh2f", "G2_rep"], writes=["xh2f"])
                S.op("dve", lambda e: e.tensor_tensor(out=xh2f[:], in0=xh2f[:], in1=SH2_rep[:], op=ALU.add), reads=["xh2f", "gate_rep2_0", "gate_rep2_1"], writes=["xh2f"])
                S.op("act", lambda e: e.activation(out=xb[:], in_=xh2f[:], func=AF.Identity), reads=["xh2f"], writes=[xbk])
                for hf in range(2):
                    for t4 in range(4):
                        kc = hf * 4 + t4
                        S.op("pe", lambda e, kc=kc, t4=t4: e.transpose(pH[:, t4, :], xh2f[:, kc * 128:(kc + 1) * 128], ident_f[:]),
                             reads=["xh2f", "ident_f"], writes=["pH"])
                    ksl = slice(hf * 4, hf * 4 + 4)
                    S.op("dve", lambda e, ksl=ksl: e.tensor_copy(h2T[:, ksl, :], pH[:]), reads=["pH"], writes=[f"h2T{hf}"])
                for kc in range(8):
                    S.op("pe", lambda e, kc=kc: e.matmul(pE[:, 320:352], h2T[:, kc, :], rw[:, kc, :], start=(kc == 0), stop=(kc == 7)),
                         reads=["h2T0", "h2T1", "rw"], writes=["pE"])
                lg_i = lgt_all[:, i, :]
                mx_i = mx8_all[:, i, :]
                gt_i = gates_all[:, i, :]
                ps_i = pos_all[:, i, :]
                S.op("dve", lambda e: e.tensor_tensor(out=lg_i, in0=pE[:, 320:352], in1=rb_rep[:], op=ALU.add),
                     reads=["pE", "rb_rep"], writes=[f"lgt{i}"])
                S.op("dve", lambda e: e.max(mx_i, lg_i), reads=[f"lgt{i}"], writes=[f"mx8{i}"])
                S.op("dve", lambda e: e.tensor_scalar(out=msk[:], in0=lg_i, scalar1=mx8_all[:, i, 3:4], scalar2=None, op0=ALU.is_ge),
                     reads=[f"lgt{i}", f"mx8{i}"], writes=["msk"])
                S.op("dve", lambda e: e.tensor_copy(msk_bf[:], msk[:]), reads=["msk"], writes=["msk_bf"])
                S.op("dve", lambda e: e.tensor_scalar(out=nmx[:], in0=mx8_all[:, i, 0:1], scalar1=-1.0, scalar2=None, op0=ALU.mult),
                     reads=[f"mx8{i}"], writes=["nmx"])
                S.op("act", lambda e: e.activation(out=eexp[:], in_=lg_i, func=AF.Exp, bias=nmx[:]), reads=[f"lgt{i}", "nmx"], writes=["eexp"])
                S.op("dve", lambda e: e.scalar_tensor_tensor(out=eexp[:], in0=eexp[:], scalar=1.0, in1=msk[:], op0=ALU.mult, op1=ALU.mult, accum_out=gsum[:]),
                     reads=["eexp", "msk"], writes=["eexp", "gsum"])
                S.op("dve", lambda e: e.reciprocal(gsum[:], gsum[:]), reads=["gsum"], writes=["gsum"])
                S.op("dve", lambda e: e.tensor_scalar(out=gt_i, in0=eexp[:], scalar1=gsum[:, 0:1], scalar2=None, op0=ALU.mult),
                     reads=["eexp", "gsum"], writes=[f"gates{i}"])
                S.op("pe", lambda e: e.matmul(pE[:, 352:384], Ust_bf[:], msk_bf[:], start=True, stop=True), reads=["Ust", "msk_bf"], writes=["pE"])
                S.op("pe", lambda e: e.matmul(pE[:, 384:416], ones_bf[:], msk_bf[:], start=True, stop=True), reads=["ones_bf", "msk_bf"], writes=["pE"])
                S.op("dve", lambda e: e.tensor_tensor(out=ps_i, in0=pE[:, 352:384], in1=base_rep[:], op=ALU.add),
                     reads=["pE", "base_rep"], writes=[f"pos{i}"])
                S.op("dve", lambda e: e.tensor_tensor(out=base_rep[:], in0=base_rep[:], in1=pE[:, 384:416], op=ALU.add),
                     reads=["pE", "base_rep"], writes=["base_rep"])
                S.op("sp", lambda e: e.dma_start(out=XH[i * 128:(i + 1) * 128, :], in_=xb[:]), reads=[xbk], writes=[f"XH{i}"], chan="st_" + xbk)

            jobs = [(i, True) for i in range(NPRE)] + [(i, False) for i in range(NMAIN)]

            def drive(gens):
                live = list(gens)
                while live:
                    for g in list(live):
                        try:
                            next(g)
                        except StopIteration:
                            live.remove(g)

            def stage2_gen(n):
                i, pre = jobs[n]
                if (not pre) and i == 0:
                    S.op("dve", lambda e: e.tensor_scalar(out=Sst[:], in0=Sst[:], scalar1=pmask[:, 0:1], scalar2=None, op0=ALU.mult),
                         reads=["Sst", "pmask"], writes=["Sst"])
                return stage2(n, i, pre)

            zper = (len(zfill) + max(len(jobs), 1) - 1) // max(len(jobs), 1)

            def zero_some():
                for _ in range(zper):
                    if zfill:
                        which, z0 = zfill.pop(0)
                        if which == "x":
                            S.op("sp", lambda e, z0=z0: e.dma_start(out=XSv[:, z0:z0 + 1024], in_=zt[:]), reads=["zt"], writes=[f"XSz{z0}"], chan="st_zt")
                        else:
                            S.op("sp", lambda e, z0=z0: e.dma_start(out=YSv[:, z0:z0 + 512], in_=zt32), reads=["zt"], writes=[f"YSz{z0}"], chan="st_zt")

            if jobs:
                drive([stage1(0, *jobs[0])])
            for n in range(len(jobs)):
                zero_some()
                late_some(len(late) if not jobs[n][1] else 1)
                gens = []
                if n + 1 < len(jobs):
                    gens.append(stage1(n + 1, *jobs[n + 1]))
                gens.append(stage2_gen(n))
                drive(gens)
            while zfill:
                zero_some() if zper else zfill.clear()
            late_some(len(late))
            S.flush()

        with ExitStack() as ED:
            nbe = sb(ED, "nbe", [128, NE])
            tmp32 = sb(ED, "tmp32", [128, NE])
            padded = sb(ED, "padded", [128, NE])
            ones32 = sb(ED, "ones32", [128, NE])
            pad_end = sb(ED, "pad_end", [128, NE])
            start_pad = sb(ED, "start_pad", [128, NE])
            kblk = sb(ED, "kblk", [128, NB])
            cmp = sb(ED, "cmp", [128, NB, NE])
            be_rep = sb(ED, "be_rep", [128, NB])
            kcp = sb(ED, "kcp", [128, 8])
            pcol = sb(ED, "pcol", [128, 1])
            widx_f = sb(ED, "widx_f", [128, NB, 8])
            bidx_f = sb(ED, "bidx_f", [128, NB])
            destf = sb(ED, "destf", [128, NE])
            oh = sb(ED, "oh", [128, NE])
            dk = sb(ED, "dk", [128, 4])
            junk32 = sb(ED, "junk32", [128, NE])
            xb2 = [sb(ED, f"xb2_{i}", [128, D], BF16) for i in range(2)]

            S.op("dve", lambda e: e.tensor_scalar(out=nbe[:], in0=base_rep[:], scalar1=0.5, scalar2=None, op0=ALU.is_gt), writes=["nbe"])
            for j in range(1, MAXBE):
                S.op("dve", lambda e, j=j: e.tensor_scalar(out=tmp32[:], in0=base_rep[:], scalar1=float(j * CAP) + 0.5, scalar2=None, op0=ALU.is_gt),
                     writes=["tmp32"])
                S.op("dve", lambda e: e.tensor_tensor(out=nbe[:], in0=nbe[:], in1=tmp32[:], op=ALU.add), reads=["nbe", "tmp32"], writes=["nbe"])
            S.op("dve", lambda e: e.tensor_scalar(out=padded[:], in0=nbe[:], scalar1=float(CAP), scalar2=None, op0=ALU.mult), reads=["nbe"], writes=["padded"])
            S.op("pool", lambda e: e.memset(ones32[:], 1.0), writes=["ones32"])
            nlive_f = sb(ED, "nlive_f", [128, 1])
            S.op("dve", lambda e: e.tensor_reduce(out=nlive_f[:], in_=nbe[:], axis=mybir.AxisListType.X, op=ALU.add), reads=["nbe"], writes=["nlive_f"])
            S.op("dve", lambda e: e.tensor_copy(nlive_i[:], nlive_f[:]), reads=["nlive_f"], writes=["nlive_i"])
            S.op("dve", lambda e: e.tensor_tensor_scan(out=pad_end[:], data0=ones32[:], data1=padded[:], initial=0.0, op0=ALU.mult, op1=ALU.add),
                 reads=["ones32", "padded"], writes=["pad_end"])
            S.op("dve", lambda e: e.tensor_tensor(out=start_pad[:], in0=pad_end[:], in1=padded[:], op=ALU.subtract),
                 reads=["pad_end", "padded"], writes=["start_pad"])
            S.op("pool", lambda e: e.iota(kblk[:], pattern=[[CAP, NB]], base=0, channel_multiplier=0, allow_small_or_imprecise_dtypes=True), writes=["kblk"])
            S.op("dve", lambda e: e.tensor_tensor(out=cmp[:], in0=bc(pad_end[:, :].unsqueeze(1), [128, NB, NE]),
                                                  in1=bc(kblk[:, :].unsqueeze(2), [128, NB, NE]), op=ALU.is_le),
                 reads=["pad_end", "kblk"], writes=["cmp"])
            S.op("dve", lambda e: e.tensor_reduce(out=be_rep[:], in_=cmp[:], axis=mybir.AxisListType.X, op=ALU.add), reads=["cmp"], writes=["be_rep"])
            S.op("dve", lambda e: e.tensor_scalar(out=be_rep[:], in0=be_rep[:], scalar1=float(NE - 1), scalar2=None, op0=ALU.min), reads=["be_rep"], writes=["be_rep"])
            S.op("pool", lambda e: e.iota(kcp[:], pattern=[[128, 8]], base=0, channel_multiplier=1, allow_small_or_imprecise_dtypes=True), writes=["kcp"])
            S.op("pool", lambda e: e.iota(pcol[:], pattern=[[0, 1]], base=0, channel_multiplier=1, allow_small_or_imprecise_dtypes=True), writes=["pcol"])
            S.op("dve", lambda e: e.tensor_scalar(out=bidx_f[:], in0=be_rep[:], scalar1=float(D), scalar2=None, op0=ALU.mult), reads=["be_rep"], writes=["bidx_f"])
            S.op("dve", lambda e: e.tensor_tensor(out=widx_f[:], in0=bc(bidx_f[:, :].unsqueeze(2), [128, NB, 8]),
                                                  in1=bc(kcp[:, :].unsqueeze(1), [128, NB, 8]), op=ALU.add),
                 reads=["bidx_f", "kcp"], writes=["widx_f"])
            S.op("dve", lambda e: e.tensor_copy(widx[:], widx_f[:]), reads=["widx_f"], writes=["widx"])
            S.op("dve", lambda e: e.tensor_scalar(out=bidx_f[:], in0=be_rep[:], scalar1=128.0, scalar2=pcol[:, 0:1], op0=ALU.mult, op1=ALU.add),
                 reads=["be_rep", "pcol", "widx_f"], writes=["bidx_f"])
            S.op("dve", lambda e: e.tensor_copy(bidx[:], bidx_f[:]), reads=["bidx_f"], writes=["bidx"])
            S.op("dve", lambda e: e.tensor_scalar(out=sel[:], in0=be_rep[:], scalar1=pcol[:, 0:1], scalar2=None, op0=ALU.is_equal),
                 reads=["be_rep", "pcol"], writes=["sel"])
            for i in range(NMAIN):
                xb = xb2[i % 2]
                xbk = f"xb2_{i % 2}"
                S.op("sp", lambda e, i=i, xb=xb: e.dma_start(out=xb[:], in_=XH[i * 128:(i + 1) * 128, :]), writes=[xbk], chan="ld_" + xbk)
                S.op("dve", lambda e, i=i: e.tensor_tensor(out=destf[:], in0=pos_all[:, i, :], in1=start_pad[:], op=ALU.add),
                     reads=["start_pad"], writes=["destf"])
                for k in range(4):
                    S.op("dve", lambda e, i=i, k=k: e.tensor_scalar(out=oh[:], in0=lgt_all[:, i, :], scalar1=mx8_all[:, i, k:k + 1], scalar2=None, op0=ALU.is_equal),
                         writes=["oh"])
                    S.op("dve", lambda e, k=k: e.scalar_tensor_tensor(out=junk32[:], in0=oh[:], scalar=1.0, in1=destf[:], op0=ALU.mult, op1=ALU.mult,
                                                                      accum_out=dk[:, k:k + 1]), reads=["oh", "destf"], writes=["junk32", f"dk{k}"])
                    S.op("dve", lambda e, i=i, k=k: e.scalar_tensor_tensor(out=junk32[:], in0=oh[:], scalar=1.0, in1=gates_all[:, i, :], op0=ALU.mult, op1=ALU.mult,
                                                                           accum_out=gate_all[:, i, k:k + 1]), reads=["oh"], writes=["junk32", f"gate_all{i}_{k}"])
                S.op("dve", lambda e, i=i: e.tensor_copy(dest_all[:, i, :], dk[:]), reads=[f"dk{k}" for k in range(4)], writes=[f"dest_all{i}"])
                for k in range(4):
                    S.op("pool", lambda e, i=i, k=k, xb=xb: e.indirect_dma_start(
                        out=XS[:, :], out_offset=bass.IndirectOffsetOnAxis(ap=dest_all[:, i, k:k + 1], axis=0),
                        in_=xb[:, :], in_offset=None, bounds_check=S.pool_bound, oob_is_err=False),
                        reads=[xbk, f"dest_all{i}"], writes=[f"XS_{i}_{k}"], chan="sc_" + xbk)
            S.flush()

        with ExitStack() as E2:
            wg_sb = [sb(E2, f"wg{i}", [128, 8, 2 * D], BF16) for i in range(2)]
            wd_sb = [sb(E2, f"wd{i}", [128, 8, D], BF16) for i in range(2)]
            xg = [sb(E2, f"xg{i}", [128, NJ, D], BF16) for i in range(2)]
            bgc = [sb(E2, f"bgc{i}", [128, 16]) for i in range(2)]
            sel_full = sb(E2, "sel_full", [32, NB, 128], BF16)
            bd_f = sb(E2, "bd_f", [32, D])
            bd_bf = sb(E2, "bd_bf", [32, D], BF16)
            XTs = [sb(E2, f"XT{i}", [128, 8, CAP], BF16) for i in range(2)]
            bgc7 = [sb(E2, f"bgc7_{i}", [128, 8]) for i in range(2)]
            AT = sb(E2, "AT", [128, 8, CAP], BF16)
            gmin = [sb(E2, f"gmin{i}", [128, CAP]) for i in range(2)]
            sgm = [sb(E2, f"sgm{i}", [128, CAP]) for i in range(2)]
            up1 = [sb(E2, f"up1{i}", [128, CAP]) for i in range(2)]
            yst = [sb(E2, f"yst{i}", [128, NJ, D]) for i in range(2)]
            qA = ps(E2, "qA", [128, 8, 128], BF16)
            qg = [ps(E2, f"qg{i}", [128, 512]) for i in range(2)]
            qu = [ps(E2, f"qu{i}", [128, 512]) for i in range(2)]
            qy = [ps(E2, f"qy{i}", [128, 512]) for i in range(2)]

            S.op("sp", lambda e: e.dma_start(out=bd_f[:], in_=bdn), writes=["bd_f"], chan="ld_bd_f")
            S.op("dve", lambda e: e.tensor_copy(bd_bf[:], bd_f[:]), reads=["bd_f"], writes=["bd_bf"])
            S.op("dve", lambda e: e.tensor_copy(sel_full[:], bc(sel[0:32, :].unsqueeze(2), [32, NB, 128])), writes=["sel_full"])

            def prefetch(k):
                sl = k % 2
                for kc in range(8):
                    S.op("pool", lambda e, kc=kc: e.indirect_dma_start(
                        out=wg_sb[sl][:, kc, :], out_offset=None, in_=wgu[:, :],
                        in_offset=bass.IndirectOffsetOnAxis(ap=widx[:, k, kc:kc + 1], axis=0),
                        bounds_check=S.pool_wbound, oob_is_err=False), writes=[f"wg{sl}_{kc}"], chan=f"ld_wg{sl}")
                for kc in range(8):
                    S.op("pool", lambda e, kc=kc: e.indirect_dma_start(
                        out=wd_sb[sl][:, kc, :], out_offset=None, in_=wdn[:, :],
                        in_offset=bass.IndirectOffsetOnAxis(ap=widx[:, k, kc:kc + 1], axis=0),
                        bounds_check=S.pool_wbound, oob_is_err=False), writes=[f"wd{sl}_{kc}"], chan=f"ld_wd{sl}")
                S.op("pool", lambda e: e.indirect_dma_start(
                    out=bgc[sl][:, :], out_offset=None, in_=bgu_tab[:, :],
                    in_offset=bass.IndirectOffsetOnAxis(ap=bidx[:, k:k + 1], axis=0),
                    bounds_check=S.pool_wbound, oob_is_err=False), writes=[f"bgc{sl}"], chan=f"ld_bgc{sl}")
                S.op("sp", lambda e: e.dma_start(out=xg[sl][:], in_=XS[k * CAP:(k + 1) * CAP, :].rearrange("(j p) d -> p j d", p=128)),
                     writes=[f"xg{sl}"], chan=f"ld_xg{sl}")

            if NB_RUN > 0:
                prefetch(0)

            def xprep(k):
                sl = k % 2
                XT = XTs[sl]
                for j in range(NJ):
                    for kc in range(8):
                        S.op("pe", lambda e, j=j, kc=kc: e.transpose(qA[:, kc, :], xg[sl][:, j, kc * 128:(kc + 1) * 128], ident_bf[:]),
                             reads=[f"xg{sl}"], writes=["qA"])
                    S.op("act", lambda e, j=j: e.activation(out=XT[:, :, j * 128:(j + 1) * 128], in_=qA[:], func=AF.Identity),
                         reads=["qA"], writes=[f"XT{sl}_{j}"])
                S.op("dve", lambda e: e.tensor_scalar(out=bgc7[sl][:], in0=bgc[sl][:, 8:16], scalar1=7.0, scalar2=None, op0=ALU.add),
                     reads=[f"bgc{sl}"], writes=[f"bgc7_{sl}"])

            def block(k):
                sl = k % 2
                XT = XTs[sl]
                if k + 1 < NB_RUN:
                    prefetch(k + 1)
                xtk = [f"XT{sl}_{j}" for j in range(NJ)]
                wgk = [f"wg{sl}_{kc}" for kc in range(8)]
                wdk = [f"wd{sl}_{kc}" for kc in range(8)]
                for fc in range(8):
                    b = fc % 2
                    for kc in range(8):
                        S.op("pe", lambda e, fc=fc, kc=kc, b=b: e.matmul(qg[b][:, 0:CAP], wg_sb[sl][:, kc, fc * 128:(fc + 1) * 128], XT[:, kc, :],
                                                                         start=(kc == 0), stop=(kc == 7)),
                             reads=xtk + wgk, writes=[f"qg{b}"])
                    for kc in range(8):
                        S.op("pe", lambda e, fc=fc, kc=kc, b=b: e.matmul(qu[b][:, 0:CAP], wg_sb[sl][:, kc, D + fc * 128:D + (fc + 1) * 128], XT[:, kc, :],
                                                                         start=(kc == 0), stop=(kc == 7)),
                             reads=xtk + wgk, writes=[f"qu{b}"])
                    S.op("dve", lambda e, fc=fc, b=b: e.tensor_scalar(out=gmin[b][:], in0=qg[b][:, 0:CAP], scalar1=bgc[sl][:, fc:fc + 1], scalar2=7.0,
                                                                      op0=ALU.add, op1=ALU.min), reads=[f"qg{b}", f"bgc{sl}"], writes=[f"gmin{b}"])
                    S.op("act", lambda e, b=b: e.activation(out=sgm[b][:], in_=gmin[b][:], func=AF.Silu, scale=1.702),
                         reads=[f"gmin{b}"], writes=[f"sgm{b}"])
                    S.op("act", lambda e, fc=fc, b=b: e.activation(out=up1[b][:], in_=qu[b][:, 0:CAP], func=AF.Relu, bias=bgc7[sl][:, fc:fc + 1]),
                         reads=[f"qu{b}", f"bgc7_{sl}"], writes=[f"up1{b}"])
                    S.op("dve", lambda e, b=b: e.tensor_scalar(out=up1[b][:], in0=up1[b][:], scalar1=14.0, scalar2=-6.0, op0=ALU.min, op1=ALU.add),
                         reads=[f"up1{b}"], writes=[f"up1{b}"])
                    S.op("dve", lambda e, fc=fc, b=b: e.scalar_tensor_tensor(out=AT[:, fc, :], in0=sgm[b][:], scalar=1.0 / 1.702, in1=up1[b][:],
                                                                             op0=ALU.mult, op1=ALU.mult),
                         reads=[f"up1{b}", f"sgm{b}"], writes=[f"AT{fc}"])
                if k + 1 < NB_RUN:
                    xprep(k + 1)
                atk = [f"AT{fc}" for fc in range(8)]
                ys = yst[sl]
                n = 0
                for j in range(NJ):
                    for half in range(2):
                        b = n % 2
                        n += 1
                        hsl = slice(half * 512, (half + 1) * 512)
                        for fc in range(8):
                            S.op("pe", lambda e, j=j, fc=fc, b=b, hsl=hsl: e.matmul(qy[b][:], AT[:, fc, j * 128:(j + 1) * 128], wd_sb[sl][:, fc, hsl],
                                                                                   start=(fc == 0), stop=False),
                                 reads=atk + wdk, writes=[f"qy{b}"])
                        S.op("pe", lambda e, b=b, hsl=hsl: e.matmul(qy[b][:], sel_full[:, k, :], bd_bf[:, hsl], start=False, stop=True),
                             reads=["bd_bf", "sel_full"], writes=[f"qy{b}"])
                        S.op("act", lambda e, j=j, b=b, hsl=hsl: e.activation(out=ys[:, j, hsl], in_=qy[b][:], func=AF.Identity),
                             reads=[f"qy{b}"], writes=[f"yst{sl}_{j}_{half}"])
                S.op("sp", lambda e: e.dma_start(out=YS[k * CAP:(k + 1) * CAP, :].rearrange("(j p) d -> p j d", p=128), in_=ys[:]),
                     reads=[f"yst{sl}_{j}_{h}" for j in range(NJ) for h in range(2)], writes=[f"YS{k}"], chan=f"st_yst{sl}")

            for i2 in range(2):
                S.op("pool", lambda e, i2=i2: e.memset(yst[i2][:], 0.0), writes=[f"yst{i2}_{j}_{h}" for j in range(NJ) for h in range(2)])
            if NB_RUN > 0:
                xprep(0)
            S.grp_ap = nlive_i[0:1, 0:1]
            for k in range(NB_RUN):
                S.grp = k if SKIP else None
                block(k)
            S.grp = None
            S.flush()

        with ExitStack() as E3:
            x1c = [sb(E3, f"x1c{i}", [128, D]) for i in range(2)]
            yk = [[sb(E3, f"yk{i}_{k}", [128, D]) for k in range(4)] for i in range(2)]
            acc = [sb(E3, f"acc{i}", [128, D]) for i in range(2)]
            acc2 = [sb(E3, f"accb{i}", [128, D]) for i in range(2)]
            ot = [sb(E3, f"ot{i}", [128, D]) for i in range(2)]
            cjunk = sb(E3, "cjunk", [128, D], BF16)
            css = sb(E3, "css", [128, 2])
            crs = sb(E3, "crs", [128, 2])
            for i2 in range(2):
                for k in range(4):
                    S.op("pool", lambda e, i2=i2, k=k: e.memset(yk[i2][k][:], 0.0), writes=[f"yk{i2}_{k}"])
            for i in range(NMAIN if COMB else 0):
                b = i % 2
                S.op("sp", lambda e, i=i, b=b: e.dma_start(out=x1c[b][:], in_=x1s[i * 128:(i + 1) * 128, :]), writes=[f"x1c{b}"], chan=f"ld_x1c{b}")
                for k in range(4):
                    S.op("pool", lambda e, i=i, b=b, k=k: e.indirect_dma_start(
                        out=yk[b][k][:, :], out_offset=None, in_=YS[:, :],
                        in_offset=bass.IndirectOffsetOnAxis(ap=dest_all[:, i, k:k + 1], axis=0),
                        bounds_check=S.pool_bound, oob_is_err=False), writes=[f"yk{b}_{k}"], chan=f"ld_yk{b}_{k}")
                S.op("act", lambda e, i=i, b=b: e.activation(out=acc[b][:], in_=yk[b][0][:], func=AF.Identity, scale=gate_all[:, i, 0:1]),
                     reads=[f"yk{b}_0"], writes=[f"acc{b}"])
                for k in range(1, 4):
                    S.op("dve", lambda e, i=i, b=b, k=k: e.scalar_tensor_tensor(out=acc[b][:], in0=yk[b][k][:], scalar=gate_all[:, i, k:k + 1], in1=acc[b][:],
                                                                               op0=ALU.mult, op1=ALU.add), reads=[f"yk{b}_{k}", f"acc{b}"], writes=[f"acc{b}"])
                S.op("dve", lambda e, b=b: e.tensor_tensor(out=acc2[b][:], in0=acc[b][:], in1=gate2_rep[:], op=ALU.mult), reads=[f"acc{b}"], writes=[f"accb{b}"])
                S.op("dve", lambda e, b=b: e.tensor_tensor(out=acc2[b][:], in0=acc2[b][:], in1=x1c[b][:], op=ALU.add), reads=[f"accb{b}", f"x1c{b}"], writes=[f"accb{b}"])
                S.op("act", lambda e, b=b: e.activation(out=cjunk[:], in_=acc2[b][:], func=AF.Square, accum_out=css[:, b:b + 1]), reads=[f"accb{b}"], writes=["cjunk", f"css{b}"])
                S.op("act", lambda e, b=b: e.activation(out=crs[:, b:b + 1], in_=css[:, b:b + 1], func=AF.Ln, scale=1.0 / D, bias=eps_t[:]), reads=[f"css{b}"], writes=[f"crs{b}"])
                S.op("act", lambda e, b=b: e.activation(out=crs[:, b:b + 1], in_=crs[:, b:b + 1], func=AF.Exp, scale=-0.5), reads=[f"crs{b}"], writes=[f"crs{b}"])
                S.op("dve", lambda e, b=b: e.scalar_tensor_tensor(out=ot[b][:], in0=acc2[b][:], scalar=crs[:, b:b + 1], in1=fg_rep[:], op0=ALU.mult, op1=ALU.mult),
                     reads=[f"accb{b}", f"crs{b}"], writes=[f"ot{b}"])
                S.op("sp", lambda e, i=i, b=b: e.dma_start(out=out_d[i * 128:(i + 1) * 128, :], in_=ot[b][:]), reads=[f"ot{b}"], writes=[f"out{i}"],
                     chan=f"st_ot{b}")
            S.flush()
    return nc


def make_in_maps(x, c, ada_w, ada_b, norm_mix_g, w_in, lb_params, hgrn_norm_g, gmlp_ln_g, gmlp_ln_b,
                 gmlp_ws, gmlp_bs, gmlp_norm_g, w_out, norm_ffn_g, router_w, router_b, w_gate_up,
                 b_gate_up, w_down, b_down, final_g):
    f = lambda a: np.ascontiguousarray(np.asarray(a, dtype=np.float32))
    x = f(x)
    c = f(c)
    col = lambda v: f(np.asarray(v, np.float32).reshape(-1, 128).T)
    shared = dict(
        ada_w=f(ada_w[0]), ada_b_col=col(ada_b[0]), ada_b=f(ada_b[0]),
        g1_col=col(norm_mix_g[0]), g2_row=f(norm_ffn_g[0]),
        w_in=f(w_in[0]), lbp=f(lb_params), hg=f(hgrn_norm_g[0]), lng=f(gmlp_ln_g[0]), lnb=f(gmlp_ln_b[0]),
        gng=f(gmlp_norm_g[0]),
        wsT=f(np.transpose(np.asarray(gmlp_ws[0], np.float32), (2, 0, 1))),
        bs_col=f(np.asarray(gmlp_bs[0], np.float32).T),
        w_out=f(w_out[0]),
        rw=f(np.asarray(router_w[0], np.float32).reshape(8, 128, NE).transpose(1, 0, 2)),
        rb=f(router_b[0]),
        wgu=f(np.asarray(w_gate_up[0], np.float32).reshape(NE * D, 2 * D)),
        bgu_tab=f(np.asarray(b_gate_up[0], np.float32).reshape(NE, 16, 128).transpose(0, 2, 1).reshape(NE * 128, 16)),
        wdn=f(np.asarray(w_down[0], np.float32).reshape(NE * D, D)), bdn=f(b_down[0]), fg=f(final_g),
    )
    maps = []
    for core in range(8):
        b, half = core // 2, core % 2
        m = dict(shared)
        m["x_main"] = f(x[b, half * TOK:(half + 1) * TOK])
        m["x_pre"] = f(x[b, 0:TOK])
        m["pmask"] = np.full((128, 1), float(half), np.float32)
        m["c_col"] = col(c[b])
        maps.append(m)
    return maps


def kernel(**inputs):
    maps = make_in_maps(**inputs)
    nc = build()
    res = run_bass_kernel_spmd(nc, maps, core_ids=list(range(8)))
    out = np.empty((4, 2 * TOK, D), np.float32)
    for core in range(8):
        b, half = core // 2, core % 2
        out[b, half * TOK:(half + 1) * TOK] = res.results[core]["out"]
    if DBG:
        kernel.dbg = [r for r in res.results]
    return out
```
